# Optimizing a Trainium2 kernel written in Bass

```python
import jax, jax.numpy as jnp
from jax import lax
import numpy as np

D_MODEL = 1024
BATCH = 2
SEQ = 16384
DEPTH = 1

N_MEM = 256
EPS = 1e-6
QBLK = 128
MLA_HEADS = 4
MLA_NOPE = 128
MLA_ROPE = 64
MLA_V = 128
MLA_Q_RANK = 256
MLA_KV_RANK = 128
ROPE_THETA = 10000.0
SWA_HEADS = 8
SWA_KV_HEADS = 2
SWA_HD = 64
WINDOW = 128
X_HEADS = 4
X_HD = 128
PEER_HEADS = 8
N_KEYS = 128
N_EXPERTS = N_KEYS * N_KEYS
PEER_QDIM = 256
PEER_HALF = PEER_QDIM // 2
PEER_TOPK = 16
PEER_CHUNK = 128

MLA_WIDTH = MLA_HEADS * MLA_V
SWA_WIDTH = SWA_HEADS * SWA_HD
MIX_WIDTH = MLA_WIDTH + SWA_WIDTH
SWA_KV_WIDTH = SWA_KV_HEADS * SWA_HD
IN_COLS = MLA_Q_RANK + MLA_KV_RANK + MLA_ROPE + SWA_WIDTH + 2 * SWA_KV_WIDTH
SPLITS = (MLA_Q_RANK,
          MLA_Q_RANK + MLA_KV_RANK,
          MLA_Q_RANK + MLA_KV_RANK + MLA_ROPE,
          MLA_Q_RANK + MLA_KV_RANK + MLA_ROPE + SWA_WIDTH,
          MLA_Q_RANK + MLA_KV_RANK + MLA_ROPE + SWA_WIDTH + SWA_KV_WIDTH)

kernel_name = 'hymba_mla_swa_peer_block'


def rmsnorm(x, g):
    xf = x.astype(jnp.float32)
    y = xf * lax.rsqrt(jnp.mean(xf * xf, axis=-1, keepdims=True) + EPS)
    return (y * g.astype(jnp.float32)).astype(x.dtype)


def rope(x, positions):
    half = x.shape[-1] // 2
    inv = ROPE_THETA ** (-jnp.arange(half, dtype=jnp.float32) / half)
    ang = positions.astype(jnp.float32)[..., None] * inv
    ang = ang.reshape(ang.shape[:2] + (1,) * (x.ndim - 3) + (half,))
    cos, sin = jnp.cos(ang), jnp.sin(ang)
    x1 = x[..., :half].astype(jnp.float32)
    x2 = x[..., half:].astype(jnp.float32)
    out = jnp.concatenate([x1 * cos - x2 * sin, x2 * cos + x1 * sin], axis=-1)
    return out.astype(x.dtype)


def alibi_slopes(n_heads):
    return 2.0 ** (-(8.0 / n_heads) * jnp.arange(1, n_heads + 1, dtype=jnp.float32))


def mla_attention(c_q, c_kv, k_rope, positions, q_a_norm, w_q_b, kv_a_norm, w_kv_b):
    B, S, _ = c_q.shape
    q = (rmsnorm(c_q, q_a_norm) @ w_q_b).reshape(B, S, MLA_HEADS, MLA_NOPE + MLA_ROPE)
    q_nope, q_rope = q[..., :MLA_NOPE], rope(q[..., MLA_NOPE:], positions)
    c = rmsnorm(c_kv, kv_a_norm)
    k_r = rope(k_rope, positions)
    w = w_kv_b.reshape(MLA_KV_RANK, MLA_HEADS, MLA_NOPE + MLA_V)
    w_uk, w_uv = w[..., :MLA_NOPE], w[..., MLA_NOPE:]
    q_lat = jnp.einsum('bshd,rhd->bshr', q_nope, w_uk)
    q_cat = jnp.concatenate([q_lat, q_rope], axis=-1)
    k_cat = jnp.concatenate([c, k_r], axis=-1)
    scale = (MLA_NOPE + MLA_ROPE) ** -0.5
    nblk = S // QBLK
    qb = q_cat.reshape(B, nblk, QBLK, MLA_HEADS, -1).transpose(1, 0, 2, 3, 4)
    k_idx = jnp.arange(S)

    def block(args):
        i, q_i = args
        s = jnp.einsum('bqhd,bkd->bhqk', q_i, k_cat).astype(jnp.float32) * scale
        q_idx = i * QBLK + jnp.arange(QBLK)
        s = jnp.where(k_idx[None, :] <= q_idx[:, None], s, -jnp.inf)
        p = jax.nn.softmax(s, axis=-1).astype(c.dtype)
        return jnp.einsum('bhqk,bkr->bqhr', p, c)

    o_lat = lax.map(block, (jnp.arange(nblk), qb))
    o_lat = o_lat.transpose(1, 0, 2, 3, 4).reshape(B, S, MLA_HEADS, MLA_KV_RANK)
    return jnp.einsum('bshr,rhd->bshd', o_lat, w_uv).reshape(B, S, MLA_WIDTH)


def swa_attention(q, k, v, sinks):
    B, S, _ = q.shape
    nblk = S // QBLK
    G = SWA_HEADS // SWA_KV_HEADS
    q = q.reshape(B, nblk, QBLK, SWA_KV_HEADS, G, SWA_HD)
    k = k.reshape(B, S, SWA_KV_HEADS, SWA_HD)
    v = v.reshape(B, S, SWA_KV_HEADS, SWA_HD)
    pad = jnp.zeros((B, QBLK, SWA_KV_HEADS, SWA_HD), k.dtype)
    kp = jnp.concatenate([pad, k], axis=1).reshape(B, nblk + 1, QBLK, SWA_KV_HEADS, SWA_HD)
    vp = jnp.concatenate([pad, v], axis=1).reshape(B, nblk + 1, QBLK, SWA_KV_HEADS, SWA_HD)
    k_band = jnp.concatenate([kp[:, :-1], kp[:, 1:]], axis=2)
    v_band = jnp.concatenate([vp[:, :-1], vp[:, 1:]], axis=2)
    s = jnp.einsum('bnqkgd,bnjkd->bnkgqj', q, k_band).astype(jnp.float32) * (SWA_HD ** -0.5)
    a = jnp.arange(QBLK)[:, None]
    j = jnp.arange(2 * QBLK)[None, :]
    dist = a + QBLK - j
    key_abs = jnp.arange(nblk)[:, None] * QBLK - QBLK + jnp.arange(2 * QBLK)[None, :]
    valid = ((dist >= 0) & (dist < WINDOW))[None] & (key_abs >= 0)[:, None, :]
    slopes = alibi_slopes(SWA_HEADS).reshape(SWA_KV_HEADS, G)
    s = s - slopes[:, :, None, None] * dist.astype(jnp.float32)
    s = jnp.where(valid[None, :, None, None], s, -jnp.inf)
    sink = sinks.astype(jnp.float32).reshape(SWA_KV_HEADS, G)[:, :, None, None]
    m = jnp.maximum(jnp.max(s, axis=-1, keepdims=True), sink)
    e = jnp.exp(s - m)
    p = e / (jnp.sum(e, axis=-1, keepdims=True) + jnp.exp(sink - m))
    o = jnp.einsum('bnkgqj,bnjkd->bnqkgd', p.astype(v.dtype), v_band)
    return o.reshape(B, S, SWA_WIDTH)


def cross_attention(h, mem, norm_mem, w_cq, w_ck, w_cv, w_co):
    B, S, _ = h.shape
    mn = rmsnorm(mem, norm_mem)
    q = (h @ w_cq).reshape(B, S, X_HEADS, X_HD)
    k = (mn @ w_ck).reshape(B, -1, X_HEADS, X_HD)
    v = (mn @ w_cv).reshape(B, -1, X_HEADS, X_HD)
    s = jnp.einsum('bshd,bmhd->bhsm', q, k).astype(jnp.float32) * (X_HD ** -0.5)
    p = jax.nn.softmax(s, axis=-1).astype(v.dtype)
    o = jnp.einsum('bhsm,bmhd->bshd', p, v).reshape(B, S, X_HEADS * X_HD)
    return o @ w_co


def peer_ffn(h, w_q, keys, u, v):
    B, S, D = h.shape
    q = (h @ w_q).reshape(B, S, PEER_HEADS, 2, PEER_HALF)
    sc = jnp.einsum('bshcd,hcnd->bshcn', q, keys).astype(jnp.float32)
    top_s, top_i = lax.top_k(sc, PEER_TOPK)
    cand_s = (top_s[..., 0, :, None] + top_s[..., 1, None, :]).reshape(B, S, PEER_HEADS, PEER_TOPK * PEER_TOPK)
    cand_i = (top_i[..., 0, :, None] * N_KEYS + top_i[..., 1, None, :]).reshape(B, S, PEER_HEADS, PEER_TOPK * PEER_TOPK)
    best_s, best_pos = lax.top_k(cand_s, PEER_TOPK)
    idx = jnp.take_along_axis(cand_i, best_pos, axis=-1)
    g = jax.nn.softmax(best_s, axis=-1).astype(h.dtype)
    nch = S // PEER_CHUNK
    h_c = h.reshape(B, nch, PEER_CHUNK, D).swapaxes(0, 1)
    idx_c = idx.reshape(B, nch, PEER_CHUNK, PEER_HEADS, PEER_TOPK).swapaxes(0, 1)
    g_c = g.reshape(B, nch, PEER_CHUNK, PEER_HEADS, PEER_TOPK).swapaxes(0, 1)

    def chunk(args):
        hc, ic, gc = args
        act = jax.nn.gelu(jnp.einsum('bcd,bchkd->bchk', hc, u[ic]), approximate=False)
        return jnp.einsum('bchk,bchkd->bcd', gc * act, v[ic])

    y = lax.map(chunk, (h_c, idx_c, g_c))
    return y.swapaxes(0, 1).reshape(B, S, D)


def setup_inputs(seed: int = 0) -> dict:
    key = jax.random.key(seed)
    ks = jax.random.split(key, 26)
    L, D = DEPTH, D_MODEL

    def nrm(k, shape, scale):
        return jax.random.normal(k, shape, jnp.float32) * scale

    def gain(k, shape):
        return 1.0 + 0.05 * jax.random.normal(k, shape, jnp.float32)

    start = jax.random.randint(ks[2], (BATCH, 1), 0, 4096, dtype=jnp.int32)
    positions = start + jnp.arange(SEQ, dtype=jnp.int32)[None, :]
    return {
        'x': nrm(ks[0], (BATCH, SEQ, D), 1.0),
        'mem': nrm(ks[1], (BATCH, N_MEM, D), 1.0),
        'positions': positions,
        'norm_mix': gain(ks[3], (L, D)),
        'w_in': nrm(ks[4], (L, D, IN_COLS), D ** -0.5),
        'q_a_norm': gain(ks[5], (L, MLA_Q_RANK)),
        'w_q_b': nrm(ks[6], (L, MLA_Q_RANK, MLA_HEADS * (MLA_NOPE + MLA_ROPE)), MLA_Q_RANK ** -0.5),
        'kv_a_norm': gain(ks[7], (L, MLA_KV_RANK)),
        'w_kv_b': nrm(ks[8], (L, MLA_KV_RANK, MLA_HEADS * (MLA_NOPE + MLA_V)), MLA_KV_RANK ** -0.5),
        'swa_sinks': nrm(ks[9], (L, SWA_HEADS), 1.0),
        'out_norm_mla': gain(ks[10], (L, MLA_WIDTH)),
        'out_norm_swa': gain(ks[11], (L, SWA_WIDTH)),
        'w_out': nrm(ks[12], (L, MIX_WIDTH, D), MIX_WIDTH ** -0.5),
        'norm_cross': gain(ks[13], (L, D)),
        'norm_mem': gain(ks[14], (L, D)),
        'w_cq': nrm(ks[15], (L, D, X_HEADS * X_HD), D ** -0.5),
        'w_ck': nrm(ks[16], (L, D, X_HEADS * X_HD), D ** -0.5),
        'w_cv': nrm(ks[17], (L, D, X_HEADS * X_HD), D ** -0.5),
        'w_co': nrm(ks[18], (L, X_HEADS * X_HD, D), (X_HEADS * X_HD) ** -0.5),
        'norm_ffn': gain(ks[19], (L, D)),
        'peer_w_q': nrm(ks[20], (L, D, PEER_HEADS * PEER_QDIM), D ** -0.5),
        'peer_keys': nrm(ks[21], (L, PEER_HEADS, 2, N_KEYS, PEER_HALF), PEER_HALF ** -0.5),
        'peer_u': nrm(ks[22], (L, N_EXPERTS, D), D ** -0.5),
        'peer_v': nrm(ks[23], (L, N_EXPERTS, D), 0.5),
        'norm_final': gain(ks[24], (D,)),
    }


def reference(x, mem, positions, norm_mix, w_in, q_a_norm, w_q_b, kv_a_norm, w_kv_b,
              swa_sinks, out_norm_mla, out_norm_swa, w_out, norm_cross, norm_mem,
              w_cq, w_ck, w_cv, w_co, norm_ffn, peer_w_q, peer_keys, peer_u, peer_v,
              norm_final):
    for l in range(DEPTH):
        h = rmsnorm(x, norm_mix[l])
        c_q, c_kv, k_rope, q_s, k_s, v_s = jnp.split(h @ w_in[l], list(SPLITS), axis=-1)
        o_a = mla_attention(c_q, c_kv, k_rope, positions, q_a_norm[l], w_q_b[l], kv_a_norm[l], w_kv_b[l])
        o_b = swa_attention(q_s, k_s, v_s, swa_sinks[l])
        mix = jnp.concatenate([rmsnorm(o_a, out_norm_mla[l]), rmsnorm(o_b, out_norm_swa[l])], axis=-1)
        x = x + mix @ w_out[l]
        x = x + cross_attention(rmsnorm(x, norm_cross[l]), mem, norm_mem[l], w_cq[l], w_ck[l], w_cv[l], w_co[l])
        x = x + peer_ffn(rmsnorm(x, norm_ffn[l]), peer_w_q[l], peer_keys[l], peer_u[l], peer_v[l])
    return rmsnorm(x, norm_final)
```

```python
import numpy as np
import ml_dtypes
from contextlib import ExitStack

import concourse.bass as bass
import concourse.mybir as mybir
from concourse.bass_utils import run_bass_kernel_spmd

F32 = mybir.dt.float32
BF16 = mybir.dt.bfloat16
I32 = mybir.dt.int32
U32 = mybir.dt.uint32
AF = mybir.ActivationFunctionType
ALU = mybir.AluOpType
AX = mybir.AxisListType

D = 1024
SEQ = 16384
NBLK = SEQ // 128
NCORE = 8
EPS = 1e-6
NOWN = 32
NEXP = 16384


def own_blocks(r):
    s = set()
    for m in range(16):
        s.add(8 * m + r)
        s.add(8 * m + 7 - r)
    return sorted(s)


class T:
    __slots__ = ("name", "w", "r", "dsem", "dcnt")

    def __init__(self, name):
        self.name = name
        self.w = None
        self.r = {}
        self.dsem = None
        self.dcnt = 0


class Sched:
    ENGS = ("pe", "act", "dve", "pool", "sp")

    def __init__(self, nc, st):
        self.nc = nc
        self.st = st
        self.prog = {e: [] for e in self.ENGS}
        self.seen = {e: {} for e in self.ENGS}
        self.esem = {}
        for e in ("pe", "act", "dve", "pool"):
            self.esem[e] = st.enter_context(nc.semaphore("s_" + e))
        self.bsem = st.enter_context(nc.semaphore("s_bar"))
        self.bcnt = 0
        self.dtiles = []
        self.fuse_waits = True

    def _deps(self, eng, reads, writes):
        evs = []
        for t in reads:
            if t.w is not None:
                evs.append(t.w)
        for t in writes:
            if t.w is not None:
                evs.append(t.w)
            evs.extend(t.r.values())
        waits = []
        seen = self.seen[eng]
        for ev in evs:
            if ev[0] == "e":
                _, f, idx = ev
                if f == eng and eng == "pe":
                    continue
                if seen.get(f, -1) >= idx:
                    continue
                seen[f] = idx
                self.prog[f][idx]["inc"] = True
                waits.append(ev)
            else:
                _, owner, cnt = ev
                key = ("d", id(owner))
                if seen.get(key, 0) >= cnt:
                    continue
                cur = owner.dcnt
                seen[key] = cur
                waits.append(("d", owner, cur))
        return waits

    def op(self, eng, fn, reads=(), writes=()):
        waits = self._deps(eng, reads, writes)
        idx = len(self.prog[eng])
        self.prog[eng].append({"fn": fn, "waits": waits, "inc": False, "dma": None})
        ev = ("e", eng, idx)
        for t in reads:
            t.r[eng] = ev
        for t in writes:
            t.w = ev
            t.r = {}

    def dma(self, fn, owner, reads=(), writes=(), q="sp"):
        waits = self._deps(q, reads, writes)
        if owner.dsem is None:
            owner.dsem = self.st.enter_context(self.nc.semaphore("d_" + owner.name))
            self.dtiles.append(owner)
        owner.dcnt += 16
        self.prog[q].append({"fn": fn, "waits": waits, "inc": False, "dma": owner})
        ev = ("d", owner, owner.dcnt)
        for t in reads:
            t.r[("d", id(owner))] = ev
        for t in writes:
            t.w = ev
            t.r = {}

    def barrier(self):
        waits = []
        for o in self.dtiles:
            if o.dcnt > 0:
                waits.append(("d", o, o.dcnt))
        for e in ("pe", "act", "dve", "pool"):
            idx = len(self.prog[e]) - 1
            while idx >= 0 and self.prog[e][idx]["fn"] is None:
                idx -= 1
            if idx >= 0:
                self.prog[e][idx]["inc"] = True
                waits.append(("e", e, idx))
        self.bcnt += 1
        self.prog["sp"].append({"fn": None, "waits": waits, "inc": False, "dma": None, "bar": self.bcnt})
        for e in ("pe", "act", "dve", "pool"):
            self.prog[e].append({"fn": None, "waits": [("b", self.bcnt)], "inc": False, "dma": None})

    def emit(self):
        nc = self.nc
        for e in ("pe", "act", "dve", "pool"):
            c = 0
            for rec in self.prog[e]:
                if rec["inc"]:
                    c += 1
                    rec["semval"] = c
        engmap = {"pe": "tensor", "act": "scalar", "dve": "vector", "pool": "gpsimd", "sp": "sync"}
        with nc.Block() as block:
            for e in self.ENGS:
                def body(engobj, e=e):
                    def semval(w):
                        if w[0] == "e":
                            return self.esem[w[1]], self.prog[w[1]][w[2]]["semval"]
                        elif w[0] == "d":
                            return w[1].dsem, w[2]
                        return self.bsem, w[1]
                    for rec in self.prog[e]:
                        ws = rec["waits"]
                        fuse = None
                        if rec["fn"] is not None and ws and rec["dma"] is None and self.fuse_waits:
                            fuse = ws[-1]
                            ws = ws[:-1]
                        for w in ws:
                            engobj.wait_ge(*semval(w))
                        if rec["fn"] is None:
                            if rec.get("bar") is not None:
                                engobj.sem_inc(self.bsem, 1)
                            continue
                        ins = rec["fn"]()
                        if fuse is not None:
                            ins._wait_ge(*semval(fuse))
                        if rec["dma"] is not None:
                            ins.then_inc(rec["dma"].dsem, 16)
                        elif rec["inc"]:
                            ins.then_inc(self.esem[e], 1)
                getattr(block, engmap[e])(body)


class Buf:
    def __init__(self, t, name):
        self.t = t
        self.s = T(name)


class K:
    def __init__(self, nc, st):
        self.nc = nc
        self.st = st
        self.S = Sched(nc, st)
        self.eng = {"pe": nc.tensor, "act": nc.scalar, "dve": nc.vector, "pool": nc.gpsimd, "sp": nc.sync}
        self._n = 0
        self.main_st = st

    def push_scope(self):
        self.st = ExitStack()

    def pop_scope(self):
        self.S.barrier()
        self.st.close()
        self.st = self.main_st

    def sb(self, name, shape, dt):
        return Buf(self.st.enter_context(self.nc.sbuf_tensor("sb_" + name, shape, dt)), name)

    def ps(self, name, shape, dt):
        return Buf(self.st.enter_context(self.nc.psum_tensor("pp_" + name, shape, dt)), name)

    def ring(self, name, shape, dt, n):
        return Ring([self.sb("%s%d" % (name, i), shape, dt) for i in range(n)])

    def op(self, eng, fn, r=(), w=()):
        e = self.eng[eng]
        self.S.op(eng, lambda: fn(e), [getattr(x, "s", x) for x in r], [getattr(x, "s", x) for x in w])

    def dma(self, out, in_, owner, r=(), w=(), q="sp", **kw):
        e = self.eng[q]
        self.S.dma(lambda: e.dma_start(out=out, in_=in_, **kw), getattr(owner, "s", owner),
                   [getattr(x, "s", x) for x in r], [getattr(x, "s", x) for x in w], q=q)

    def mm(self, out, lhsT, rhs, start, stop, r, w, skip=False):
        self.op("pe", lambda e: e.matmul(out, lhsT, rhs, start=start, stop=stop, skip_group_check=skip), r, w)

    def tr(self, out, in_, ident, r, w):
        self.op("pe", lambda e: e.transpose(out, in_, ident), r, w)

    def act(self, out, in_, func, r, w, scale=1.0, bias=0.0):
        self.op("act", lambda e: e.activation(out, in_, func, bias=bias, scale=scale), r, w)


class Ring:
    def __init__(self, bufs):
        self.bufs = bufs
        self.i = 0

    def next(self):
        b = self.bufs[self.i % len(self.bufs)]
        self.i += 1
        return b


G_MIX, G_CROSS, G_MEM, G_FFN, G_QA, G_KVA, G_OUT = 0, 8, 16, 24, 32, 34, 35
NGC = 43
C1 = 6.28125
C2 = float(2.0 * np.pi - 6.28125)
MAGIC = 12582912.0
PI_LO = 3.1415925
NEG = -1.0e30


def n_keyblocks(kidx):
    m = kidx // 2
    return 8 * m + 4 if kidx % 2 == 0 else 8 * m + 8


class StopBuild(Exception):
    pass


class Prog:
    def __init__(self, cfg):
        self.cfg = cfg
        nc = bass.Bass("TRN2", target_bir_lowering=False)
        self.nc = nc
        self.st = ExitStack()
        self.k = K(nc, self.st)
        self.dram = {}

    def din(self, name, shape, dt):
        t = self.nc.dram_tensor(name, list(shape), dt, kind="ExternalInput").ap()
        self.dram[name] = t
        return t

    def dscr(self, name, shape, dt):
        t = self.nc.dram_tensor(name, list(shape), dt, kind="Internal").ap()
        self.dram[name] = t
        return t

    def dout(self, name, shape, dt):
        t = self.nc.dram_tensor(name, list(shape), dt, kind="ExternalOutput").ap()
        self.dram[name] = t
        return t


def build(cfg=None):
    cfg = dict(cfg or {})
    n_sb = cfg.get("n_sb", 16)
    n_own = cfg.get("n_own", NOWN)
    n_grp = cfg.get("n_grp", NOWN // 2)
    n_eg = cfg.get("n_eg", 32)
    do_p0 = cfg.get("p0", True)
    do_p1a = cfg.get("p1a", True)
    do_p1b = cfg.get("p1b", True)
    do_p2 = cfg.get("p2", True)
    dbg = cfg.get("dbg", None)

    P = Prog(cfg)
    nc, k, st = P.nc, P.k, P.st

    xb = P.din("xb", [SEQ, D], F32)
    xown = P.din("xown", [NOWN * 128, D], F32)
    xprev = P.din("xprev", [NOWN * 128, D], F32)
    memb = P.din("memb", [256, D], F32)
    posb = P.din("posb", [128, NBLK], I32)
    poso = P.din("poso", [128, NOWN], I32)
    mmask_d = P.din("mmask", [NOWN, 128, 4 * 128], BF16)
    smask_d = P.din("smask", [NOWN, 128, 2 * 128], BF16)
    identb_d = P.din("identb", [128, 128], BF16)
    identf_d = P.din("identf", [128, 128], F32)
    invf_d = P.din("invf", [128, 32], F32)
    kaug_d = P.din("kaug", [2, 2 * 128], BF16)
    qaug_d = P.din("qaug", [2, 2 * 512], BF16)
    iota128_d = P.din("iota128", [128, 128], F32)
    iota16_d = P.din("iota16", [128, 16], F32)
    gcols_d = P.din("gcols", [128, NGC], F32)
    kvarow_d = P.din("kvarow", [1, 128], F32)
    sinks_d = P.din("sinks", [1, 8], F32)
    gfin_d = P.din("gfin", [1, D], F32)
    w_kvl_d = P.din("w_kvl", [D, 192], F32)
    w_in_d = P.din("w_in", [D, 1216], F32)
    w_qb_d = P.din("w_qb", [256, 768], F32)
    w_ukT_d = P.din("w_ukT", [128, 512], F32)
    w_uv_d = P.din("w_uv", [128, 512], F32)
    w_out_d = P.din("w_out", [D, D], F32)
    w_cq_d = P.din("w_cq", [D, 512], F32)
    w_ck_d = P.din("w_ck", [D, 512], F32)
    w_cv_d = P.din("w_cv", [D, 512], F32)
    w_co_d = P.din("w_co", [512, D], F32)
    w_pq_d = P.din("w_pq", [D, 2048], F32)
    keysT_d = P.din("keysT", [128, 2048], F32)
    uT_d = P.din("uT", [D, NEXP], F32)
    v_d = P.din("v", [NEXP, D], F32)

    kc_s = P.dscr("kc_s", [16, 128, 1024], BF16)
    kr_s = P.dscr("kr_s", [16, 64, 1024], BF16)
    vc_s = P.dscr("vc_s", [16, 128, 8 * 129], BF16)
    x2_s = P.dscr("x2_s", [NOWN * 128, D], F32)
    us_s = P.dscr("us_s", [32, 128, 4096], BF16)
    vs_s = P.dscr("vs_s", [32, 128, 4096], BF16)
    out_d = P.dout("out", [NOWN * 128, D], F32)
    dbg_d = P.dout("dbg", [128, 4096], F32) if dbg else None

    ps_tp = k.ps("ps_tp", [128, 1024], BF16)
    pb = [k.ps("ps_b%d" % i, [128, 512], F32) for i in range(7)]

    identb = k.sb("identb", [128, 128], BF16)
    identf = k.sb("identf", [128, 128], F32)
    invf = k.sb("invf", [128, 32], F32)
    gcols = k.sb("gcols", [128, NGC], F32)
    k.dma(identb.t[:], identb_d, identb, w=[identb])
    k.dma(identf.t[:], identf_d, identf, w=[identf])
    k.dma(invf.t[:], invf_d, invf, w=[invf])
    k.dma(gcols.t[:], gcols_d, gcols, w=[gcols])

    scal_t = k.sb("scal", [128, 64], F32)
    scal_s = [T("scal%d" % i) for i in range(64)]
    scal_i = [0]

    def new_scal():
        i = scal_i[0] % 64
        scal_i[0] += 1
        return scal_t.t[:, i:i + 1], scal_s[i]

    junk = k.sb("junk", [128, 1024], BF16)
    xring = k.ring("xt", [128, D], F32, 3)
    hbring = k.ring("hb", [128, D], BF16, 2)
    hTring = k.ring("hT", [128, 8, 128], BF16, 2)
    ropet = k.ring("ropet", [128, 2, 8, 32], F32, 2)
    stage = k.ring("stage", [128, 2048], F32, 2)

    def rstd_of(x_ap, F, x_tiles, eng_sq="dve"):
        ss, ss_s = new_scal()
        k.op("dve", lambda e: e.scalar_tensor_tensor(out=junk.t[:, 0:F], in0=x_ap, scalar=1.0, in1=x_ap,
                                                     op0=ALU.mult, op1=ALU.mult, accum_out=ss),
             r=x_tiles, w=[junk, ss_s])
        ln, ln_s = new_scal()
        k.act(ln, ss, AF.Ln, r=[ss_s], w=[ln_s], scale=1.0 / F, bias=EPS)
        rs, rs_s = new_scal()
        k.act(rs, ln, AF.Exp, r=[ln_s], w=[rs_s], scale=-0.5)
        return rs, rs_s

    def load_cast(dst, src_d, KC, N, gcol0=None, eng_cycle=("dve", "pool")):
        src_v = src_d.rearrange("(c p) n -> p c n", p=128)
        per = max(1, 2048 // N)
        ei = 0
        for c0 in range(0, KC, per):
            c1 = min(KC, c0 + per)
            sg = stage.next()
            k.dma(sg.t[:, 0:(c1 - c0) * N].rearrange("p (c n) -> p c n", n=N), src_v[:, c0:c1, :], sg, w=[sg])
            for c in range(c0, c1):
                eng = eng_cycle[ei % len(eng_cycle)]
                ei += 1
                src_ap = sg.t[:, (c - c0) * N:(c - c0 + 1) * N]
                dst_ap = dst.t[:, c, :]
                if gcol0 is None:
                    k.op(eng, lambda e, o=dst_ap, i=src_ap: e.tensor_copy(out=o, in_=i), r=[sg], w=[dst])
                else:
                    gc = gcols.t[:, gcol0 + c:gcol0 + c + 1]
                    k.op(eng, lambda e, o=dst_ap, i=src_ap, g=gc: e.tensor_scalar(out=o, in0=i, scalar1=g, scalar2=1.0,
                                                                                 op0=ALU.mult, op1=ALU.mult),
                         r=[sg, gcols], w=[dst])

    def transposes(dst, src, nchunk, width, r, w, evac_eng="act", dst_ap=None, pst=None):
        pst = pst or ps_tp
        for c in range(nchunk):
            k.tr(pst.t[0:width, c * 128:(c + 1) * 128], src[:, c * width:(c + 1) * width], identb.t[:],
                 r=r + [identb], w=[pst])
        o = dst_ap if dst_ap is not None else dst.t[0:width, 0:nchunk, :]
        i = pst.t[0:width, 0:nchunk * 128].rearrange("p (c t) -> p c t", t=128)
        if evac_eng == "act":
            k.op("act", lambda e: e.copy(out=o, in_=i), r=[pst], w=w)
        else:
            k.op(evac_eng, lambda e: e.tensor_copy(out=o, in_=i), r=[pst], w=w)

    k.push_scope()
    castring = [None]
    P0Q = cfg.get("p0q", "sp")

    def p0_gen():
        uT_v = uT_d.rearrange("(c p) e -> p c e", p=128)
        v_v = v_d.rearrange("(g c p) d -> g p c d", p=128, c=4)
        engs3 = ("pool", "pool", "pool")
        ei = 0
        for g in range(n_eg):
            for hf in range(2):
                sg = stage.next()
                k.dma(sg.t[:].rearrange("p (c e) -> p c e", e=512), uT_v[:, hf * 4:(hf + 1) * 4, g * 512:(g + 1) * 512], sg, w=[sg], q=P0Q)
                cb = castring[0].next()
                for cc in range(4):
                    c = hf * 4 + cc
                    eng = engs3[ei % 3]
                    ei += 1
                    o = cb.t[:, cc * 512:(cc + 1) * 512]
                    i = sg.t[:, cc * 512:(cc + 1) * 512]
                    gc = gcols.t[:, G_FFN + c:G_FFN + c + 1]
                    if eng == "act":
                        k.op("act", lambda e, o=o, i=i, gc=gc: e.activation(o, i, AF.Copy, scale=gc), r=[sg, gcols], w=[cb])
                    else:
                        k.op(eng, lambda e, o=o, i=i, gc=gc: e.tensor_scalar(out=o, in0=i, scalar1=gc, scalar2=1.0,
                                                                             op0=ALU.mult, op1=ALU.mult),
                             r=[sg, gcols], w=[cb])
                k.dma(us_s[g][:, hf * 2048:(hf + 1) * 2048], cb.t[:], cb, r=[cb], q=P0Q)
                yield
            for hf in range(2):
                sg = stage.next()
                k.dma(sg.t[:].rearrange("p (c d) -> p c d", d=1024), v_v[g][:, hf * 2:(hf + 1) * 2, :], sg, w=[sg], q=P0Q)
                cb = castring[0].next()
                for cc in range(2):
                    eng = engs3[ei % 3]
                    ei += 1
                    o = cb.t[:, cc * 1024:(cc + 1) * 1024]
                    i = sg.t[:, cc * 1024:(cc + 1) * 1024]
                    if eng == "act":
                        k.op("act", lambda e, o=o, i=i: e.copy(out=o, in_=i), r=[sg], w=[cb])
                    else:
                        k.op(eng, lambda e, o=o, i=i: e.tensor_copy(out=o, in_=i), r=[sg], w=[cb])
                k.dma(vs_s[g][:, hf * 2048:(hf + 1) * 2048], cb.t[:], cb, r=[cb], q=P0Q)
                yield

    castring[0] = k.ring("castA", [128, 2048], BF16, 2)
    p0g = p0_gen() if do_p0 else None

    wkvl = k.sb("wkvl", [128, 8, 192], BF16)
    load_cast(wkvl, w_kvl_d, 8, 192, G_MIX)
    posb_i = k.sb("posb_i", [128, NBLK], I32)
    posb_f = k.sb("posb_f", [128, NBLK], F32)
    k.dma(posb_i.t[:], posb, posb_i, w=[posb_i])
    k.op("dve", lambda e: e.tensor_copy(out=posb_f.t[:], in_=posb_i.t[:]), r=[posb_i], w=[posb_f])

    kvsring = k.ring("kvs", [128, 192], F32, 4)
    krb_ring = k.ring("krb", [128, 64], BF16, 3)
    kcst = k.ring("kcst", [128, 1024], BF16, 2)
    krst = k.ring("krst", [64, 1024], BF16, 2)
    vcst = k.ring("vcst", [128, 8, 129], BF16, 2)

    def rope_tables_bulk(name, pos_f, nb, tab=None):
        if tab is None:
            tab = k.sb(name, [128, 2, nb, 32], F32)
        wk = k.sb(name + "_w", [128, 3, nb, 32], F32)
        ang = wk.t[:, 0]
        k.op("dve", lambda e: e.tensor_tensor(out=ang, in0=pos_f.t[:].unsqueeze(2).to_broadcast([128, nb, 32]),
                                              in1=invf.t[:].unsqueeze(1).to_broadcast([128, nb, 32]), op=ALU.mult),
             r=[pos_f, invf], w=[wk])
        for which in (0, 1):
            a2 = wk.t[:, 1]
            nn = wk.t[:, 2]
            off = 0.0 if which == 0 else float(np.pi / 2)
            k.op("dve", lambda e, off=off: e.tensor_scalar(out=a2, in0=ang, scalar1=off, scalar2=None, op0=ALU.add), r=[wk], w=[wk])
            k.op("dve", lambda e: e.tensor_scalar(out=nn, in0=a2, scalar1=float(1.0 / (2 * np.pi)), scalar2=MAGIC,
                                                  op0=ALU.mult, op1=ALU.add), r=[wk], w=[wk])
            k.op("dve", lambda e: e.tensor_scalar(out=nn, in0=nn, scalar1=-MAGIC, scalar2=None, op0=ALU.add), r=[wk], w=[wk])
            k.op("dve", lambda e: e.scalar_tensor_tensor(out=a2, in0=nn, scalar=-C1, in1=a2, op0=ALU.mult, op1=ALU.add), r=[wk], w=[wk])
            k.op("dve", lambda e: e.scalar_tensor_tensor(out=a2, in0=nn, scalar=-C2, in1=a2, op0=ALU.mult, op1=ALU.add), r=[wk], w=[wk])
            k.op("dve", lambda e: e.tensor_scalar(out=a2, in0=a2, scalar1=-PI_LO, scalar2=PI_LO, op0=ALU.max, op1=ALU.min), r=[wk], w=[wk])
            k.act(tab.t[:, which], a2, AF.Sin, r=[wk], w=[tab])
        return tab

    def apply_rope(out_bf, x_ap, nh, sin_ap, cos_ap, tg, x_tiles, out_tiles):
        rt = ropet.next()
        xv = x_ap.rearrange("p (h two j) -> p h two j", two=2, j=32)
        cosb = cos_ap.unsqueeze(1).unsqueeze(1).to_broadcast([128, nh, 2, 32])
        sinb = sin_ap.unsqueeze(1).unsqueeze(1).to_broadcast([128, nh, 2, 32])
        xc = rt.t[:, 0, 0:2 * nh, :].rearrange("p (h two) j -> p h two j", two=2)
        xs = rt.t[:, 1, 0:2 * nh, :].rearrange("p (h two) j -> p h two j", two=2)
        k.op("dve", lambda e: e.tensor_tensor(out=xc, in0=xv, in1=cosb, op=ALU.mult), r=x_tiles + [tg], w=[rt])
        k.op("dve", lambda e: e.tensor_tensor(out=xs, in0=xv, in1=sinb, op=ALU.mult), r=x_tiles + [tg], w=[rt])
        ov = out_bf.rearrange("p (h two j) -> p h two j", two=2, j=32)
        k.op("dve", lambda e: e.tensor_tensor(out=ov[:, :, 0, :], in0=xc[:, :, 0, :], in1=xs[:, :, 1, :], op=ALU.subtract),
             r=[rt], w=out_tiles)
        k.op("dve", lambda e: e.tensor_tensor(out=ov[:, :, 1, :], in0=xc[:, :, 1, :], in1=xs[:, :, 0, :], op=ALU.add),
             r=[rt], w=out_tiles)

    def norm_T(x_ap, x_tiles, hbr=None, hTr=None, evac_eng="act"):
        hbr = hbr or hbring
        hTr = hTr or hTring
        rs, rs_s = rstd_of(x_ap, D, x_tiles)
        hb = hbr.next()
        k.op("dve", lambda e: e.tensor_scalar(out=hb.t[:], in0=x_ap, scalar1=rs, scalar2=None, op0=ALU.mult),
             r=x_tiles + [rs_s], w=[hb])
        hT = hTr.next()
        transposes(hT, hb.t[:], 8, 128, [hb], [hT], evac_eng=evac_eng)
        return hT

    tabb = rope_tables_bulk("tabb", posb_f, NBLK)
    if do_p1a:
        sbufs = {}
        kvs_of = {}
        hbk_r = k.ring("hbk", [128, D], BF16, 3)
        hTk_r = k.ring("hTk", [128, 8, 128], BF16, 3)

        xk6 = k.ring("xk6", [128, D], F32, 6)
        sA = {}

        def stageA1(kb):
            sbi, j = kb // 8, kb % 8
            if j == 0:
                kcs, krs, vcs = kcst.next(), krst.next(), vcst.next()
                k.op("pool", lambda e: e.memset(vcs.t[:, :, 128:129], 1.0), w=[vcs])
                sbufs[sbi] = (kcs, krs, vcs)
            xt = xk6.next()
            k.dma(xt.t[:], xb[kb * 128:(kb + 1) * 128, :], xt, w=[xt], q="act")
            rs, rs_s = rstd_of(xt.t[:], D, [xt])
            sA[kb] = (xt, rs, rs_s)

        def stageA2(kb):
            xt, rs, rs_s = sA.pop(kb)
            hb = hbk_r.next()
            k.op("dve", lambda e: e.tensor_scalar(out=hb.t[:], in0=xt.t[:], scalar1=rs, scalar2=None, op0=ALU.mult),
                 r=[xt, rs_s], w=[hb])
            hT = hTk_r.next()
            transposes(hT, hb.t[:], 8, 128, [hb], [hT])
            pk = pb[3 + kb % 3]
            for c in range(8):
                k.mm(pk.t[:, 0:192], hT.t[:, c, :], wkvl.t[:, c, :], c == 0, c == 7, r=[hT, wkvl], w=[pk])
            kvs = kvsring.next()
            k.op("act", lambda e: e.copy(out=kvs.t[:], in_=pk.t[:, 0:192]), r=[pk], w=[kvs])
            kvs_of[kb] = kvs

        sB = {}

        def stageB1(kb):
            kvs = kvs_of[kb]
            rs, rs_s = rstd_of(kvs.t[:, 0:128], 128, [kvs])
            krb = krb_ring.next()
            apply_rope(krb.t[:], kvs.t[:, 128:192], 1, tabb.t[:, 0, kb, :], tabb.t[:, 1, kb, :], tabb, [kvs], [krb])
            sB[kb] = (rs, rs_s, krb)

        def stageB2(kb):
            sbi, j = kb // 8, kb % 8
            kcs, krs, vcs = sbufs[sbi]
            kvs = kvs_of.pop(kb)
            rs, rs_s, krb = sB.pop(kb)
            k.op("dve", lambda e: e.tensor_scalar(out=vcs.t[:, j, 0:128], in0=kvs.t[:, 0:128],
                                                  scalar1=rs, scalar2=None, op0=ALU.mult), r=[kvs, rs_s], w=[vcs])
            pt = pb[2]
            ptb = pt.t[:].bitcast(BF16)
            k.tr(ptb[:, 0:128], vcs.t[:, j, 0:128], identb.t[:], r=[vcs, identb], w=[pt])
            k.tr(ptb[0:64, 128:256], krb.t[:], identb.t[:], r=[krb, identb], w=[pt])
            k.op("act", lambda e: e.copy(out=kcs.t[:, j * 128:(j + 1) * 128], in_=ptb[:, 0:128]), r=[pt], w=[kcs])
            k.op("act", lambda e: e.copy(out=krs.t[:, j * 128:(j + 1) * 128], in_=ptb[0:64, 128:256]), r=[pt], w=[krs])
            if j == 7:
                k.dma(kc_s[sbi], kcs.t[:], kcs, r=[kcs])
                k.dma(kr_s[sbi], krs.t[:], krs, r=[krs])
                k.dma(vc_s[sbi], vcs.t[:].rearrange("p a b -> p (a b)"), vcs, r=[vcs])

        nkb = n_sb * 8
        for t_ in range(nkb + 3):
            if t_ < nkb:
                stageA1(t_)
            if 0 <= t_ - 1 < nkb:
                stageA2(t_ - 1)
            if 0 <= t_ - 2 < nkb:
                stageB1(t_ - 2)
            if 0 <= t_ - 3 < nkb:
                stageB2(t_ - 3)
                if p0g is not None and (t_ % 2 == 0 or not do_p1b):
                    next(p0g, None)
        kcs, krs, vcs = sbufs[n_sb - 1]
    if p0g is not None and not do_p1b:
        for _ in p0g:
            pass

    if dbg == "p1a":
        dt_ = k.sb("dbgt", [128, 4096], F32)
        k.op("dve", lambda e: e.memset(dt_.t[:], 0.0), w=[dt_])
        k.op("dve", lambda e: e.tensor_copy(out=dt_.t[:, 0:1024], in_=kcs.t[:]), r=[kcs], w=[dt_])
        k.op("dve", lambda e: e.tensor_copy(out=dt_.t[0:64, 1024:2048], in_=krs.t[:]), r=[krs], w=[dt_])
        k.op("dve", lambda e: e.tensor_copy(out=dt_.t[:, 2048:2048 + 1032], in_=vcs.t[:].rearrange("p a b -> p (a b)")),
             r=[vcs], w=[dt_])
        k.dma(dbg_d, dt_.t[:], dt_, r=[dt_])


    k.pop_scope()
    k.push_scope()
    if do_p1b:
        castring[0] = k.ring("castB", [128, 2048], BF16, 2)
        win = k.sb("win", [128, 8, 1216], BF16)
        load_cast(win, w_in_d, 8, 1216, G_MIX)
        wqb = k.sb("wqb", [128, 2, 768], BF16)
        load_cast(wqb, w_qb_d, 2, 768, G_QA)
        wout = k.sb("wout", [128, 8, 1024], BF16)
        load_cast(wout, w_out_d, 8, 1024, G_OUT)
        wcq = k.sb("wcq", [128, 8, 512], BF16)
        load_cast(wcq, w_cq_d, 8, 512, G_CROSS)
        wco = k.sb("wco", [128, 4, 1024], BF16)
        load_cast(wco, w_co_d, 4, 1024, None)
        lvl = cfg.get("lvl", 99)
        if lvl < 1:
            raise StopBuild(P)
        kvab = k.sb("kvab", [128, 128], F32)
        k.dma(kvab.t[:], kvarow_d[0].partition_broadcast(128), kvab, w=[kvab])
        wukT = k.sb("wukT", [128, 4, 128], BF16)
        sg = stage.next()
        k.dma(sg.t[:, 0:512], w_ukT_d, sg, w=[sg])
        k.op("dve", lambda e, sg=sg: e.tensor_tensor(out=wukT.t[:], in0=sg.t[:, 0:512].rearrange("p (h r) -> p h r", r=128),
                                                     in1=kvab.t[:].unsqueeze(1).to_broadcast([128, 4, 128]), op=ALU.mult),
             r=[sg, kvab], w=[wukT])
        wuv = k.sb("wuv", [128, 4, 128], BF16)
        sg = stage.next()
        k.dma(sg.t[:, 0:512], w_uv_d, sg, w=[sg])
        k.op("dve", lambda e, sg=sg: e.tensor_scalar(out=wuv.t[:].rearrange("p h d -> p (h d)"), in0=sg.t[:, 0:512],
                                                     scalar1=gcols.t[:, G_KVA:G_KVA + 1], scalar2=None, op0=ALU.mult),
             r=[sg, gcols], w=[wuv])
        if lvl < 2:
            raise StopBuild(P)
        esink = k.sb("esink", [128, 8], F32)
        k.dma(esink.t[:], sinks_d[0].partition_broadcast(128), esink, w=[esink])
        k.act(esink.t[:], esink.t[:], AF.Exp, r=[esink], w=[esink])
        if lvl < 3:
            raise StopBuild(P)
        kaug = k.sb("kaug", [2, 2, 128], BF16)
        qaug = k.sb("qaug", [2, 2, 512], BF16)
        k.dma(kaug.t[:].rearrange("p a b -> p (a b)"), kaug_d, kaug, w=[kaug])
        k.dma(qaug.t[:].rearrange("p a b -> p (a b)"), qaug_d, qaug, w=[qaug])
        poso_i = k.sb("poso_i", [128, NOWN], I32)
        poso_f = k.sb("poso_f", [128, NOWN], F32)
        k.dma(poso_i.t[:], poso, poso_i, w=[poso_i])
        k.op("dve", lambda e: e.tensor_copy(out=poso_f.t[:], in_=poso_i.t[:]), r=[poso_i], w=[poso_f])
        tabo = k.sb("tabo", [128, 2, NOWN, 32], F32)

        if lvl < 4:
            raise StopBuild(P)
        KxT = k.sb("KxT", [128, 4, 256], BF16)
        Vx = k.sb("Vx", [128, 2, 4, 129], BF16)
        k.op("pool", lambda e: e.memset(Vx.t[:, :, :, 128:129], 1.0), w=[Vx])
        main_scope = k.st
        k.st = ExitStack()
        rope_tables_bulk("tabo", poso_f, NOWN, tab=tabo)
        memT = k.sb("memT", [128, 8, 256], BF16)
        wck = k.sb("wck", [128, 8, 512], BF16)
        wcv = k.sb("wcv", [128, 8, 512], BF16)
        load_cast(wck, w_ck_d, 8, 512, G_MEM)
        load_cast(wcv, w_cv_d, 8, 512, G_MEM)
        for mc in range(2):
            xt = xring.next()
            k.dma(xt.t[:], memb[mc * 128:(mc + 1) * 128, :], xt, w=[xt])
            rs, rs_s = rstd_of(xt.t[:], D, [xt])
            hb = hbring.next()
            k.op("dve", lambda e, hb=hb, xt=xt, rs=rs: e.tensor_scalar(out=hb.t[:], in0=xt.t[:], scalar1=rs, scalar2=None,
                                                                       op0=ALU.mult), r=[xt, rs_s], w=[hb])
            transposes(None, hb.t[:], 8, 128, [hb], [memT], dst_ap=memT.t[:, :, mc * 128:(mc + 1) * 128])
        for h in range(4):
            for c in range(8):
                k.mm(pb[0].t[:, 0:256], wck.t[:, c, h * 128:(h + 1) * 128], memT.t[:, c, :], c == 0, c == 7,
                     r=[wck, memT], w=[pb[0]])
            k.op("act", lambda e, h=h: e.copy(out=KxT.t[:, h, :], in_=pb[0].t[:, 0:256]), r=[pb[0]], w=[KxT])
        for mc in range(2):
            for c in range(8):
                k.mm(pb[1].t[:, 0:512], memT.t[:, c, mc * 128:(mc + 1) * 128], wcv.t[:, c, :], c == 0, c == 7,
                     r=[wcv, memT], w=[pb[1]])
            k.op("act", lambda e, mc=mc: e.copy(out=Vx.t[:, mc, :, 0:128],
                                                in_=pb[1].t[:, 0:512].rearrange("p (h d) -> p h d", d=128)),
                 r=[pb[1]], w=[Vx])

        k.S.barrier()
        k.st.close()
        k.st = main_scope
        if lvl < 5:
            raise StopBuild(P)
        prj = k.ring("prj", [128, 1216], F32, 1)
        qsb_r = k.ring("qsb", [128, 512], BF16, 2)
        QsT_r = k.ring("QsT", [64, 8, 128], BF16, 2)
        KsT_r = k.ring("KsT", [64, 2, 2, 128], BF16, 2)
        Vs_r = k.ring("Vs", [128, 2, 2, 65], BF16, 2)
        ksb_r = k.ring("ksb", [128, 2, 256], BF16, 2)
        cqn_r = k.ring("cqn", [128, 256], BF16, 2)
        cqnT_r = k.ring("cqnT", [128, 2, 128], BF16, 2)
        qnT_r = k.ring("qnT", [128, 4, 128], BF16, 2)
        QT_r = k.ring("QT", [128, 512], BF16, 2)
        qrb_r = k.ring("qrb", [128, 256], BF16, 2)
        QrT_r = k.ring("QrT", [128, 4, 128], BF16, 2)
        PT_r = k.ring("PT", [128, 512], BF16, 3)
        PS_r = k.ring("PS", [128, 512], BF16, 3)
        mk_r = k.ring("mk", [128, 4, 128], BF16, 2)
        smk_r = k.ring("smk", [128, 2, 128], BF16, 2)
        kcc_r = k.ring("kcc", [128, 1024], BF16, 3)
        krc_r = k.ring("krc", [128, 1024], BF16, 3)
        vcc_r = k.ring("vcc", [128, 8, 129], BF16, 3)
        den_r = k.ring("den", [128, 8], F32, 4)
        olat_r = k.ring("olat", [128, 512], BF16, 2)
        olatT_r = k.ring("olatT", [128, 4, 128], BF16, 2)
        of32_r = k.ring("of32", [128, 512], F32, 2)
        mix_r = k.ring("mix", [128, 1024], BF16, 2)
        x1_r = k.ring("x1", [128, D], F32, 2)
        QxT_r = k.ring("QxT", [128, 4, 128], BF16, 2)
        for bq in QrT_r.bufs:
            k.op("pool", lambda e, bq=bq: e.memset(bq.t[64:128, :, :], 0.0), w=[bq])
        for bq in krc_r.bufs:
            k.op("pool", lambda e, bq=bq: e.memset(bq.t[64:128, :], 0.0), w=[bq])
        xo_r = Ring(xring.bufs[0:2])
        xp_r = Ring([xring.bufs[2], k.sb("xp1", [128, D], F32)])

        def norm_bank(pbuf, nhb, dv, extra_ap, extra_tiles, out_ap, out_tiles):
            dn = den_r.next()
            pv = pbuf.t[:, 0:nhb * (dv + 1)].rearrange("p (h d) -> p h d", d=dv + 1)
            den_src = pv[:, :, dv:dv + 1].rearrange("p h o -> p (h o)")
            if extra_ap is not None:
                k.op("dve", lambda e: e.tensor_tensor(out=dn.t[:, 0:nhb], in0=den_src, in1=extra_ap, op=ALU.add),
                     r=[pbuf] + extra_tiles, w=[dn])
            else:
                k.op("dve", lambda e: e.tensor_copy(out=dn.t[:, 0:nhb], in_=den_src), r=[pbuf], w=[dn])
            k.op("dve", lambda e: e.reciprocal(out=dn.t[:, 0:nhb], in_=dn.t[:, 0:nhb]), r=[dn], w=[dn])
            for hh in range(nhb):
                k.op("dve", lambda e, hh=hh: e.tensor_scalar(out=out_ap[:, hh * dv:(hh + 1) * dv], in0=pv[:, hh, 0:dv],
                                                            scalar1=dn.t[:, hh:hh + 1], scalar2=None, op0=ALU.mult),
                     r=[pbuf, dn], w=out_tiles)

        SC_MLA = float(192.0 ** -0.5)
        SC_SWA = 0.125
        SC_X = float(128.0 ** -0.5)
        st_ = {}

        def pre(ki):
            S = {}
            st_[ki] = S
            xt = xo_r.next()
            S["xt"] = xt
            k.dma(xt.t[:], xown[ki * 128:(ki + 1) * 128, :], xt, w=[xt], q="act")
            hT = norm_T(xt.t[:], [xt], evac_eng="dve")
            yield
            pj = prj.next()
            for (c0, c1, pbk) in ((0, 512, pb[0]), (512, 1024, pb[1]), (1024, 1216, pb[2])):
                for c in range(8):
                    k.mm(pbk.t[:, 0:c1 - c0], hT.t[:, c, :], win.t[:, c, c0:c1], c == 0, c == 7, r=[hT, win], w=[pbk])
                k.op("dve", lambda e, pbk=pbk, c0=c0, c1=c1: e.tensor_copy(out=pj.t[:, c0:c1], in_=pbk.t[:, 0:c1 - c0]),
                     r=[pbk], w=[pj])
                yield
            xp = xp_r.next()
            k.dma(xp.t[:], xprev[ki * 128:(ki + 1) * 128, :], xp, w=[xp], q="act")
            hTp = norm_T(xp.t[:], [xp], evac_eng="dve")
            yield
            for c in range(8):
                k.mm(pb[2].t[:, 0:256], hTp.t[:, c, :], win.t[:, c, 960:1216], c == 0, c == 7, r=[hTp, win], w=[pb[2]])
            ksb = ksb_r.next()
            k.op("dve", lambda e: e.tensor_copy(out=ksb.t[:, 0, :], in_=pb[2].t[:, 0:256]), r=[pb[2]], w=[ksb])
            k.op("dve", lambda e: e.tensor_copy(out=ksb.t[:, 1, :], in_=pj.t[:, 960:1216]), r=[pj], w=[ksb])
            yield
            KsT = KsT_r.next()
            Vs = Vs_r.next()
            k.op("dve", lambda e: e.memset(Vs.t[:, :, :, 64:65], 1.0), w=[Vs])
            for slot in range(2):
                for kvh in range(2):
                    k.tr(ps_tp.t[0:64, (slot * 2 + kvh) * 128:(slot * 2 + kvh + 1) * 128], ksb.t[:, slot, kvh * 64:(kvh + 1) * 64],
                         identb.t[:], r=[ksb, identb], w=[ps_tp])
                k.op("dve", lambda e, slot=slot: e.tensor_copy(
                    out=Vs.t[:, slot, :, 0:64], in_=ksb.t[:, slot, 128:256].rearrange("p (h d) -> p h d", d=64)),
                    r=[ksb], w=[Vs])
            k.op("dve", lambda e: e.tensor_copy(out=KsT.t[:].rearrange("p a b t -> p (a b t)"), in_=ps_tp.t[0:64, 0:512]),
                 r=[ps_tp], w=[KsT])
            yield
            qsb = qsb_r.next()
            k.op("dve", lambda e: e.tensor_copy(out=qsb.t[:], in_=pj.t[:, 448:960]), r=[pj], w=[qsb])
            QsT = QsT_r.next()
            transposes(QsT, qsb.t[:], 8, 64, [qsb], [QsT], evac_eng="dve")
            smk = smk_r.next()
            k.dma(smk.t[:].rearrange("p a b -> p (a b)"), smask_d[ki], smk, w=[smk], q="act")
            yield
            ob = of32_r.next()
            for kvh in range(2):
                for slot in range(2):
                    psc = pb[slot % 2]
                    k.mm(psc.t[:], KsT.t[:, slot, kvh, :], QsT.t[:, kvh * 4:(kvh + 1) * 4, :].rearrange("p g t -> p (g t)"),
                         True, False, r=[KsT, QsT], w=[psc])
                    k.mm(psc.t[:], kaug.t[:, slot, :], qaug.t[:, kvh, :], False, True, r=[kaug, qaug], w=[psc])
                    PT = PS_r.next()
                    k.act(PT.t[:], psc.t[:], AF.Exp, r=[psc], w=[PT], scale=SC_SWA)
                    k.op("dve", lambda e, PT=PT, slot=slot: e.tensor_tensor(
                        out=PT.t[:].rearrange("p (g t) -> p g t", t=128), in0=PT.t[:].rearrange("p (g t) -> p g t", t=128),
                        in1=smk.t[:, slot, :].unsqueeze(1).to_broadcast([128, 4, 128]), op=ALU.mult), r=[PT, smk], w=[PT])
                    yield
                    for g in range(4):
                        k.mm(pb[2].t[:, g * 65:(g + 1) * 65], PT.t[:, g * 128:(g + 1) * 128], Vs.t[:, slot, kvh, :],
                             slot == 0 and g == 0, slot == 1, r=[PT, Vs], w=[pb[2]], skip=True)
                    yield
                norm_bank(pb[2], 4, 64, esink.t[:, kvh * 4:(kvh + 1) * 4], [esink], ob.t[:, kvh * 256:(kvh + 1) * 256], [ob])
                yield
            mix = mix_r.next()
            S["mix"] = mix
            rs, rs_s = rstd_of(ob.t[:], 512, [ob])
            k.op("dve", lambda e: e.tensor_scalar(out=mix.t[:, 512:1024], in0=ob.t[:], scalar1=rs,
                                                  scalar2=None, op0=ALU.mult), r=[ob, rs_s], w=[mix])
            yield
            rs2, rs2_s = rstd_of(pj.t[:, 0:256], 256, [pj])
            cqn = cqn_r.next()
            k.op("dve", lambda e: e.tensor_scalar(out=cqn.t[:], in0=pj.t[:, 0:256], scalar1=rs2,
                                                  scalar2=None, op0=ALU.mult), r=[pj, rs2_s], w=[cqn])
            cqnT = cqnT_r.next()
            transposes(cqnT, cqn.t[:], 2, 128, [cqn], [cqnT], evac_eng="dve")
            yield
            for h in range(4):
                for c in range(2):
                    k.mm(pb[0].t[:, h * 128:(h + 1) * 128], wqb.t[:, c, h * 128:(h + 1) * 128], cqnT.t[:, c, :], c == 0, c == 1,
                         r=[wqb, cqnT], w=[pb[0]])
            qnT = qnT_r.next()
            k.op("dve", lambda e: e.tensor_copy(out=qnT.t[:].rearrange("p h t -> p (h t)"), in_=pb[0].t[:]), r=[pb[0]], w=[qnT])
            yield
            for h in range(4):
                k.mm(pb[1].t[:, h * 128:(h + 1) * 128], wukT.t[:, h, :], qnT.t[:, h, :], True, True, r=[wukT, qnT], w=[pb[1]])
            QT = QT_r.next()
            S["QT"] = QT
            k.op("dve", lambda e: e.tensor_copy(out=QT.t[:], in_=pb[1].t[:]), r=[pb[1]], w=[QT])
            yield
            for c in range(2):
                k.mm(pb[2].t[:, 0:256], cqnT.t[:, c, :], wqb.t[:, c, 512:768], c == 0, c == 1, r=[wqb, cqnT], w=[pb[2]])
            qrb = qrb_r.next()
            apply_rope(qrb.t[:], pb[2].t[:, 0:256], 4, tabo.t[:, 0, ki, :], tabo.t[:, 1, ki, :], tabo, [pb[2]], [qrb])
            yield
            QrT = QrT_r.next()
            S["QrT"] = QrT
            transposes(QrT, qrb.t[:], 4, 64, [qrb], [QrT], evac_eng="dve")
            mk = mk_r.next()
            S["mk"] = mk
            k.dma(mk.t[:].rearrange("p a b -> p (a b)"), mmask_d[ki], mk, w=[mk], q="act")
            yield

        def mla(ki, gens):
            S = st_[ki]
            QT, QrT, mk = S["QT"], S["QrT"], S["mk"]
            L = n_keyblocks(ki)
            nch = (L + 7) // 8
            chunks = []
            po = [pb[5], pb[6]]

            def load_chunk(ci):
                kcc, krc, vcc = kcc_r.next(), krc_r.next(), vcc_r.next()
                nb = min(8, L - ci * 8)
                KQ = cfg.get("kvq", "act")
                k.dma(kcc.t[:, 0:nb * 128], kc_s[ci, :, 0:nb * 128], kcc, w=[kcc], q=KQ)
                k.dma(krc.t[0:64, 0:nb * 128], kr_s[ci, :, 0:nb * 128], krc, w=[krc], q=KQ)
                k.dma(vcc.t[:, 0:nb, :].rearrange("p a b -> p (a b)"), vc_s[ci, :, 0:nb * 129], vcc, w=[vcc], q=KQ)
                return (kcc, krc, vcc)

            def scores(j, psc):
                kcc, krc, vcc = chunks[j // 8]
                jj = j % 8
                k.mm(psc.t[:], kcc.t[:, jj * 128:(jj + 1) * 128], QT.t[:], True, False, r=[kcc, QT], w=[psc])
                k.mm(psc.t[:], krc.t[:, jj * 128:(jj + 1) * 128], QrT.t[:].rearrange("p h t -> p (h t)"), False, True,
                     r=[krc, QrT], w=[psc])

            def step_others():
                for gx in gens:
                    try:
                        next(gx)
                        return
                    except StopIteration:
                        continue

            chunks.append(load_chunk(0))
            if nch > 1:
                chunks.append(load_chunk(1))
            scores(0, pb[3])
            for j in range(L):
                if j + 1 < L:
                    if (j + 1) % 8 == 0 and (j + 1) // 8 + 1 < nch:
                        chunks.append(load_chunk((j + 1) // 8 + 1))
                    scores(j + 1, pb[3 + ((j + 1) % 2)])
                psc = pb[3 + (j % 2)]
                PT = PT_r.next()
                k.act(PT.t[:], psc.t[:], AF.Exp, r=[psc], w=[PT], scale=SC_MLA)
                if j >= L - 4:
                    jj = j - (L - 4)
                    k.op("dve", lambda e, PT=PT, jj=jj: e.tensor_tensor(
                        out=PT.t[:].rearrange("p (g t) -> p g t", t=128), in0=PT.t[:].rearrange("p (g t) -> p g t", t=128),
                        in1=mk.t[:, jj, :].unsqueeze(1).to_broadcast([128, 4, 128]), op=ALU.mult), r=[PT, mk], w=[PT])
                vcc = chunks[j // 8][2]
                for h in range(4):
                    pbo = po[h // 2]
                    hh = h % 2
                    k.mm(pbo.t[:, hh * 129:(hh + 1) * 129], PT.t[:, h * 128:(h + 1) * 128], vcc.t[:, j % 8, :],
                         j == 0 and hh == 0, j == L - 1, r=[PT, vcc], w=[pbo], skip=True)
                step_others()
            olat = olat_r.next()
            S["olat"] = olat
            norm_bank(pb[5], 2, 128, None, [], olat.t[:, 0:256], [olat])
            norm_bank(pb[6], 2, 128, None, [], olat.t[:, 256:512], [olat])

        def post(ki):
            S = st_[ki]
            olat, mix, xt = S["olat"], S["mix"], S["xt"]
            olatT = olatT_r.next()
            transposes(olatT, olat.t[:], 4, 128, [olat], [olatT], evac_eng="dve")
            yield
            for h in range(4):
                k.mm(pb[0].t[:, h * 128:(h + 1) * 128], olatT.t[:, h, :], wuv.t[:, h, :], True, True, r=[olatT, wuv], w=[pb[0]])
            oa = of32_r.next()
            k.op("dve", lambda e: e.tensor_copy(out=oa.t[:], in_=pb[0].t[:]), r=[pb[0]], w=[oa])
            yield
            rs, rs_s = rstd_of(oa.t[:], 512, [oa])
            k.op("dve", lambda e: e.tensor_scalar(out=mix.t[:, 0:512], in0=oa.t[:], scalar1=rs,
                                                  scalar2=None, op0=ALU.mult), r=[oa, rs_s], w=[mix])
            mixT = hTring.next()
            transposes(mixT, mix.t[:], 8, 128, [mix], [mixT], evac_eng="dve")
            yield
            x1 = x1_r.next()
            for half in range(2):
                for c in range(8):
                    k.mm(pb[half].t[:], mixT.t[:, c, :], wout.t[:, c, half * 512:(half + 1) * 512], c == 0, c == 7,
                         r=[mixT, wout], w=[pb[half]])
                k.op("dve", lambda e, half=half: e.tensor_tensor(
                    out=x1.t[:, half * 512:(half + 1) * 512], in0=pb[half].t[:], in1=xt.t[:, half * 512:(half + 1) * 512],
                    op=ALU.add), r=[pb[half], xt], w=[x1])
                yield
            if dbg == "p1b" and ki == n_own - 1:
                x1c = k.sb("x1c", [128, D], F32)
                S["x1c"] = x1c
                k.op("pool", lambda e: e.tensor_copy(out=x1c.t[:], in_=x1.t[:]), r=[x1], w=[x1c])
            h2T = norm_T(x1.t[:], [x1], evac_eng="dve")
            yield
            for h in range(4):
                for c in range(8):
                    k.mm(pb[2].t[:, h * 128:(h + 1) * 128], wcq.t[:, c, h * 128:(h + 1) * 128], h2T.t[:, c, :], c == 0, c == 7,
                         r=[wcq, h2T], w=[pb[2]])
                if h % 2 == 1:
                    yield
            QxT = QxT_r.next()
            k.op("dve", lambda e: e.tensor_copy(out=QxT.t[:].rearrange("p h t -> p (h t)"), in_=pb[2].t[:]), r=[pb[2]], w=[QxT])
            yield
            PTs = []
            for mc in range(2):
                psc = pb[mc]
                for h in range(4):
                    k.mm(psc.t[:, h * 128:(h + 1) * 128], KxT.t[:, h, mc * 128:(mc + 1) * 128], QxT.t[:, h, :], True, True,
                         r=[KxT, QxT], w=[psc])
                PT = PS_r.next()
                k.act(PT.t[:], psc.t[:], AF.Exp, r=[psc], w=[PT], scale=SC_X)
                PTs.append(PT)
                yield
            ox = olat_r.next()
            for hp in range(2):
                for mc in range(2):
                    for hh in range(2):
                        h = hp * 2 + hh
                        k.mm(pb[2].t[:, hh * 129:(hh + 1) * 129], PTs[mc].t[:, h * 128:(h + 1) * 128], Vx.t[:, mc, h, :],
                             mc == 0 and hh == 0, mc == 1, r=[PTs[mc], Vx], w=[pb[2]], skip=True)
                norm_bank(pb[2], 2, 128, None, [], ox.t[:, hp * 256:(hp + 1) * 256], [ox])
                yield
            oxT = olatT_r.next()
            transposes(oxT, ox.t[:], 4, 128, [ox], [oxT], evac_eng="dve")
            yield
            x2 = x1
            for half in range(2):
                for c in range(4):
                    k.mm(pb[half].t[:], oxT.t[:, c, :], wco.t[:, c, half * 512:(half + 1) * 512], c == 0, c == 3,
                         r=[oxT, wco], w=[pb[half]])
                k.op("dve", lambda e, half=half: e.tensor_tensor(
                    out=x2.t[:, half * 512:(half + 1) * 512], in0=pb[half].t[:], in1=x1.t[:, half * 512:(half + 1) * 512],
                    op=ALU.add), r=[pb[half], x1], w=[x2])
                yield
            k.dma(x2_s[ki * 128:(ki + 1) * 128, :], x2.t[:], x2, r=[x2])
            if dbg == "p1b" and ki == n_own - 1:
                dt_ = k.sb("dbgt", [128, 1024], F32)
                for (c0, srcb, wd) in ((0, mix, 1024), (1024, S["x1c"], 1024), (2048, x2, 1024), (3072, olat, 512)):
                    k.op("dve", lambda e, srcb=srcb, wd=wd: e.tensor_copy(out=dt_.t[:, 0:wd], in_=srcb.t[:]), r=[srcb], w=[dt_])
                    k.dma(dbg_d[:, c0:c0 + wd], dt_.t[:, 0:wd], dt_, r=[dt_], w=[])
            st_.pop(ki)

        stop = cfg.get("p1b_stop")
        n_run = 0 if stop == "setup" else n_own
        if n_run > 0:
            for _ in pre(0):
                pass
        prev_post = None
        for ki in range(n_run):
            gens = []
            if prev_post is not None:
                gens.append(prev_post)
            nxt = pre(ki + 1) if ki + 1 < n_run else None
            if nxt is not None:
                gens.append(nxt)
            mla(ki, gens + ([p0g] if p0g is not None else []))
            for gx in gens:
                for _ in gx:
                    pass
            prev_post = post(ki)
        if prev_post is not None:
            for _ in prev_post:
                pass
        if p0g is not None:
            for _ in p0g:
                pass

    k.pop_scope()


    k.push_scope()
    if do_p2:
        wq_s = P.dscr("wq_s", [8, 128, 2048], BF16)
        gfin = k.sb("gfin", [128, D], F32)
        k.dma(gfin.t[:], gfin_d[0].partition_broadcast(128), gfin, w=[gfin])
        iota128 = k.sb("iota128", [128, 128], F32)
        iota16 = k.sb("iota16", [128, 16], F32)
        k.dma(iota128.t[:], iota128_d, iota128, w=[iota128])
        k.dma(iota16.t[:], iota16_d, iota16, w=[iota16])
        keysT = k.sb("keysT", [128, 16, 128], BF16)
        sg = stage.next()
        k.dma(sg.t[:], keysT_d, sg, w=[sg])
        k.op("dve", lambda e, sg=sg: e.tensor_copy(out=keysT.t[:].rearrange("p a b -> p (a b)"), in_=sg.t[:]), r=[sg], w=[keysT])
        wq_r = k.ring("wqc", [128, 8, 256], BF16, 2)
        wpq_v = w_pq_d.rearrange("(c p) n -> p c n", p=128)
        for fp in range(8):
            sg = stage.next()
            k.dma(sg.t[:].rearrange("p (c n) -> p c n", n=256), wpq_v[:, :, fp * 256:(fp + 1) * 256], sg, w=[sg])
            wc = wq_r.next()
            for c in range(8):
                eng = ("dve", "pool")[c % 2]
                k.op(eng, lambda e, wc=wc, sg=sg, c=c: e.tensor_scalar(out=wc.t[:, c, :], in0=sg.t[:, c * 256:(c + 1) * 256],
                                                                      scalar1=gcols.t[:, G_FFN + c:G_FFN + c + 1], scalar2=1.0,
                                                                      op0=ALU.mult, op1=ALU.mult), r=[sg, gcols], w=[wc])
            k.dma(wq_s[fp], wc.t[:].rearrange("p c n -> p (c n)"), wc, r=[wc])
        k.S.barrier()

        GT = k.sb("GT", [128, 128, 256], BF16)
        hfT2 = [k.sb("hfT%d" % i, [128, 8, 256], BF16) for i in range(2)]
        trip2 = [k.sb("trip%d" % i, [128, 3, 256], F32) for i in range(2)]
        x2r = Ring(list(xring.bufs) + [k.sb("xt3", [128, D], F32)])
        qT = k.sb("qT", [128, 16, 256], BF16)
        sc = k.sb("sc", [128, 2048], F32)
        sc_s = [T("sc%d" % i) for i in range(16)]
        tops = k.sb("tops", [128, 16, 16], F32)
        tops_s = [T("tops%d" % i) for i in range(16)]
        idxu = k.sb("idxu", [128, 16, 16], U32)
        idxu_s = [T("idxu%d" % i) for i in range(16)]
        idxf = k.sb("idxf", [128, 16, 16], F32)
        big8 = k.sb("big8", [128, 2048], F32)
        cand_s = [T("cand%d" % i) for i in range(8)]
        best = k.sb("best", [128, 8, 16], F32)
        best_s = [T("best%d" % i) for i in range(8)]
        posu = k.sb("posu", [128, 8, 16], U32)
        posu_s = [T("posu%d" % i) for i in range(8)]
        apu = k.sb("apu", [128, 8, 16], U32)
        bpu = k.sb("bpu", [128, 8, 16], U32)
        apf = k.sb("apf", [128, 8, 16], F32)
        bpf = k.sb("bpf", [128, 8, 16], F32)
        sel = k.sb("sel", [128, 3, 128], F32)
        exb = k.sb("exb", [128, 8, 16], F32)
        sm8 = k.sb("sm8", [128, 8], F32)
        Pm_r = k.ring("Pm", [128, 8, 64], BF16, 3)
        Qm_r = k.ring("Qm", [128, 8, 128], BF16, 3)
        ubn = [k.sb("ub%d" % i, [128, 4096], BF16) for i in range(2)]
        ub_r = Ring(ubn)

        class _V:
            pass
        vbn = []
        for sgb in stage.bufs:
            o = _V()
            o.t = sgb.t[:].bitcast(BF16)
            o.s = sgb.s
            vbn.append(o)
        vb_r = Ring(vbn)
        gl_r = k.ring("gl", [128, 256], BF16, 4)
        ga_r = k.ring("ga", [128, 256], BF16, 4)
        xts_of = {}
        pq = pb[0]

        def phase1(g):
            bs = g % 2
            hfT, trip = hfT2[bs], trip2[bs]
            xts = []
            xts_of[g] = xts
            for blk in range(2):
                xt = x2r.next()
                row0 = (g * 2 + blk) * 128
                k.dma(xt.t[:], x2_s[row0:row0 + 128, :], xt, w=[xt])
                xts.append(xt)
                rs, rs_s = rstd_of(xt.t[:], D, [xt])
                hb = hbring.next()
                k.op("dve", lambda e, hb=hb, xt=xt, rs=rs: e.tensor_scalar(out=hb.t[:], in0=xt.t[:], scalar1=rs, scalar2=None,
                                                                           op0=ALU.mult), r=[xt, rs_s], w=[hb])
                yield
                transposes(None, hb.t[:], 8, 128, [hb], [hfT], dst_ap=hfT.t[:, :, blk * 128:(blk + 1) * 128], pst=tp0)
                yield
            for fp in range(8):
                wc = wq_r.next()
                k.dma(wc.t[:].rearrange("p c n -> p (c n)"), wq_s[fp], wc, w=[wc])
                for f2 in range(2):
                    for c in range(8):
                        k.mm(pq.t[:, f2 * 256:(f2 + 1) * 256], wc.t[:, c, f2 * 128:(f2 + 1) * 128], hfT.t[:, c, :], c == 0, c == 7,
                             r=[wc, hfT], w=[pq])
                k.op("act", lambda e, fp=fp: e.copy(out=qT.t[:, fp * 2:fp * 2 + 2, :].rearrange("p a b -> p (a b)"),
                                                   in_=pq.t[:]), r=[pq], w=[qT])
                yield
            for blk in range(2):
                for qd in range(4):
                    for i4 in range(4):
                        hc = qd * 4 + i4
                        k.mm(pq.t[:, i4 * 128:(i4 + 1) * 128], qT.t[:, hc, blk * 128:(blk + 1) * 128], keysT.t[:, hc, :], True, True,
                             r=[qT, keysT], w=[pq])
                    k.op("act", lambda e, qd=qd: e.copy(out=sc.t[:, qd * 512:(qd + 1) * 512], in_=pq.t[:]),
                         r=[pq], w=sc_s[qd * 4:qd * 4 + 4])
                    yield

                def grp(i):
                    return sc.t[:, i * 128:(i + 1) * 128]
                for i in range(16):
                    k.op("dve", lambda e, i=i: e.max(out=tops.t[:, i, 0:8], in_=grp(i)), r=[sc_s[i]], w=[tops_s[i]])
                yield
                for i in range(16):
                    k.op("dve", lambda e, i=i: e.max_index(out=idxu.t[:, i, 0:8], in_max=tops.t[:, i, 0:8], in_values=grp(i)),
                         r=[sc_s[i], tops_s[i]], w=[idxu_s[i]])
                yield
                for i in range(16):
                    k.op("dve", lambda e, i=i: e.match_replace(out=grp(i), in_to_replace=tops.t[:, i, 0:8], in_values=grp(i),
                                                              imm_value=NEG), r=[tops_s[i], sc_s[i]], w=[sc_s[i]])
                yield
                for i in range(16):
                    k.op("dve", lambda e, i=i: e.max(out=tops.t[:, i, 8:16], in_=grp(i)), r=[sc_s[i]], w=[tops_s[i]])
                yield
                for i in range(16):
                    k.op("dve", lambda e, i=i: e.max_index(out=idxu.t[:, i, 8:16], in_max=tops.t[:, i, 8:16], in_values=grp(i)),
                         r=[sc_s[i], tops_s[i]], w=[idxu_s[i]])
                k.op("dve", lambda e: e.tensor_copy(out=idxf.t[:], in_=idxu.t[:]), r=idxu_s, w=[idxf])
                yield
                tv = tops.t[:].rearrange("p (h c) a -> p h c a", c=2)
                candv = big8.t[:].rearrange("p (h a b) -> p h a b", a=16, b=16)
                k.op("dve", lambda e: e.tensor_tensor(out=candv, in0=tv[:, :, 0, :].unsqueeze(3).to_broadcast([128, 8, 16, 16]),
                                                      in1=tv[:, :, 1, :].unsqueeze(2).to_broadcast([128, 8, 16, 16]), op=ALU.add),
                     r=tops_s, w=cand_s)
                yield

                def cnd(h):
                    return big8.t[:, h * 256:(h + 1) * 256]
                for h in range(8):
                    k.op("dve", lambda e, h=h: e.max(out=best.t[:, h, 0:8], in_=cnd(h)), r=[cand_s[h]], w=[best_s[h]])
                yield
                for h in range(8):
                    k.op("dve", lambda e, h=h: e.max_index(out=posu.t[:, h, 0:8], in_max=best.t[:, h, 0:8], in_values=cnd(h)),
                         r=[cand_s[h], best_s[h]], w=[posu_s[h]])
                yield
                for h in range(8):
                    k.op("dve", lambda e, h=h: e.match_replace(out=cnd(h), in_to_replace=best.t[:, h, 0:8], in_values=cnd(h),
                                                              imm_value=NEG), r=[best_s[h], cand_s[h]], w=[cand_s[h]])
                yield
                for h in range(8):
                    k.op("dve", lambda e, h=h: e.max(out=best.t[:, h, 8:16], in_=cnd(h)), r=[cand_s[h]], w=[best_s[h]])
                yield
                for h in range(8):
                    k.op("dve", lambda e, h=h: e.max_index(out=posu.t[:, h, 8:16], in_max=best.t[:, h, 8:16], in_values=cnd(h)),
                         r=[cand_s[h], best_s[h]], w=[posu_s[h]])
                yield
                k.op("dve", lambda e: e.tensor_single_scalar(out=apu.t[:], in_=posu.t[:], scalar=4, op=ALU.logical_shift_right),
                     r=posu_s, w=[apu])
                k.op("dve", lambda e: e.tensor_single_scalar(out=bpu.t[:], in_=posu.t[:], scalar=15, op=ALU.bitwise_and),
                     r=posu_s, w=[bpu])
                k.op("dve", lambda e: e.tensor_copy(out=apf.t[:], in_=apu.t[:]), r=[apu], w=[apf])
                k.op("dve", lambda e: e.tensor_copy(out=bpf.t[:], in_=bpu.t[:]), r=[bpu], w=[bpf])
                yield
                Ev = big8.t[:].rearrange("p (h r a) -> p h r a", r=16, a=16)
                iov = iota16.t[:].unsqueeze(1).unsqueeze(1).to_broadcast([128, 8, 16, 16])
                fv = idxf.t[:].rearrange("p (h c) a -> p h c a", c=2)
                for which, pf in ((0, apf), (1, bpf)):
                    k.op("dve", lambda e, pf=pf: e.tensor_tensor(out=Ev, in0=pf.t[:].unsqueeze(3).to_broadcast([128, 8, 16, 16]),
                                                                in1=iov, op=ALU.is_equal), r=[pf, iota16] + cand_s, w=cand_s)
                    yield
                    k.op("dve", lambda e, which=which: e.tensor_tensor(
                        out=Ev, in0=Ev, in1=fv[:, :, which, :].unsqueeze(2).to_broadcast([128, 8, 16, 16]), op=ALU.mult),
                        r=[idxf] + cand_s, w=cand_s)
                    yield
                    k.op("dve", lambda e, which=which: e.tensor_reduce(
                        out=sel.t[:, which, :].rearrange("p (h r) -> p h r", r=16), in_=Ev, axis=AX.X, op=ALU.add),
                        r=cand_s, w=[sel])
                    yield
                k.op("dve", lambda e: e.tensor_tensor(out=exb.t[:], in0=best.t[:], in1=best.t[:, :, 0:1].to_broadcast([128, 8, 16]),
                                                      op=ALU.subtract), r=best_s, w=[exb])
                k.act(exb.t[:], exb.t[:], AF.Exp, r=[exb], w=[exb])
                k.op("dve", lambda e: e.tensor_reduce(out=sm8.t[:], in_=exb.t[:], axis=AX.X, op=ALU.add), r=[exb], w=[sm8])
                k.op("dve", lambda e: e.reciprocal(out=sm8.t[:], in_=sm8.t[:]), r=[sm8], w=[sm8])
                k.op("dve", lambda e: e.tensor_tensor(out=sel.t[:, 2, :].rearrange("p (h r) -> p h r", r=16), in0=exb.t[:],
                                                      in1=sm8.t[:].unsqueeze(2).to_broadcast([128, 8, 16]), op=ALU.mult),
                     r=[exb, sm8], w=[sel])
                yield
                for w3 in range(3):
                    k.tr(pq.t[:, w3 * 128:(w3 + 1) * 128], sel.t[:, w3, :], identf.t[:], r=[sel, identf], w=[pq])
                k.op("act", lambda e, blk=blk: e.copy(out=trip.t[:, :, blk * 128:(blk + 1) * 128],
                                                      in_=pq.t[:, 0:384].rearrange("p (a t) -> p a t", t=128)),
                     r=[pq], w=[trip])
                yield

        GT_s = [T("GT_lo"), T("GT_hi")]
        gbank = _V()
        gbank.t = ps_tp.t[:].bitcast(F32)
        gbank.s = ps_tp.s

        def ggen_half(g, half):
            trip = trip2[g % 2]
            i0_ = half * 64
            io_q = iota128.t[:].unsqueeze(1).to_broadcast([128, 8, 128])
            io_p = iota128.t[:, i0_:i0_ + 64].unsqueeze(1).to_broadcast([128, 8, 64])
            slabs = {}

            def build(sl):
                t0 = sl * 8
                Pm, Qm = Pm_r.next(), Qm_r.next()
                slabs[sl] = (Pm, Qm)
                k.op("dve", lambda e: e.tensor_tensor(
                    out=Qm.t[:], in0=io_q, in1=trip.t[:, 1, t0:t0 + 8].unsqueeze(2).to_broadcast([128, 8, 128]), op=ALU.is_equal),
                    r=[iota128, trip], w=[Qm])
                k.op("dve", lambda e: e.tensor_tensor(
                    out=Pm.t[:], in0=io_p, in1=trip.t[:, 0, t0:t0 + 8].unsqueeze(2).to_broadcast([128, 8, 64]),
                    op=ALU.is_equal), r=[iota128, trip], w=[Pm])
                k.op("pool", lambda e: e.tensor_tensor(
                    out=Pm.t[:], in0=Pm.t[:], in1=trip.t[:, 2, t0:t0 + 8].unsqueeze(2).to_broadcast([128, 8, 64]),
                    op=ALU.mult), r=[Pm, trip], w=[Pm])

            def consume(sl):
                t0 = sl * 8
                Pm, Qm = slabs.pop(sl)
                for t in range(8):
                    k.mm(gbank.t[:, t * 64:(t + 1) * 64], Qm.t[:, t, :], Pm.t[:, t, :], True, True, r=[Qm, Pm], w=[gbank])
                k.op("act", lambda e: e.copy(out=GT.t[:, i0_:i0_ + 64, t0:t0 + 8],
                                             in_=gbank.t[:].rearrange("p (t i) -> p i t", t=8)),
                     r=[gbank], w=[GT_s[half]])

            build(0)
            yield
            build(1)
            yield
            for sl in range(32):
                consume(sl)
                if sl + 2 < 32:
                    build(sl + 2)
                yield

        ybank = [pb[1], pb[2], pb[3], pb[4]]
        tp0 = _V()
        tp0.t = pb[0].t[:].bitcast(BF16)
        tp0.s = pb[0].s
        pu_slots = [(pb[5].t[:, 0:256], pb[5].s), (pb[6].t[:, 0:256], pb[6].s)]

        def step(gens):
            for gx in gens:
                next(gx, None)

        def drain(gens):
            for gx in gens:
                for _ in gx:
                    pass

        def eloop(g, first_gens, second_gens):
            hfT = hfT2[g % 2]
            n_i = n_eg * 4
            n_half = 64
            bufs = {}

            def u_part(i):
                eg, c = i // 4, i % 4
                if c == 0:
                    ub, vb = ub_r.next(), vb_r.next()
                    k.dma(ub.t[:], us_s[eg], ub, w=[ub], q="act")
                    k.dma(vb.t, vs_s[eg], vb, w=[vb], q="act")
                    bufs[eg] = (ub, vb)
                ub, vb = bufs[eg]
                pu_ap, pu_s = pu_slots[i % 2]
                for kc in range(8):
                    k.mm(pu_ap, ub.t[:, kc * 512 + c * 128:kc * 512 + (c + 1) * 128], hfT.t[:, kc, :], kc == 0, kc == 7,
                         r=[ub, hfT], w=[pu_s])
                gl = gl_r.next()
                k.act(gl.t[:], pu_ap, AF.Gelu, r=[pu_s], w=[gl])
                ga = ga_r.next()
                gts = GT_s[0] if i < 64 else GT_s[1]
                k.op("dve", lambda e, ga=ga, gl=gl, i=i: e.tensor_tensor(out=ga.t[:], in0=gl.t[:], in1=GT.t[:, i, :], op=ALU.mult),
                     r=[gl, gts], w=[ga])
                return ga

            def v_part(i, ga):
                eg, c = i // 4, i % 4
                ub, vb = bufs[eg]
                for blk in range(2):
                    for half in range(2):
                        k.mm(ybank[blk * 2 + half].t[:], ga.t[:, blk * 128:(blk + 1) * 128],
                             vb.t[:, c * 1024 + half * 512:c * 1024 + (half + 1) * 512], i == 0, i == n_i - 1,
                             r=[ga, vb], w=[ybank[blk * 2 + half]])

            gas = {0: u_part(0)}
            for i in range(n_i):
                if i == n_half:
                    drain(first_gens)
                if i + 1 < n_i:
                    if i + 1 == n_half:
                        drain(first_gens)
                    gas[i + 1] = u_part(i + 1)
                v_part(i, gas.pop(i))
                if i >= 1:
                    step(first_gens if i < n_half else second_gens)
            drain(first_gens)
            drain(second_gens)

        def finalize(g):
            xts = xts_of.pop(g)
            for blk in range(2):
                xt = xts[blk]
                xo = xt
                for half in range(2):
                    k.op("dve", lambda e, xo=xo, xt=xt, blk=blk, half=half: e.tensor_tensor(
                        out=xo.t[:, half * 512:(half + 1) * 512], in0=ybank[blk * 2 + half].t[:],
                        in1=xt.t[:, half * 512:(half + 1) * 512], op=ALU.add), r=[ybank[blk * 2 + half], xt], w=[xo])
                rs, rs_s = rstd_of(xo.t[:], D, [xo])
                k.op("dve", lambda e, xo=xo, rs=rs: e.scalar_tensor_tensor(out=xo.t[:], in0=xo.t[:], scalar=rs, in1=gfin.t[:],
                                                                         op0=ALU.mult, op1=ALU.mult), r=[xo, rs_s, gfin], w=[xo])
                row0 = (g * 2 + blk) * 128
                k.dma(out_d[row0:row0 + 128, :], xo.t[:], xo, r=[xo])

        for _ in phase1(0):
            pass
        drain([ggen_half(0, 0), ggen_half(0, 1)])
        for g in range(n_grp):
            first = []
            if g > 0:
                first.append(ggen_half(g, 1))
            if g + 1 < n_grp:
                first.append(phase1(g + 1))
            second = [ggen_half(g + 1, 0)] if g + 1 < n_grp else []
            eloop(g, first, second)
            finalize(g)
            trip = trip2[g % 2]
            if dbg == "p2" and g == n_grp - 1:
                dt_ = k.sb("dbgt", [128, 1024], F32)
                k.op("pool", lambda e: e.tensor_copy(out=dt_.t[:, 0:768], in_=trip.t[:].rearrange("p a t -> p (a t)")), r=[trip], w=[dt_])
                k.dma(dbg_d[:, 0:768], dt_.t[:, 0:768], dt_, r=[dt_])
    k.pop_scope()

    P.state = dict(locals())
    return P


def finish(P):
    P.k.S.barrier()
    P.k.S.emit()
    P.st.close()
    return P.nc


def _bf(a):
    return np.asarray(a, dtype=np.float32).astype(ml_dtypes.bfloat16)


def make_in_maps(x, mem, positions, norm_mix, w_in, q_a_norm, w_q_b, kv_a_norm, w_kv_b,
                 swa_sinks, out_norm_mla, out_norm_swa, w_out, norm_cross, norm_mem,
                 w_cq, w_ck, w_cv, w_co, norm_ffn, peer_w_q, peer_keys, peer_u, peer_v, norm_final):
    f32 = np.float32
    x = np.asarray(x, f32)
    mem = np.asarray(mem, f32)
    positions = np.asarray(positions, np.int32)
    w_in0 = np.asarray(w_in[0], f32)
    shared = {}
    shared["identb"] = np.eye(128, dtype=f32).astype(ml_dtypes.bfloat16)
    shared["identf"] = np.eye(128, dtype=f32)
    inv = (np.float32(10000.0) ** (-np.arange(32, dtype=f32) / np.float32(32))).astype(f32)
    shared["invf"] = np.ascontiguousarray(np.broadcast_to(inv[None, :], (128, 32))).astype(f32)
    kk = np.arange(128, dtype=f32)
    kaug = np.zeros((2, 2, 128), f32)
    kaug[0, :, :] = 1.0
    kaug[1, 0, :] = kk - 128.0
    kaug[1, 1, :] = kk
    shared["kaug"] = _bf(kaug.reshape(2, 256))
    slopes = 2.0 ** (-(np.arange(1, 9, dtype=f32)))
    qaug = np.zeros((2, 2, 4, 128), f32)
    for kvh in range(2):
        for g in range(4):
            s = slopes[kvh * 4 + g]
            qaug[0, kvh, g, :] = -s * kk * 8.0
            qaug[1, kvh, g, :] = s * 8.0
    shared["qaug"] = _bf(qaug.reshape(2, 1024))
    shared["iota128"] = np.ascontiguousarray(np.broadcast_to(kk[None, :], (128, 128))).astype(f32)
    shared["iota16"] = np.ascontiguousarray(np.broadcast_to(kk[None, :16], (128, 16))).astype(f32)
    gc = np.zeros((128, NGC), f32)

    def cols(vec, c0):
        v = np.asarray(vec, f32).reshape(-1, 128)
        gc[:, c0:c0 + v.shape[0]] = v.T

    cols(norm_mix[0], G_MIX)
    cols(norm_cross[0], G_CROSS)
    cols(norm_mem[0], G_MEM)
    cols(norm_ffn[0], G_FFN)
    cols(q_a_norm[0], G_QA)
    cols(kv_a_norm[0], G_KVA)
    cols(np.concatenate([np.asarray(out_norm_mla[0], f32), np.asarray(out_norm_swa[0], f32)]), G_OUT)
    shared["gcols"] = gc
    shared["kvarow"] = np.asarray(kv_a_norm[0], f32).reshape(1, 128)
    shared["sinks"] = np.asarray(swa_sinks[0], f32).reshape(1, 8)
    shared["gfin"] = np.asarray(norm_final, f32).reshape(1, D)
    shared["w_kvl"] = np.ascontiguousarray(w_in0[:, 256:448])
    shared["w_in"] = w_in0
    wqb = np.asarray(w_q_b[0], f32).reshape(256, 4, 192)
    shared["w_qb"] = np.ascontiguousarray(np.concatenate([wqb[:, :, :128].reshape(256, 512),
                                                          wqb[:, :, 128:].reshape(256, 256)], axis=1))
    wkvb = np.asarray(w_kv_b[0], f32).reshape(128, 4, 256)
    shared["w_ukT"] = np.ascontiguousarray(wkvb[:, :, :128].transpose(2, 1, 0)).reshape(128, 512)
    shared["w_uv"] = np.ascontiguousarray(wkvb[:, :, 128:]).reshape(128, 512)
    shared["w_out"] = np.asarray(w_out[0], f32)
    shared["w_cq"] = np.asarray(w_cq[0], f32)
    shared["w_ck"] = np.asarray(w_ck[0], f32)
    shared["w_cv"] = np.asarray(w_cv[0], f32)
    shared["w_co"] = np.asarray(w_co[0], f32)
    shared["w_pq"] = np.asarray(peer_w_q[0], f32)
    pk = np.asarray(peer_keys[0], f32)
    shared["keysT"] = np.ascontiguousarray(pk.transpose(3, 0, 1, 2)).reshape(128, 2048)
    shared["uT"] = np.ascontiguousarray(np.asarray(peer_u[0], f32).T)
    shared["v"] = np.asarray(peer_v[0], f32)

    tri_c = (kk[:, None] <= kk[None, :]).astype(f32)
    tri_p = (kk[:, None] > kk[None, :]).astype(f32)
    in_maps = []
    for c in range(NCORE):
        b, r = c // 4, c % 4
        qb = own_blocks(r)
        m = dict(shared)
        m["xb"] = x[b]
        xo = x[b].reshape(NBLK, 128, D)
        m["xown"] = np.ascontiguousarray(xo[qb]).reshape(NOWN * 128, D)
        prev = [max(n - 1, 0) for n in qb]
        m["xprev"] = np.ascontiguousarray(xo[prev]).reshape(NOWN * 128, D)
        m["memb"] = mem[b]
        pb_ = positions[b].reshape(NBLK, 128)
        m["posb"] = np.ascontiguousarray(pb_.T)
        m["poso"] = np.ascontiguousarray(pb_[qb].T)
        mm_ = np.zeros((NOWN, 128, 4, 128), f32)
        sm_ = np.zeros((NOWN, 128, 2, 128), f32)
        for ki, n in enumerate(qb):
            L = n_keyblocks(ki)
            for jj in range(4):
                j = L - 4 + jj
                if j < n:
                    mm_[ki, :, jj, :] = 1.0
                elif j == n:
                    mm_[ki, :, jj, :] = tri_c
            if n > 0:
                sm_[ki, :, 0, :] = tri_p
            sm_[ki, :, 1, :] = tri_c
        m["mmask"] = _bf(mm_.reshape(NOWN, 128, 512))
        m["smask"] = _bf(sm_.reshape(NOWN, 128, 256))
        in_maps.append(m)
    return in_maps


def kernel(**inputs):
    in_maps = make_in_maps(**inputs)
    P = build()
    nc = finish(P)
    res = run_bass_kernel_spmd(nc, in_maps, core_ids=list(range(NCORE)))
    out = np.zeros((2, SEQ, D), np.float32)
    for c in range(NCORE):
        b, r = c // 4, c % 4
        qb = own_blocks(r)
        o = np.asarray(res.results[c]["out"], np.float32).reshape(NOWN, 128, D)
        ov = out[b].reshape(NBLK, 128, D)
        ov[qb] = o
    return out
```

```python
import numpy as np
import ml_dtypes
from contextlib import ExitStack

import concourse.bass as bass
import concourse.mybir as mybir
from concourse.bass_utils import run_bass_kernel_spmd

F32 = mybir.dt.float32
BF16 = mybir.dt.bfloat16
I32 = mybir.dt.int32
U32 = mybir.dt.uint32
AF = mybir.ActivationFunctionType
ALU = mybir.AluOpType
AX = mybir.AxisListType

D = 1024
SEQ = 16384
NBLK = SEQ // 128
NCORE = 8
EPS = 1e-6
NOWN = 32
NEXP = 16384


def own_blocks(r):
    s = set()
    for m in range(16):
        s.add(8 * m + r)
        s.add(8 * m + 7 - r)
    return sorted(s)


class T:
    __slots__ = ("name", "w", "r", "dsem", "dcnt")

    def __init__(self, name):
        self.name = name
        self.w = None
        self.r = {}
        self.dsem = None
        self.dcnt = 0


class Sched:
    ENGS = ("pe", "act", "dve", "pool", "sp")

    def __init__(self, nc, st):
        self.nc = nc
        self.st = st
        self.prog = {e: [] for e in self.ENGS}
        self.seen = {e: {} for e in self.ENGS}
        self.esem = {}
        for e in ("pe", "act", "dve", "pool"):
            self.esem[e] = st.enter_context(nc.semaphore("s_" + e))
        self.bsem = st.enter_context(nc.semaphore("s_bar"))
        self.bcnt = 0
        self.dtiles = []
        self.fuse_waits = True

    def _deps(self, eng, reads, writes):
        evs = []
        for t in reads:
            if t.w is not None:
                evs.append(t.w)
        for t in writes:
            if t.w is not None:
                evs.append(t.w)
            evs.extend(t.r.values())
        waits = []
        seen = self.seen[eng]
        for ev in evs:
            if ev[0] == "e":
                _, f, idx = ev
                if f == eng and eng == "pe":
                    continue
                if seen.get(f, -1) >= idx:
                    continue
                seen[f] = idx
                self.prog[f][idx]["inc"] = True
                waits.append(ev)
            else:
                _, owner, cnt = ev
                key = ("d", id(owner))
                if seen.get(key, 0) >= cnt:
                    continue
                cur = owner.dcnt
                seen[key] = cur
                waits.append(("d", owner, cur))
        return waits

    def op(self, eng, fn, reads=(), writes=()):
        waits = self._deps(eng, reads, writes)
        idx = len(self.prog[eng])
        self.prog[eng].append({"fn": fn, "waits": waits, "inc": False, "dma": None})
        ev = ("e", eng, idx)
        for t in reads:
            t.r[eng] = ev
        for t in writes:
            t.w = ev
            t.r = {}

    def dma(self, fn, owner, reads=(), writes=(), q="sp"):
        waits = self._deps(q, reads, writes)
        if owner.dsem is None:
            owner.dsem = self.st.enter_context(self.nc.semaphore("d_" + owner.name))
            self.dtiles.append(owner)
        owner.dcnt += 16
        self.prog[q].append({"fn": fn, "waits": waits, "inc": False, "dma": owner})
        ev = ("d", owner, owner.dcnt)
        for t in reads:
            t.r[("d", id(owner))] = ev
        for t in writes:
            t.w = ev
            t.r = {}

    def barrier(self):
        waits = []
        for o in self.dtiles:
            if o.dcnt > 0:
                waits.append(("d", o, o.dcnt))
        for e in ("pe", "act", "dve", "pool"):
            idx = len(self.prog[e]) - 1
            while idx >= 0 and self.prog[e][idx]["fn"] is None:
                idx -= 1
            if idx >= 0:
                self.prog[e][idx]["inc"] = True
                waits.append(("e", e, idx))
        self.bcnt += 1
        self.prog["sp"].append({"fn": None, "waits": waits, "inc": False, "dma": None, "bar": self.bcnt})
        for e in ("pe", "act", "dve", "pool"):
            self.prog[e].append({"fn": None, "waits": [("b", self.bcnt)], "inc": False, "dma": None})

    def emit(self):
        nc = self.nc
        for e in ("pe", "act", "dve", "pool"):
            c = 0
            for rec in self.prog[e]:
                if rec["inc"]:
                    c += 1
                    rec["semval"] = c
        engmap = {"pe": "tensor", "act": "scalar", "dve": "vector", "pool": "gpsimd", "sp": "sync"}
        with nc.Block() as block:
            for e in self.ENGS:
                def body(engobj, e=e):
                    def semval(w):
                        if w[0] == "e":
                            return self.esem[w[1]], self.prog[w[1]][w[2]]["semval"]
                        elif w[0] == "d":
                            return w[1].dsem, w[2]
                        return self.bsem, w[1]
                    for rec in self.prog[e]:
                        ws = rec["waits"]
                        fuse = None
                        if rec["fn"] is not None and ws and rec["dma"] is None and self.fuse_waits:
                            fuse = ws[-1]
                            ws = ws[:-1]
                        for w in ws:
                            engobj.wait_ge(*semval(w))
                        if rec["fn"] is None:
                            if rec.get("bar") is not None:
                                engobj.sem_inc(self.bsem, 1)
                            continue
                        ins = rec["fn"]()
                        if fuse is not None:
                            ins._wait_ge(*semval(fuse))
                        if rec["dma"] is not None:
                            ins.then_inc(rec["dma"].dsem, 16)
                        elif rec["inc"]:
                            ins.then_inc(self.esem[e], 1)
                getattr(block, engmap[e])(body)


class Buf:
    def __init__(self, t, name):
        self.t = t
        self.s = T(name)


class K:
    def __init__(self, nc, st):
        self.nc = nc
        self.st = st
        self.S = Sched(nc, st)
        self.eng = {"pe": nc.tensor, "act": nc.scalar, "dve": nc.vector, "pool": nc.gpsimd, "sp": nc.sync}
        self._n = 0
        self.main_st = st

    def push_scope(self):
        self.st = ExitStack()

    def pop_scope(self):
        self.S.barrier()
        self.st.close()
        self.st = self.main_st

    def sb(self, name, shape, dt):
        return Buf(self.st.enter_context(self.nc.sbuf_tensor("sb_" + name, shape, dt)), name)

    def ps(self, name, shape, dt):
        return Buf(self.st.enter_context(self.nc.psum_tensor("pp_" + name, shape, dt)), name)

    def ring(self, name, shape, dt, n):
        return Ring([self.sb("%s%d" % (name, i), shape, dt) for i in range(n)])

    def op(self, eng, fn, r=(), w=()):
        e = self.eng[eng]
        self.S.op(eng, lambda: fn(e), [getattr(x, "s", x) for x in r], [getattr(x, "s", x) for x in w])

    def dma(self, out, in_, owner, r=(), w=(), q="sp", **kw):
        e = self.eng[q]
        self.S.dma(lambda: e.dma_start(out=out, in_=in_, **kw), getattr(owner, "s", owner),
                   [getattr(x, "s", x) for x in r], [getattr(x, "s", x) for x in w], q=q)

    def mm(self, out, lhsT, rhs, start, stop, r, w, skip=False):
        self.op("pe", lambda e: e.matmul(out, lhsT, rhs, start=start, stop=stop, skip_group_check=skip), r, w)

    def tr(self, out, in_, ident, r, w):
        self.op("pe", lambda e: e.transpose(out, in_, ident), r, w)

    def act(self, out, in_, func, r, w, scale=1.0, bias=0.0):
        self.op("act", lambda e: e.activation(out, in_, func, bias=bias, scale=scale), r, w)


class Ring:
    def __init__(self, bufs):
        self.bufs = bufs
        self.i = 0

    def next(self):
        b = self.bufs[self.i % len(self.bufs)]
        self.i += 1
        return b


G_MIX, G_CROSS, G_MEM, G_FFN, G_QA, G_KVA, G_OUT = 0, 8, 16, 24, 32, 34, 35
NGC = 43
C1 = 6.28125
C2 = float(2.0 * np.pi - 6.28125)
MAGIC = 12582912.0
PI_LO = 3.1415925
NEG = -1.0e30


def n_keyblocks(kidx):
    m = kidx // 2
    return 8 * m + 4 if kidx % 2 == 0 else 8 * m + 8


class StopBuild(Exception):
    pass


class Prog:
    def __init__(self, cfg):
        self.cfg = cfg
        nc = bass.Bass("TRN2", target_bir_lowering=False)
        self.nc = nc
        self.st = ExitStack()
        self.k = K(nc, self.st)
        self.dram = {}

    def din(self, name, shape, dt):
        t = self.nc.dram_tensor(name, list(shape), dt, kind="ExternalInput").ap()
        self.dram[name] = t
        return t

    def dscr(self, name, shape, dt):
        t = self.nc.dram_tensor(name, list(shape), dt, kind="Internal").ap()
        self.dram[name] = t
        return t

    def dout(self, name, shape, dt):
        t = self.nc.dram_tensor(name, list(shape), dt, kind="ExternalOutput").ap()
        self.dram[name] = t
        return t


def build(cfg=None):
    cfg = dict(cfg or {})
    n_sb = cfg.get("n_sb", 16)
    n_own = cfg.get("n_own", NOWN)
    n_grp = cfg.get("n_grp", NOWN // 2)
    n_eg = cfg.get("n_eg", 32)
    do_p0 = cfg.get("p0", True)
    do_p1a = cfg.get("p1a", True)
    do_p1b = cfg.get("p1b", True)
    do_p2 = cfg.get("p2", True)
    dbg = cfg.get("dbg", None)

    P = Prog(cfg)
    nc, k, st = P.nc, P.k, P.st

    xb = P.din("xb", [SEQ, D], F32)
    xown = P.din("xown", [NOWN * 128, D], F32)
    xprev = P.din("xprev", [NOWN * 128, D], F32)
    memb = P.din("memb", [256, D], F32)
    posb = P.din("posb", [128, NBLK], I32)
    poso = P.din("poso", [128, NOWN], I32)
    mmask_d = P.din("mmask", [NOWN, 128, 4 * 128], BF16)
    smask_d = P.din("smask", [NOWN, 128, 2 * 128], BF16)
    identb_d = P.din("identb", [128, 128], BF16)
    identf_d = P.din("identf", [128, 128], F32)
    invf_d = P.din("invf", [128, 32], F32)
    kaug_d = P.din("kaug", [2, 2 * 128], BF16)
    qaug_d = P.din("qaug", [2, 2 * 512], BF16)
    iota128_d = P.din("iota128", [128, 128], F32)
    iota16_d = P.din("iota16", [128, 16], F32)
    gcols_d = P.din("gcols", [128, NGC], F32)
    kvarow_d = P.din("kvarow", [1, 128], F32)
    sinks_d = P.din("sinks", [1, 8], F32)
    gfin_d = P.din("gfin", [1, D], F32)
    w_kvl_d = P.din("w_kvl", [D, 192], F32)
    w_in_d = P.din("w_in", [D, 1216], F32)
    w_qb_d = P.din("w_qb", [256, 768], F32)
    w_ukT_d = P.din("w_ukT", [128, 512], F32)
    w_uv_d = P.din("w_uv", [128, 512], F32)
    w_out_d = P.din("w_out", [D, D], F32)
    w_cq_d = P.din("w_cq", [D, 512], F32)
    w_ck_d = P.din("w_ck", [D, 512], F32)
    w_cv_d = P.din("w_cv", [D, 512], F32)
    w_co_d = P.din("w_co", [512, D], F32)
    w_pq_d = P.din("w_pq", [D, 2048], F32)
    keysT_d = P.din("keysT", [128, 2048], F32)
    uT_d = P.din("uT", [D, NEXP], F32)
    v_d = P.din("v", [NEXP, D], F32)

    kc_s = P.dscr("kc_s", [16, 128, 1024], BF16)
    kr_s = P.dscr("kr_s", [16, 64, 1024], BF16)
    vc_s = P.dscr("vc_s", [16, 128, 8 * 129], BF16)
    x2_s = P.dscr("x2_s", [NOWN * 128, D], F32)
    us_s = P.dscr("us_s", [32, 128, 4096], BF16)
    vs_s = P.dscr("vs_s", [32, 128, 4096], BF16)
    out_d = P.dout("out", [NOWN * 128, D], F32)
    dbg_d = P.dout("dbg", [128, 4096], F32) if dbg else None

    ps_tp = k.ps("ps_tp", [128, 1024], BF16)
    pb = [k.ps("ps_b%d" % i, [128, 512], F32) for i in range(7)]

    identb = k.sb("identb", [128, 128], BF16)
    identf = k.sb("identf", [128, 128], F32)
    invf = k.sb("invf", [128, 32], F32)
    gcols = k.sb("gcols", [128, NGC], F32)
    k.dma(identb.t[:], identb_d, identb, w=[identb])
    k.dma(identf.t[:], identf_d, identf, w=[identf])
    k.dma(invf.t[:], invf_d, invf, w=[invf])
    k.dma(gcols.t[:], gcols_d, gcols, w=[gcols])

    scal_t = k.sb("scal", [128, 64], F32)
    scal_s = [T("scal%d" % i) for i in range(64)]
    scal_i = [0]

    def new_scal():
        i = scal_i[0] % 64
        scal_i[0] += 1
        return scal_t.t[:, i:i + 1], scal_s[i]

    junk = k.sb("junk", [128, 1024], BF16)
    xring = k.ring("xt", [128, D], F32, 3)
    hbring = k.ring("hb", [128, D], BF16, 2)
    hTring = k.ring("hT", [128, 8, 128], BF16, 2)
    ropet = k.ring("ropet", [128, 2, 8, 32], F32, 2)
    stage = k.ring("stage", [128, 2048], F32, 2)

    def rstd_of(x_ap, F, x_tiles, eng_sq="dve"):
        ss, ss_s = new_scal()
        k.op("dve", lambda e: e.scalar_tensor_tensor(out=junk.t[:, 0:F], in0=x_ap, scalar=1.0, in1=x_ap,
                                                     op0=ALU.mult, op1=ALU.mult, accum_out=ss),
             r=x_tiles, w=[junk, ss_s])
        ln, ln_s = new_scal()
        k.act(ln, ss, AF.Ln, r=[ss_s], w=[ln_s], scale=1.0 / F, bias=EPS)
        rs, rs_s = new_scal()
        k.act(rs, ln, AF.Exp, r=[ln_s], w=[rs_s], scale=-0.5)
        return rs, rs_s

    def load_cast(dst, src_d, KC, N, gcol0=None, eng_cycle=("dve", "pool")):
        src_v = src_d.rearrange("(c p) n -> p c n", p=128)
        per = max(1, 2048 // N)
        ei = 0
        for c0 in range(0, KC, per):
            c1 = min(KC, c0 + per)
            sg = stage.next()
            k.dma(sg.t[:, 0:(c1 - c0) * N].rearrange("p (c n) -> p c n", n=N), src_v[:, c0:c1, :], sg, w=[sg])
            for c in range(c0, c1):
                eng = eng_cycle[ei % len(eng_cycle)]
                ei += 1
                src_ap = sg.t[:, (c - c0) * N:(c - c0 + 1) * N]
                dst_ap = dst.t[:, c, :]
                if gcol0 is None:
                    k.op(eng, lambda e, o=dst_ap, i=src_ap: e.tensor_copy(out=o, in_=i), r=[sg], w=[dst])
                else:
                    gc = gcols.t[:, gcol0 + c:gcol0 + c + 1]
                    k.op(eng, lambda e, o=dst_ap, i=src_ap, g=gc: e.tensor_scalar(out=o, in0=i, scalar1=g, scalar2=1.0,
                                                                                 op0=ALU.mult, op1=ALU.mult),
                         r=[sg, gcols], w=[dst])

    def transposes(dst, src, nchunk, width, r, w, evac_eng="act", dst_ap=None, pst=None):
        pst = pst or ps_tp
        for c in range(nchunk):
            k.tr(pst.t[0:width, c * 128:(c + 1) * 128], src[:, c * width:(c + 1) * width], identb.t[:],
                 r=r + [identb], w=[pst])
        o = dst_ap if dst_ap is not None else dst.t[0:width, 0:nchunk, :]
        i = pst.t[0:width, 0:nchunk * 128].rearrange("p (c t) -> p c t", t=128)
        if evac_eng == "act":
            k.op("act", lambda e: e.copy(out=o, in_=i), r=[pst], w=w)
        else:
            k.op(evac_eng, lambda e: e.tensor_copy(out=o, in_=i), r=[pst], w=w)

    k.push_scope()
    castring = [None]
    P0Q = cfg.get("p0q", "sp")

    def p0_gen():
        uT_v = uT_d.rearrange("(c p) e -> p c e", p=128)
        v_v = v_d.rearrange("(g c p) d -> g p c d", p=128, c=4)
        engs3 = ("pool", "pool", "pool")
        ei = 0
        for g in range(n_eg):
            for hf in range(2):
                sg = stage.next()
                k.dma(sg.t[:].rearrange("p (c e) -> p c e", e=512), uT_v[:, hf * 4:(hf + 1) * 4, g * 512:(g + 1) * 512], sg, w=[sg], q=P0Q)
                cb = castring[0].next()
                for cc in range(4):
                    c = hf * 4 + cc
                    eng = engs3[ei % 3]
                    ei += 1
                    o = cb.t[:, cc * 512:(cc + 1) * 512]
                    i = sg.t[:, cc * 512:(cc + 1) * 512]
                    gc = gcols.t[:, G_FFN + c:G_FFN + c + 1]
                    if eng == "act":
                        k.op("act", lambda e, o=o, i=i, gc=gc: e.activation(o, i, AF.Copy, scale=gc), r=[sg, gcols], w=[cb])
                    else:
                        k.op(eng, lambda e, o=o, i=i, gc=gc: e.tensor_scalar(out=o, in0=i, scalar1=gc, scalar2=1.0,
                                                                             op0=ALU.mult, op1=ALU.mult),
                             r=[sg, gcols], w=[cb])
                k.dma(us_s[g][:, hf * 2048:(hf + 1) * 2048], cb.t[:], cb, r=[cb], q=P0Q)
                yield
            for hf in range(2):
                sg = stage.next()
                k.dma(sg.t[:].rearrange("p (c d) -> p c d", d=1024), v_v[g][:, hf * 2:(hf + 1) * 2, :], sg, w=[sg], q=P0Q)
                cb = castring[0].next()
                for cc in range(2):
                    eng = engs3[ei % 3]
                    ei += 1
                    o = cb.t[:, cc * 1024:(cc + 1) * 1024]
                    i = sg.t[:, cc * 1024:(cc + 1) * 1024]
                    if eng == "act":
                        k.op("act", lambda e, o=o, i=i: e.copy(out=o, in_=i), r=[sg], w=[cb])
                    else:
                        k.op(eng, lambda e, o=o, i=i: e.tensor_copy(out=o, in_=i), r=[sg], w=[cb])
                k.dma(vs_s[g][:, hf * 2048:(hf + 1) * 2048], cb.t[:], cb, r=[cb], q=P0Q)
                yield

    castring[0] = k.ring("castA", [128, 2048], BF16, 2)
    p0g = p0_gen() if do_p0 else None

    wkvl = k.sb("wkvl", [128, 8, 192], BF16)
    load_cast(wkvl, w_kvl_d, 8, 192, G_MIX)
    posb_i = k.sb("posb_i", [128, NBLK], I32)
    posb_f = k.sb("posb_f", [128, NBLK], F32)
    k.dma(posb_i.t[:], posb, posb_i, w=[posb_i])
    k.op("dve", lambda e: e.tensor_copy(out=posb_f.t[:], in_=posb_i.t[:]), r=[posb_i], w=[posb_f])

    kvsring = k.ring("kvs", [128, 192], F32, 4)
    krb_ring = k.ring("krb", [128, 64], BF16, 3)
    kcst = k.ring("kcst", [128, 1024], BF16, 2)
    krst = k.ring("krst", [64, 1024], BF16, 2)
    vcst = k.ring("vcst", [128, 8, 129], BF16, 2)

    def rope_tables_bulk(name, pos_f, nb, tab=None):
        if tab is None:
            tab = k.sb(name, [128, 2, nb, 32], F32)
        wk = k.sb(name + "_w", [128, 3, nb, 32], F32)
        ang = wk.t[:, 0]
        k.op("dve", lambda e: e.tensor_tensor(out=ang, in0=pos_f.t[:].unsqueeze(2).to_broadcast([128, nb, 32]),
                                              in1=invf.t[:].unsqueeze(1).to_broadcast([128, nb, 32]), op=ALU.mult),
             r=[pos_f, invf], w=[wk])
        for which in (0, 1):
            a2 = wk.t[:, 1]
            nn = wk.t[:, 2]
            off = 0.0 if which == 0 else float(np.pi / 2)
            k.op("dve", lambda e, off=off: e.tensor_scalar(out=a2, in0=ang, scalar1=off, scalar2=None, op0=ALU.add), r=[wk], w=[wk])
            k.op("dve", lambda e: e.tensor_scalar(out=nn, in0=a2, scalar1=float(1.0 / (2 * np.pi)), scalar2=MAGIC,
                                                  op0=ALU.mult, op1=ALU.add), r=[wk], w=[wk])
            k.op("dve", lambda e: e.tensor_scalar(out=nn, in0=nn, scalar1=-MAGIC, scalar2=None, op0=ALU.add), r=[wk], w=[wk])
            k.op("dve", lambda e: e.scalar_tensor_tensor(out=a2, in0=nn, scalar=-C1, in1=a2, op0=ALU.mult, op1=ALU.add), r=[wk], w=[wk])
            k.op("dve", lambda e: e.scalar_tensor_tensor(out=a2, in0=nn, scalar=-C2, in1=a2, op0=ALU.mult, op1=ALU.add), r=[wk], w=[wk])
            k.op("dve", lambda e: e.tensor_scalar(out=a2, in0=a2, scalar1=-PI_LO, scalar2=PI_LO, op0=ALU.max, op1=ALU.min), r=[wk], w=[wk])
            k.act(tab.t[:, which], a2, AF.Sin, r=[wk], w=[tab])
        return tab

    def apply_rope(out_bf, x_ap, nh, sin_ap, cos_ap, tg, x_tiles, out_tiles):
        rt = ropet.next()
        xv = x_ap.rearrange("p (h two j) -> p h two j", two=2, j=32)
        cosb = cos_ap.unsqueeze(1).unsqueeze(1).to_broadcast([128, nh, 2, 32])
        sinb = sin_ap.unsqueeze(1).unsqueeze(1).to_broadcast([128, nh, 2, 32])
        xc = rt.t[:, 0, 0:2 * nh, :].rearrange("p (h two) j -> p h two j", two=2)
        xs = rt.t[:, 1, 0:2 * nh, :].rearrange("p (h two) j -> p h two j", two=2)
        k.op("dve", lambda e: e.tensor_tensor(out=xc, in0=xv, in1=cosb, op=ALU.mult), r=x_tiles + [tg], w=[rt])
        k.op("dve", lambda e: e.tensor_tensor(out=xs, in0=xv, in1=sinb, op=ALU.mult), r=x_tiles + [tg], w=[rt])
        ov = out_bf.rearrange("p (h two j) -> p h two j", two=2, j=32)
        k.op("dve", lambda e: e.tensor_tensor(out=ov[:, :, 0, :], in0=xc[:, :, 0, :], in1=xs[:, :, 1, :], op=ALU.subtract),
             r=[rt], w=out_tiles)
        k.op("dve", lambda e: e.tensor_tensor(out=ov[:, :, 1, :], in0=xc[:, :, 1, :], in1=xs[:, :, 0, :], op=ALU.add),
             r=[rt], w=out_tiles)

    def norm_T(x_ap, x_tiles, hbr=None, hTr=None, evac_eng="act"):
        hbr = hbr or hbring
        hTr = hTr or hTring
        rs, rs_s = rstd_of(x_ap, D, x_tiles)
        hb = hbr.next()
        k.op("dve", lambda e: e.tensor_scalar(out=hb.t[:], in0=x_ap, scalar1=rs, scalar2=None, op0=ALU.mult),
             r=x_tiles + [rs_s], w=[hb])
        hT = hTr.next()
        transposes(hT, hb.t[:], 8, 128, [hb], [hT], evac_eng=evac_eng)
        return hT

    tabb = rope_tables_bulk("tabb", posb_f, NBLK)
    if do_p1a:
        sbufs = {}
        kvs_of = {}
        hbk_r = k.ring("hbk", [128, D], BF16, 3)
        hTk_r = k.ring("hTk", [128, 8, 128], BF16, 3)

        xk6 = k.ring("xk6", [128, D], F32, 6)
        sA = {}

        def stageA1(kb):
            sbi, j = kb // 8, kb % 8
            if j == 0:
                kcs, krs, vcs = kcst.next(), krst.next(), vcst.next()
                k.op("pool", lambda e: e.memset(vcs.t[:, :, 128:129], 1.0), w=[vcs])
                sbufs[sbi] = (kcs, krs, vcs)
            xt = xk6.next()
            k.dma(xt.t[:], xb[kb * 128:(kb + 1) * 128, :], xt, w=[xt], q="act")
            rs, rs_s = rstd_of(xt.t[:], D, [xt])
            sA[kb] = (xt, rs, rs_s)

        def stageA2(kb):
            xt, rs, rs_s = sA.pop(kb)
            hb = hbk_r.next()
            k.op("dve", lambda e: e.tensor_scalar(out=hb.t[:], in0=xt.t[:], scalar1=rs, scalar2=None, op0=ALU.mult),
                 r=[xt, rs_s], w=[hb])
            hT = hTk_r.next()
            transposes(hT, hb.t[:], 8, 128, [hb], [hT])
            pk = pb[3 + kb % 3]
            for c in range(8):
                k.mm(pk.t[:, 0:192], hT.t[:, c, :], wkvl.t[:, c, :], c == 0, c == 7, r=[hT, wkvl], w=[pk])
            kvs = kvsring.next()
            k.op("act", lambda e: e.copy(out=kvs.t[:], in_=pk.t[:, 0:192]), r=[pk], w=[kvs])
            kvs_of[kb] = kvs

        sB = {}

        def stageB1(kb):
            kvs = kvs_of[kb]
            rs, rs_s = rstd_of(kvs.t[:, 0:128], 128, [kvs])
            krb = krb_ring.next()
            apply_rope(krb.t[:], kvs.t[:, 128:192], 1, tabb.t[:, 0, kb, :], tabb.t[:, 1, kb, :], tabb, [kvs], [krb])
            sB[kb] = (rs, rs_s, krb)

        def stageB2(kb):
            sbi, j = kb // 8, kb % 8
            kcs, krs, vcs = sbufs[sbi]
            kvs = kvs_of.pop(kb)
            rs, rs_s, krb = sB.pop(kb)
            k.op("dve", lambda e: e.tensor_scalar(out=vcs.t[:, j, 0:128], in0=kvs.t[:, 0:128],
                                                  scalar1=rs, scalar2=None, op0=ALU.mult), r=[kvs, rs_s], w=[vcs])
            pt = pb[2]
            ptb = pt.t[:].bitcast(BF16)
            k.tr(ptb[:, 0:128], vcs.t[:, j, 0:128], identb.t[:], r=[vcs, identb], w=[pt])
            k.tr(ptb[0:64, 128:256], krb.t[:], identb.t[:], r=[krb, identb], w=[pt])
            k.op("act", lambda e: e.copy(out=kcs.t[:, j * 128:(j + 1) * 128], in_=ptb[:, 0:128]), r=[pt], w=[kcs])
            k.op("act", lambda e: e.copy(out=krs.t[:, j * 128:(j + 1) * 128], in_=ptb[0:64, 128:256]), r=[pt], w=[krs])
            if j == 7:
                k.dma(kc_s[sbi], kcs.t[:], kcs, r=[kcs])
                k.dma(kr_s[sbi], krs.t[:], krs, r=[krs])
                k.dma(vc_s[sbi], vcs.t[:].rearrange("p a b -> p (a b)"), vcs, r=[vcs])

        nkb = n_sb * 8
        for t_ in range(nkb + 3):
            if t_ < nkb:
                stageA1(t_)
            if 0 <= t_ - 1 < nkb:
                stageA2(t_ - 1)
            if 0 <= t_ - 2 < nkb:
                stageB1(t_ - 2)
            if 0 <= t_ - 3 < nkb:
                stageB2(t_ - 3)
                if p0g is not None and (t_ % 2 == 0 or not do_p1b):
                    next(p0g, None)
        kcs, krs, vcs = sbufs[n_sb - 1]
    if p0g is not None and not do_p1b:
        for _ in p0g:
            pass

    if dbg == "p1a":
        dt_ = k.sb("dbgt", [128, 4096], F32)
        k.op("dve", lambda e: e.memset(dt_.t[:], 0.0), w=[dt_])
        k.op("dve", lambda e: e.tensor_copy(out=dt_.t[:, 0:1024], in_=kcs.t[:]), r=[kcs], w=[dt_])
        k.op("dve", lambda e: e.tensor_copy(out=dt_.t[0:64, 1024:2048], in_=krs.t[:]), r=[krs], w=[dt_])
        k.op("dve", lambda e: e.tensor_copy(out=dt_.t[:, 2048:2048 + 1032], in_=vcs.t[:].rearrange("p a b -> p (a b)")),
             r=[vcs], w=[dt_])
        k.dma(dbg_d, dt_.t[:], dt_, r=[dt_])


    k.pop_scope()
    k.push_scope()
    if do_p1b:
        castring[0] = k.ring("castB", [128, 2048], BF16, 2)
        win = k.sb("win", [128, 8, 1216], BF16)
        load_cast(win, w_in_d, 8, 1216, G_MIX)
        wqb = k.sb("wqb", [128, 2, 768], BF16)
        load_cast(wqb, w_qb_d, 2, 768, G_QA)
        wout = k.sb("wout", [128, 8, 1024], BF16)
        load_cast(wout, w_out_d, 8, 1024, G_OUT)
        wcq = k.sb("wcq", [128, 8, 512], BF16)
        load_cast(wcq, w_cq_d, 8, 512, G_CROSS)
        wco = k.sb("wco", [128, 4, 1024], BF16)
        load_cast(wco, w_co_d, 4, 1024, None)
        lvl = cfg.get("lvl", 99)
        if lvl < 1:
            raise StopBuild(P)
        kvab = k.sb("kvab", [128, 128], F32)
        k.dma(kvab.t[:], kvarow_d[0].partition_broadcast(128), kvab, w=[kvab])
        wukT = k.sb("wukT", [128, 4, 128], BF16)
        sg = stage.next()
        k.dma(sg.t[:, 0:512], w_ukT_d, sg, w=[sg])
        k.op("dve", lambda e, sg=sg: e.tensor_tensor(out=wukT.t[:], in0=sg.t[:, 0:512].rearrange("p (h r) -> p h r", r=128),
                                                     in1=kvab.t[:].unsqueeze(1).to_broadcast([128, 4, 128]), op=ALU.mult),
             r=[sg, kvab], w=[wukT])
        wuv = k.sb("wuv", [128, 4, 128], BF16)
        sg = stage.next()
        k.dma(sg.t[:, 0:512], w_uv_d, sg, w=[sg])
        k.op("dve", lambda e, sg=sg: e.tensor_scalar(out=wuv.t[:].rearrange("p h d -> p (h d)"), in0=sg.t[:, 0:512],
                                                     scalar1=gcols.t[:, G_KVA:G_KVA + 1], scalar2=None, op0=ALU.mult),
             r=[sg, gcols], w=[wuv])
        if lvl < 2:
            raise StopBuild(P)
        esink = k.sb("esink", [128, 8], F32)
        k.dma(esink.t[:], sinks_d[0].partition_broadcast(128), esink, w=[esink])
        k.act(esink.t[:], esink.t[:], AF.Exp, r=[esink], w=[esink])
        if lvl < 3:
            raise StopBuild(P)
        kaug = k.sb("kaug", [2, 2, 128], BF16)
        qaug = k.sb("qaug", [2, 2, 512], BF16)
        k.dma(kaug.t[:].rearrange("p a b -> p (a b)"), kaug_d, kaug, w=[kaug])
        k.dma(qaug.t[:].rearrange("p a b -> p (a b)"), qaug_d, qaug, w=[qaug])
        poso_i = k.sb("poso_i", [128, NOWN], I32)
        poso_f = k.sb("poso_f", [128, NOWN], F32)
        k.dma(poso_i.t[:], poso, poso_i, w=[poso_i])
        k.op("dve", lambda e: e.tensor_copy(out=poso_f.t[:], in_=poso_i.t[:]), r=[poso_i], w=[poso_f])
        tabo = k.sb("tabo", [128, 2, NOWN, 32], F32)

        if lvl < 4:
            raise StopBuild(P)
        KxT = k.sb("KxT", [128, 4, 256], BF16)
        Vx = k.sb("Vx", [128, 2, 4, 129], BF16)
        k.op("pool", lambda e: e.memset(Vx.t[:, :, :, 128:129], 1.0), w=[Vx])
        main_scope = k.st
        k.st = ExitStack()
        rope_tables_bulk("tabo", poso_f, NOWN, tab=tabo)
        memT = k.sb("memT", [128, 8, 256], BF16)
        wck = k.sb("wck", [128, 8, 512], BF16)
        wcv = k.sb("wcv", [128, 8, 512], BF16)
        load_cast(wck, w_ck_d, 8, 512, G_MEM)
        load_cast(wcv, w_cv_d, 8, 512, G_MEM)
        for mc in range(2):
            xt = xring.next()
            k.dma(xt.t[:], memb[mc * 128:(mc + 1) * 128, :], xt, w=[xt])
            rs, rs_s = rstd_of(xt.t[:], D, [xt])
            hb = hbring.next()
            k.op("dve", lambda e, hb=hb, xt=xt, rs=rs: e.tensor_scalar(out=hb.t[:], in0=xt.t[:], scalar1=rs, scalar2=None,
                                                                       op0=ALU.mult), r=[xt, rs_s], w=[hb])
            transposes(None, hb.t[:], 8, 128, [hb], [memT], dst_ap=memT.t[:, :, mc * 128:(mc + 1) * 128])
        for h in range(4):
            for c in range(8):
                k.mm(pb[0].t[:, 0:256], wck.t[:, c, h * 128:(h + 1) * 128], memT.t[:, c, :], c == 0, c == 7,
                     r=[wck, memT], w=[pb[0]])
            k.op("act", lambda e, h=h: e.copy(out=KxT.t[:, h, :], in_=pb[0].t[:, 0:256]), r=[pb[0]], w=[KxT])
        for mc in range(2):
            for c in range(8):
                k.mm(pb[1].t[:, 0:512], memT.t[:, c, mc * 128:(mc + 1) * 128], wcv.t[:, c, :], c == 0, c == 7,
                     r=[wcv, memT], w=[pb[1]])
            k.op("act", lambda e, mc=mc: e.copy(out=Vx.t[:, mc, :, 0:128],
                                                in_=pb[1].t[:, 0:512].rearrange("p (h d) -> p h d", d=128)),
                 r=[pb[1]], w=[Vx])

        k.S.barrier()
        k.st.close()
        k.st = main_scope
        if lvl < 5:
            raise StopBuild(P)
        prj = k.ring("prj", [128, 1216], F32, 1)
        qsb_r = k.ring("qsb", [128, 512], BF16, 2)
        QsT_r = k.ring("QsT", [64, 8, 128], BF16, 2)
        KsT_r = k.ring("KsT", [64, 2, 2, 128], BF16, 2)
        Vs_r = k.ring("Vs", [128, 2, 2, 65], BF16, 2)
        ksb_r = k.ring("ksb", [128, 2, 256], BF16, 2)
        cqn_r = k.ring("cqn", [128, 256], BF16, 2)
        cqnT_r = k.ring("cqnT", [128, 2, 128], BF16, 2)
        qnT_r = k.ring("qnT", [128, 4, 128], BF16, 2)
        QT_r = k.ring("QT", [128, 512], BF16, 2)
        qrb_r = k.ring("qrb", [128, 256], BF16, 2)
        QrT_r = k.ring("QrT", [128, 4, 128], BF16, 2)
        PT_r = k.ring("PT", [128, 512], BF16, 3)
        PS_r = k.ring("PS", [128, 512], BF16, 3)
        mk_r = k.ring("mk", [128, 4, 128], BF16, 2)
        smk_r = k.ring("smk", [128, 2, 128], BF16, 2)
        kcc_r = k.ring("kcc", [128, 1024], BF16, 3)
        krc_r = k.ring("krc", [128, 1024], BF16, 3)
        vcc_r = k.ring("vcc", [128, 8, 129], BF16, 3)
        den_r = k.ring("den", [128, 8], F32, 4)
        olat_r = k.ring("olat", [128, 512], BF16, 2)
        olatT_r = k.ring("olatT", [128, 4, 128], BF16, 2)
        of32_r = k.ring("of32", [128, 512], F32, 2)
        mix_r = k.ring("mix", [128, 1024], BF16, 2)
        x1_r = k.ring("x1", [128, D], F32, 2)
        QxT_r = k.ring("QxT", [128, 4, 128], BF16, 2)
        for bq in QrT_r.bufs:
            k.op("pool", lambda e, bq=bq: e.memset(bq.t[64:128, :, :], 0.0), w=[bq])
        for bq in krc_r.bufs:
            k.op("pool", lambda e, bq=bq: e.memset(bq.t[64:128, :], 0.0), w=[bq])
        xo_r = Ring(xring.bufs[0:2])
        xp_r = Ring([xring.bufs[2], k.sb("xp1", [128, D], F32)])

        def norm_bank(pbuf, nhb, dv, extra_ap, extra_tiles, out_ap, out_tiles):
            dn = den_r.next()
            pv = pbuf.t[:, 0:nhb * (dv + 1)].rearrange("p (h d) -> p h d", d=dv + 1)
            den_src = pv[:, :, dv:dv + 1].rearrange("p h o -> p (h o)")
            if extra_ap is not None:
                k.op("dve", lambda e: e.tensor_tensor(out=dn.t[:, 0:nhb], in0=den_src, in1=extra_ap, op=ALU.add),
                     r=[pbuf] + extra_tiles, w=[dn])
            else:
                k.op("dve", lambda e: e.tensor_copy(out=dn.t[:, 0:nhb], in_=den_src), r=[pbuf], w=[dn])
            k.op("dve", lambda e: e.reciprocal(out=dn.t[:, 0:nhb], in_=dn.t[:, 0:nhb]), r=[dn], w=[dn])
            for hh in range(nhb):
                k.op("dve", lambda e, hh=hh: e.tensor_scalar(out=out_ap[:, hh * dv:(hh + 1) * dv], in0=pv[:, hh, 0:dv],
                                                            scalar1=dn.t[:, hh:hh + 1], scalar2=None, op0=ALU.mult),
                     r=[pbuf, dn], w=out_tiles)

        SC_MLA = float(192.0 ** -0.5)
        SC_SWA = 0.125
        SC_X = float(128.0 ** -0.5)
        st_ = {}

        def pre(ki):
            S = {}
            st_[ki] = S
            xt = xo_r.next()
            S["xt"] = xt
            k.dma(xt.t[:], xown[ki * 128:(ki + 1) * 128, :], xt, w=[xt], q="act")
            hT = norm_T(xt.t[:], [xt], evac_eng="dve")
            yield
            pj = prj.next()
            for (c0, c1, pbk) in ((0, 512, pb[0]), (512, 1024, pb[1]), (1024, 1216, pb[2])):
                for c in range(8):
                    k.mm(pbk.t[:, 0:c1 - c0], hT.t[:, c, :], win.t[:, c, c0:c1], c == 0, c == 7, r=[hT, win], w=[pbk])
                k.op("dve", lambda e, pbk=pbk, c0=c0, c1=c1: e.tensor_copy(out=pj.t[:, c0:c1], in_=pbk.t[:, 0:c1 - c0]),
                     r=[pbk], w=[pj])
                yield
            xp = xp_r.next()
            k.dma(xp.t[:], xprev[ki * 128:(ki + 1) * 128, :], xp, w=[xp], q="act")
            hTp = norm_T(xp.t[:], [xp], evac_eng="dve")
            yield
            for c in range(8):
                k.mm(pb[2].t[:, 0:256], hTp.t[:, c, :], win.t[:, c, 960:1216], c == 0, c == 7, r=[hTp, win], w=[pb[2]])
            ksb = ksb_r.next()
            k.op("dve", lambda e: e.tensor_copy(out=ksb.t[:, 0, :], in_=pb[2].t[:, 0:256]), r=[pb[2]], w=[ksb])
            k.op("dve", lambda e: e.tensor_copy(out=ksb.t[:, 1, :], in_=pj.t[:, 960:1216]), r=[pj], w=[ksb])
            yield
            KsT = KsT_r.next()
            Vs = Vs_r.next()
            k.op("dve", lambda e: e.memset(Vs.t[:, :, :, 64:65], 1.0), w=[Vs])
            for slot in range(2):
                for kvh in range(2):
                    k.tr(ps_tp.t[0:64, (slot * 2 + kvh) * 128:(slot * 2 + kvh + 1) * 128], ksb.t[:, slot, kvh * 64:(kvh + 1) * 64],
                         identb.t[:], r=[ksb, identb], w=[ps_tp])
                k.op("dve", lambda e, slot=slot: e.tensor_copy(
                    out=Vs.t[:, slot, :, 0:64], in_=ksb.t[:, slot, 128:256].rearrange("p (h d) -> p h d", d=64)),
                    r=[ksb], w=[Vs])
            k.op("dve", lambda e: e.tensor_copy(out=KsT.t[:].rearrange("p a b t -> p (a b t)"), in_=ps_tp.t[0:64, 0:512]),
                 r=[ps_tp], w=[KsT])
            yield
            qsb = qsb_r.next()
            k.op("dve", lambda e: e.tensor_copy(out=qsb.t[:], in_=pj.t[:, 448:960]), r=[pj], w=[qsb])
            QsT = QsT_r.next()
            transposes(QsT, qsb.t[:], 8, 64, [qsb], [QsT], evac_eng="dve")
            smk = smk_r.next()
            k.dma(smk.t[:].rearrange("p a b -> p (a b)"), smask_d[ki], smk, w=[smk], q="act")
            yield
            ob = of32_r.next()
            for kvh in range(2):
                for slot in range(2):
                    psc = pb[slot % 2]
                    k.mm(psc.t[:], KsT.t[:, slot, kvh, :], QsT.t[:, kvh * 4:(kvh + 1) * 4, :].rearrange("p g t -> p (g t)"),
                         True, False, r=[KsT, QsT], w=[psc])
                    k.mm(psc.t[:], kaug.t[:, slot, :], qaug.t[:, kvh, :], False, True, r=[kaug, qaug], w=[psc])
                    PT = PS_r.next()
                    k.act(PT.t[:], psc.t[:], AF.Exp, r=[psc], w=[PT], scale=SC_SWA)
                    k.op("dve", lambda e, PT=PT, slot=slot: e.tensor_tensor(
                        out=PT.t[:].rearrange("p (g t) -> p g t", t=128), in0=PT.t[:].rearrange("p (g t) -> p g t", t=128),
                        in1=smk.t[:, slot, :].unsqueeze(1).to_broadcast([128, 4, 128]), op=ALU.mult), r=[PT, smk], w=[PT])
                    yield
                    for g in range(4):
                        k.mm(pb[2].t[:, g * 65:(g + 1) * 65], PT.t[:, g * 128:(g + 1) * 128], Vs.t[:, slot, kvh, :],
                             slot == 0 and g == 0, slot == 1, r=[PT, Vs], w=[pb[2]], skip=True)
                    yield
                norm_bank(pb[2], 4, 64, esink.t[:, kvh * 4:(kvh + 1) * 4], [esink], ob.t[:, kvh * 256:(kvh + 1) * 256], [ob])
                yield
            mix = mix_r.next()
            S["mix"] = mix
            rs, rs_s = rstd_of(ob.t[:], 512, [ob])
            k.op("dve", lambda e: e.tensor_scalar(out=mix.t[:, 512:1024], in0=ob.t[:], scalar1=rs,
                                                  scalar2=None, op0=ALU.mult), r=[ob, rs_s], w=[mix])
            yield
            rs2, rs2_s = rstd_of(pj.t[:, 0:256], 256, [pj])
            cqn = cqn_r.next()
            k.op("dve", lambda e: e.tensor_scalar(out=cqn.t[:], in0=pj.t[:, 0:256], scalar1=rs2,
                                                  scalar2=None, op0=ALU.mult), r=[pj, rs2_s], w=[cqn])
            cqnT = cqnT_r.next()
            transposes(cqnT, cqn.t[:], 2, 128, [cqn], [cqnT], evac_eng="dve")
            yield
            for h in range(4):
                for c in range(2):
                    k.mm(pb[0].t[:, h * 128:(h + 1) * 128], wqb.t[:, c, h * 128:(h + 1) * 128], cqnT.t[:, c, :], c == 0, c == 1,
                         r=[wqb, cqnT], w=[pb[0]])
            qnT = qnT_r.next()
            k.op("dve", lambda e: e.tensor_copy(out=qnT.t[:].rearrange("p h t -> p (h t)"), in_=pb[0].t[:]), r=[pb[0]], w=[qnT])
            yield
            for h in range(4):
                k.mm(pb[1].t[:, h * 128:(h + 1) * 128], wukT.t[:, h, :], qnT.t[:, h, :], True, True, r=[wukT, qnT], w=[pb[1]])
            QT = QT_r.next()
            S["QT"] = QT
            k.op("dve", lambda e: e.tensor_copy(out=QT.t[:], in_=pb[1].t[:]), r=[pb[1]], w=[QT])
            yield
            for c in range(2):
                k.mm(pb[2].t[:, 0:256], cqnT.t[:, c, :], wqb.t[:, c, 512:768], c == 0, c == 1, r=[wqb, cqnT], w=[pb[2]])
            qrb = qrb_r.next()
            apply_rope(qrb.t[:], pb[2].t[:, 0:256], 4, tabo.t[:, 0, ki, :], tabo.t[:, 1, ki, :], tabo, [pb[2]], [qrb])
            yield
            QrT = QrT_r.next()
            S["QrT"] = QrT
            transposes(QrT, qrb.t[:], 4, 64, [qrb], [QrT], evac_eng="dve")
            mk = mk_r.next()
            S["mk"] = mk
            k.dma(mk.t[:].rearrange("p a b -> p (a b)"), mmask_d[ki], mk, w=[mk], q="act")
            yield

        def mla(ki, gens):
            S = st_[ki]
            QT, QrT, mk = S["QT"], S["QrT"], S["mk"]
            L = n_keyblocks(ki)
            nch = (L + 7) // 8
            chunks = []
            po = [pb[5], pb[6]]

            def load_chunk(ci):
                kcc, krc, vcc = kcc_r.next(), krc_r.next(), vcc_r.next()
                nb = min(8, L - ci * 8)
                KQ = cfg.get("kvq", "act")
                k.dma(kcc.t[:, 0:nb * 128], kc_s[ci, :, 0:nb * 128], kcc, w=[kcc], q=KQ)
                k.dma(krc.t[0:64, 0:nb * 128], kr_s[ci, :, 0:nb * 128], krc, w=[krc], q=KQ)
                k.dma(vcc.t[:, 0:nb, :].rearrange("p a b -> p (a b)"), vc_s[ci, :, 0:nb * 129], vcc, w=[vcc], q=KQ)
                return (kcc, krc, vcc)

            def scores(j, psc):
                kcc, krc, vcc = chunks[j // 8]
                jj = j % 8
                k.mm(psc.t[:], kcc.t[:, jj * 128:(jj + 1) * 128], QT.t[:], True, False, r=[kcc, QT], w=[psc])
                k.mm(psc.t[:], krc.t[:, jj * 128:(jj + 1) * 128], QrT.t[:].rearrange("p h t -> p (h t)"), False, True,
                     r=[krc, QrT], w=[psc])

            def step_others():
                for gx in gens:
                    try:
                        next(gx)
                        return
                    except StopIteration:
                        continue

            chunks.append(load_chunk(0))
            if nch > 1:
                chunks.append(load_chunk(1))
            scores(0, pb[3])
            for j in range(L):
                if j + 1 < L:
                    if (j + 1) % 8 == 0 and (j + 1) // 8 + 1 < nch:
                        chunks.append(load_chunk((j + 1) // 8 + 1))
                    scores(j + 1, pb[3 + ((j + 1) % 2)])
                psc = pb[3 + (j % 2)]
                PT = PT_r.next()
                k.act(PT.t[:], psc.t[:], AF.Exp, r=[psc], w=[PT], scale=SC_MLA)
                if j >= L - 4:
                    jj = j - (L - 4)
                    k.op("dve", lambda e, PT=PT, jj=jj: e.tensor_tensor(
                        out=PT.t[:].rearrange("p (g t) -> p g t", t=128), in0=PT.t[:].rearrange("p (g t) -> p g t", t=128),
                        in1=mk.t[:, jj, :].unsqueeze(1).to_broadcast([128, 4, 128]), op=ALU.mult), r=[PT, mk], w=[PT])
                vcc = chunks[j // 8][2]
                for h in range(4):
                    pbo = po[h // 2]
                    hh = h % 2
                    k.mm(pbo.t[:, hh * 129:(hh + 1) * 129], PT.t[:, h * 128:(h + 1) * 128], vcc.t[:, j % 8, :],
                         j == 0 and hh == 0, j == L - 1, r=[PT, vcc], w=[pbo], skip=True)
                step_others()
            olat = olat_r.next()
            S["olat"] = olat
            norm_bank(pb[5], 2, 128, None, [], olat.t[:, 0:256], [olat])
            norm_bank(pb[6], 2, 128, None, [], olat.t[:, 256:512], [olat])

        def post(ki):
            S = st_[ki]
            olat, mix, xt = S["olat"], S["mix"], S["xt"]
            olatT = olatT_r.next()
            transposes(olatT, olat.t[:], 4, 128, [olat], [olatT], evac_eng="dve")
            yield
            for h in range(4):
                k.mm(pb[0].t[:, h * 128:(h + 1) * 128], olatT.t[:, h, :], wuv.t[:, h, :], True, True, r=[olatT, wuv], w=[pb[0]])
            oa = of32_r.next()
            k.op("dve", lambda e: e.tensor_copy(out=oa.t[:], in_=pb[0].t[:]), r=[pb[0]], w=[oa])
            yield
            rs, rs_s = rstd_of(oa.t[:], 512, [oa])
            k.op("dve", lambda e: e.tensor_scalar(out=mix.t[:, 0:512], in0=oa.t[:], scalar1=rs,
                                                  scalar2=None, op0=ALU.mult), r=[oa, rs_s], w=[mix])
            mixT = hTring.next()
            transposes(mixT, mix.t[:], 8, 128, [mix], [mixT], evac_eng="dve")
            yield
            x1 = x1_r.next()
            for half in range(2):
                for c in range(8):
                    k.mm(pb[half].t[:], mixT.t[:, c, :], wout.t[:, c, half * 512:(half + 1) * 512], c == 0, c == 7,
                         r=[mixT, wout], w=[pb[half]])
                k.op("dve", lambda e, half=half: e.tensor_tensor(
                    out=x1.t[:, half * 512:(half + 1) * 512], in0=pb[half].t[:], in1=xt.t[:, half * 512:(half + 1) * 512],
                    op=ALU.add), r=[pb[half], xt], w=[x1])
                yield
            if dbg == "p1b" and ki == n_own - 1:
                x1c = k.sb("x1c", [128, D], F32)
                S["x1c"] = x1c
                k.op("pool", lambda e: e.tensor_copy(out=x1c.t[:], in_=x1.t[:]), r=[x1], w=[x1c])
            h2T = norm_T(x1.t[:], [x1], evac_eng="dve")
            yield
            for h in range(4):
                for c in range(8):
                    k.mm(pb[2].t[:, h * 128:(h + 1) * 128], wcq.t[:, c, h * 128:(h + 1) * 128], h2T.t[:, c, :], c == 0, c == 7,
                         r=[wcq, h2T], w=[pb[2]])
                if h % 2 == 1:
                    yield
            QxT = QxT_r.next()
            k.op("dve", lambda e: e.tensor_copy(out=QxT.t[:].rearrange("p h t -> p (h t)"), in_=pb[2].t[:]), r=[pb[2]], w=[QxT])
            yield
            PTs = []
            for mc in range(2):
                psc = pb[mc]
                for h in range(4):
                    k.mm(psc.t[:, h * 128:(h + 1) * 128], KxT.t[:, h, mc * 128:(mc + 1) * 128], QxT.t[:, h, :], True, True,
                         r=[KxT, QxT], w=[psc])
                PT = PS_r.next()
                k.act(PT.t[:], psc.t[:], AF.Exp, r=[psc], w=[PT], scale=SC_X)
                PTs.append(PT)
                yield
            ox = olat_r.next()
            for hp in range(2):
                for mc in range(2):
                    for hh in range(2):
                        h = hp * 2 + hh
                        k.mm(pb[2].t[:, hh * 129:(hh + 1) * 129], PTs[mc].t[:, h * 128:(h + 1) * 128], Vx.t[:, mc, h, :],
                             mc == 0 and hh == 0, mc == 1, r=[PTs[mc], Vx], w=[pb[2]], skip=True)
                norm_bank(pb[2], 2, 128, None, [], ox.t[:, hp * 256:(hp + 1) * 256], [ox])
                yield
            oxT = olatT_r.next()
            transposes(oxT, ox.t[:], 4, 128, [ox], [oxT], evac_eng="dve")
            yield
            x2 = x1
            for half in range(2):
                for c in range(4):
                    k.mm(pb[half].t[:], oxT.t[:, c, :], wco.t[:, c, half * 512:(half + 1) * 512], c == 0, c == 3,
                         r=[oxT, wco], w=[pb[half]])
                k.op("dve", lambda e, half=half: e.tensor_tensor(
                    out=x2.t[:, half * 512:(half + 1) * 512], in0=pb[half].t[:], in1=x1.t[:, half * 512:(half + 1) * 512],
                    op=ALU.add), r=[pb[half], x1], w=[x2])
                yield
            k.dma(x2_s[ki * 128:(ki + 1) * 128, :], x2.t[:], x2, r=[x2])
            if dbg == "p1b" and ki == n_own - 1:
                dt_ = k.sb("dbgt", [128, 1024], F32)
                for (c0, srcb, wd) in ((0, mix, 1024), (1024, S["x1c"], 1024), (2048, x2, 1024), (3072, olat, 512)):
                    k.op("dve", lambda e, srcb=srcb, wd=wd: e.tensor_copy(out=dt_.t[:, 0:wd], in_=srcb.t[:]), r=[srcb], w=[dt_])
                    k.dma(dbg_d[:, c0:c0 + wd], dt_.t[:, 0:wd], dt_, r=[dt_], w=[])
            st_.pop(ki)

        stop = cfg.get("p1b_stop")
        n_run = 0 if stop == "setup" else n_own
        if n_run > 0:
            for _ in pre(0):
                pass
        prev_post = None
        for ki in range(n_run):
            gens = []
            if prev_post is not None:
                gens.append(prev_post)
            nxt = pre(ki + 1) if ki + 1 < n_run else None
            if nxt is not None:
                gens.append(nxt)
            mla(ki, gens + ([p0g] if p0g is not None else []))
            for gx in gens:
                for _ in gx:
                    pass
            prev_post = post(ki)
        if prev_post is not None:
            for _ in prev_post:
                pass
        if p0g is not None:
            for _ in p0g:
                pass

    k.pop_scope()


    k.push_scope()
    if do_p2:
        wq_s = P.dscr("wq_s", [8, 128, 2048], BF16)
        gfin = k.sb("gfin", [128, D], F32)
        k.dma(gfin.t[:], gfin_d[0].partition_broadcast(128), gfin, w=[gfin])
        iota128 = k.sb("iota128", [128, 128], F32)
        iota16 = k.sb("iota16", [128, 16], F32)
        k.dma(iota128.t[:], iota128_d, iota128, w=[iota128])
        k.dma(iota16.t[:], iota16_d, iota16, w=[iota16])
        keysT = k.sb("keysT", [128, 16, 128], BF16)
        sg = stage.next()
        k.dma(sg.t[:], keysT_d, sg, w=[sg])
        k.op("dve", lambda e, sg=sg: e.tensor_copy(out=keysT.t[:].rearrange("p a b -> p (a b)"), in_=sg.t[:]), r=[sg], w=[keysT])
        wq_r = k.ring("wqc", [128, 8, 256], BF16, 2)
        wpq_v = w_pq_d.rearrange("(c p) n -> p c n", p=128)
        for fp in range(8):
            sg = stage.next()
            k.dma(sg.t[:].rearrange("p (c n) -> p c n", n=256), wpq_v[:, :, fp * 256:(fp + 1) * 256], sg, w=[sg])
            wc = wq_r.next()
            for c in range(8):
                eng = ("dve", "pool")[c % 2]
                k.op(eng, lambda e, wc=wc, sg=sg, c=c: e.tensor_scalar(out=wc.t[:, c, :], in0=sg.t[:, c * 256:(c + 1) * 256],
                                                                      scalar1=gcols.t[:, G_FFN + c:G_FFN + c + 1], scalar2=1.0,
                                                                      op0=ALU.mult, op1=ALU.mult), r=[sg, gcols], w=[wc])
            k.dma(wq_s[fp], wc.t[:].rearrange("p c n -> p (c n)"), wc, r=[wc])
        k.S.barrier()

        GT = k.sb("GT", [128, 128, 256], BF16)
        hfT2 = [k.sb("hfT%d" % i, [128, 8, 256], BF16) for i in range(2)]
        trip2 = [k.sb("trip%d" % i, [128, 3, 256], F32) for i in range(2)]
        x2r = Ring(list(xring.bufs) + [k.sb("xt3", [128, D], F32)])
        qT = k.sb("qT", [128, 16, 256], BF16)
        sc = k.sb("sc", [128, 2048], F32)
        sc_s = [T("sc%d" % i) for i in range(16)]
        tops = k.sb("tops", [128, 16, 16], F32)
        tops_s = [T("tops%d" % i) for i in range(16)]
        idxu = k.sb("idxu", [128, 16, 16], U32)
        idxu_s = [T("idxu%d" % i) for i in range(16)]
        idxf = k.sb("idxf", [128, 16, 16], F32)
        big8 = k.sb("big8", [128, 2048], F32)
        cand_s = [T("cand%d" % i) for i in range(8)]
        best = k.sb("best", [128, 8, 16], F32)
        best_s = [T("best%d" % i) for i in range(8)]
        posu = k.sb("posu", [128, 8, 16], U32)
        posu_s = [T("posu%d" % i) for i in range(8)]
        apu = k.sb("apu", [128, 8, 16], U32)
        bpu = k.sb("bpu", [128, 8, 16], U32)
        apf = k.sb("apf", [128, 8, 16], F32)
        bpf = k.sb("bpf", [128, 8, 16], F32)
        sel = k.sb("sel", [128, 3, 128], F32)
        exb = k.sb("exb", [128, 8, 16], F32)
        sm8 = k.sb("sm8", [128, 8], F32)
        Pm_r = k.ring("Pm", [128, 8, 64], BF16, 3)
        Qm_r = k.ring("Qm", [128, 8, 128], BF16, 3)
        ubn = [k.sb("ub%d" % i, [128, 4096], BF16) for i in range(2)]
        ub_r = Ring(ubn)

        class _V:
            pass
        vbn = []
        for sgb in stage.bufs:
            o = _V()
            o.t = sgb.t[:].bitcast(BF16)
            o.s = sgb.s
            vbn.append(o)
        vb_r = Ring(vbn)
        gl_r = k.ring("gl", [128, 256], BF16, 4)
        ga_r = k.ring("ga", [128, 256], BF16, 4)
        xts_of = {}
        pq = pb[0]

        def phase1(g):
            bs = g % 2
            hfT, trip = hfT2[bs], trip2[bs]
            xts = []
            xts_of[g] = xts
            for blk in range(2):
                xt = x2r.next()
                row0 = (g * 2 + blk) * 128
                k.dma(xt.t[:], x2_s[row0:row0 + 128, :], xt, w=[xt])
                xts.append(xt)
                rs, rs_s = rstd_of(xt.t[:], D, [xt])
                hb = hbring.next()
                k.op("dve", lambda e, hb=hb, xt=xt, rs=rs: e.tensor_scalar(out=hb.t[:], in0=xt.t[:], scalar1=rs, scalar2=None,
                                                                           op0=ALU.mult), r=[xt, rs_s], w=[hb])
                yield
                transposes(None, hb.t[:], 8, 128, [hb], [hfT], dst_ap=hfT.t[:, :, blk * 128:(blk + 1) * 128], pst=tp0)
                yield
            for fp in range(8):
                wc = wq_r.next()
                k.dma(wc.t[:].rearrange("p c n -> p (c n)"), wq_s[fp], wc, w=[wc])
                for f2 in range(2):
                    for c in range(8):
                        k.mm(pq.t[:, f2 * 256:(f2 + 1) * 256], wc.t[:, c, f2 * 128:(f2 + 1) * 128], hfT.t[:, c, :], c == 0, c == 7,
                             r=[wc, hfT], w=[pq])
                k.op("act", lambda e, fp=fp: e.copy(out=qT.t[:, fp * 2:fp * 2 + 2, :].rearrange("p a b -> p (a b)"),
                                                   in_=pq.t[:]), r=[pq], w=[qT])
                yield
            for blk in range(2):
                for qd in range(4):
                    for i4 in range(4):
                        hc = qd * 4 + i4
                        k.mm(pq.t[:, i4 * 128:(i4 + 1) * 128], qT.t[:, hc, blk * 128:(blk + 1) * 128], keysT.t[:, hc, :], True, True,
                             r=[qT, keysT], w=[pq])
                    k.op("act", lambda e, qd=qd: e.copy(out=sc.t[:, qd * 512:(qd + 1) * 512], in_=pq.t[:]),
                         r=[pq], w=sc_s[qd * 4:qd * 4 + 4])
                    yield

                def grp(i):
                    return sc.t[:, i * 128:(i + 1) * 128]
                for i in range(16):
                    k.op("dve", lambda e, i=i: e.max(out=tops.t[:, i, 0:8], in_=grp(i)), r=[sc_s[i]], w=[tops_s[i]])
                yield
                for i in range(16):
                    k.op("dve", lambda e, i=i: e.max_index(out=idxu.t[:, i, 0:8], in_max=tops.t[:, i, 0:8], in_values=grp(i)),
                         r=[sc_s[i], tops_s[i]], w=[idxu_s[i]])
                yield
                for i in range(16):
                    k.op("dve", lambda e, i=i: e.match_replace(out=grp(i), in_to_replace=tops.t[:, i, 0:8], in_values=grp(i),
                                                              imm_value=NEG), r=[tops_s[i], sc_s[i]], w=[sc_s[i]])
                yield
                for i in range(16):
                    k.op("dve", lambda e, i=i: e.max(out=tops.t[:, i, 8:16], in_=grp(i)), r=[sc_s[i]], w=[tops_s[i]])
                yield
                for i in range(16):
                    k.op("dve", lambda e, i=i: e.max_index(out=idxu.t[:, i, 8:16], in_max=tops.t[:, i, 8:16], in_values=grp(i)),
                         r=[sc_s[i], tops_s[i]], w=[idxu_s[i]])
                k.op("dve", lambda e: e.tensor_copy(out=idxf.t[:], in_=idxu.t[:]), r=idxu_s, w=[idxf])
                yield
                tv = tops.t[:].rearrange("p (h c) a -> p h c a", c=2)
                candv = big8.t[:].rearrange("p (h a b) -> p h a b", a=16, b=16)
                k.op("dve", lambda e: e.tensor_tensor(out=candv, in0=tv[:, :, 0, :].unsqueeze(3).to_broadcast([128, 8, 16, 16]),
                                                      in1=tv[:, :, 1, :].unsqueeze(2).to_broadcast([128, 8, 16, 16]), op=ALU.add),
                     r=tops_s, w=cand_s)
                yield

                def cnd(h):
                    return big8.t[:, h * 256:(h + 1) * 256]
                for h in range(8):
                    k.op("dve", lambda e, h=h: e.max(out=best.t[:, h, 0:8], in_=cnd(h)), r=[cand_s[h]], w=[best_s[h]])
                yield
                for h in range(8):
                    k.op("dve", lambda e, h=h: e.max_index(out=posu.t[:, h, 0:8], in_max=best.t[:, h, 0:8], in_values=cnd(h)),
                         r=[cand_s[h], best_s[h]], w=[posu_s[h]])
                yield
                for h in range(8):
                    k.op("dve", lambda e, h=h: e.match_replace(out=cnd(h), in_to_replace=best.t[:, h, 0:8], in_values=cnd(h),
                                                              imm_value=NEG), r=[best_s[h], cand_s[h]], w=[cand_s[h]])
                yield
                for h in range(8):
                    k.op("dve", lambda e, h=h: e.max(out=best.t[:, h, 8:16], in_=cnd(h)), r=[cand_s[h]], w=[best_s[h]])
                yield
                for h in range(8):
                    k.op("dve", lambda e, h=h: e.max_index(out=posu.t[:, h, 8:16], in_max=best.t[:, h, 8:16], in_values=cnd(h)),
                         r=[cand_s[h], best_s[h]], w=[posu_s[h]])
                yield
                k.op("dve", lambda e: e.tensor_single_scalar(out=apu.t[:], in_=posu.t[:], scalar=4, op=ALU.logical_shift_right),
                     r=posu_s, w=[apu])
                k.op("dve", lambda e: e.tensor_single_scalar(out=bpu.t[:], in_=posu.t[:], scalar=15, op=ALU.bitwise_and),
                     r=posu_s, w=[bpu])
                k.op("dve", lambda e: e.tensor_copy(out=apf.t[:], in_=apu.t[:]), r=[apu], w=[apf])
                k.op("dve", lambda e: e.tensor_copy(out=bpf.t[:], in_=bpu.t[:]), r=[bpu], w=[bpf])
                yield
                Ev = big8.t[:].rearrange("p (h r a) -> p h r a", r=16, a=16)
                iov = iota16.t[:].unsqueeze(1).unsqueeze(1).to_broadcast([128, 8, 16, 16])
                fv = idxf.t[:].rearrange("p (h c) a -> p h c a", c=2)
                for which, pf in ((0, apf), (1, bpf)):
                    k.op("dve", lambda e, pf=pf: e.tensor_tensor(out=Ev, in0=pf.t[:].unsqueeze(3).to_broadcast([128, 8, 16, 16]),
                                                                in1=iov, op=ALU.is_equal), r=[pf, iota16] + cand_s, w=cand_s)
                    yield
                    k.op("dve", lambda e, which=which: e.tensor_tensor(
                        out=Ev, in0=Ev, in1=fv[:, :, which, :].unsqueeze(2).to_broadcast([128, 8, 16, 16]), op=ALU.mult),
                        r=[idxf] + cand_s, w=cand_s)
                    yield
                    k.op("dve", lambda e, which=which: e.tensor_reduce(
                        out=sel.t[:, which, :].rearrange("p (h r) -> p h r", r=16), in_=Ev, axis=AX.X, op=ALU.add),
                        r=cand_s, w=[sel])
                    yield
                k.op("dve", lambda e: e.tensor_tensor(out=exb.t[:], in0=best.t[:], in1=best.t[:, :, 0:1].to_broadcast([128, 8, 16]),
                                                      op=ALU.subtract), r=best_s, w=[exb])
                k.act(exb.t[:], exb.t[:], AF.Exp, r=[exb], w=[exb])
                k.op("dve", lambda e: e.tensor_reduce(out=sm8.t[:], in_=exb.t[:], axis=AX.X, op=ALU.add), r=[exb], w=[sm8])
                k.op("dve", lambda e: e.reciprocal(out=sm8.t[:], in_=sm8.t[:]), r=[sm8], w=[sm8])
                k.op("dve", lambda e: e.tensor_tensor(out=sel.t[:, 2, :].rearrange("p (h r) -> p h r", r=16), in0=exb.t[:],
                                                      in1=sm8.t[:].unsqueeze(2).to_broadcast([128, 8, 16]), op=ALU.mult),
                     r=[exb, sm8], w=[sel])
                yield
                for w3 in range(3):
                    k.tr(pq.t[:, w3 * 128:(w3 + 1) * 128], sel.t[:, w3, :], identf.t[:], r=[sel, identf], w=[pq])
                k.op("act", lambda e, blk=blk: e.copy(out=trip.t[:, :, blk * 128:(blk + 1) * 128],
                                                      in_=pq.t[:, 0:384].rearrange("p (a t) -> p a t", t=128)),
                     r=[pq], w=[trip])
                yield

        GT_s = [T("GT_lo"), T("GT_hi")]
        gbank = _V()
        gbank.t = ps_tp.t[:].bitcast(F32)
        gbank.s = ps_tp.s

        def ggen_half(g, half):
            trip = trip2[g % 2]
            i0_ = half * 64
            io_q = iota128.t[:].unsqueeze(1).to_broadcast([128, 8, 128])
            io_p = iota128.t[:, i0_:i0_ + 64].unsqueeze(1).to_broadcast([128, 8, 64])
            slabs = {}

            def build(sl):
                t0 = sl * 8
                Pm, Qm = Pm_r.next(), Qm_r.next()
                slabs[sl] = (Pm, Qm)
                k.op("dve", lambda e: e.tensor_tensor(
                    out=Qm.t[:], in0=io_q, in1=trip.t[:, 1, t0:t0 + 8].unsqueeze(2).to_broadcast([128, 8, 128]), op=ALU.is_equal),
                    r=[iota128, trip], w=[Qm])
                k.op("dve", lambda e: e.tensor_tensor(
                    out=Pm.t[:], in0=io_p, in1=trip.t[:, 0, t0:t0 + 8].unsqueeze(2).to_broadcast([128, 8, 64]),
                    op=ALU.is_equal), r=[iota128, trip], w=[Pm])
                k.op("pool", lambda e: e.tensor_tensor(
                    out=Pm.t[:], in0=Pm.t[:], in1=trip.t[:, 2, t0:t0 + 8].unsqueeze(2).to_broadcast([128, 8, 64]),
                    op=ALU.mult), r=[Pm, trip], w=[Pm])

            def consume(sl):
                t0 = sl * 8
                Pm, Qm = slabs.pop(sl)
                for t in range(8):
                    k.mm(gbank.t[:, t * 64:(t + 1) * 64], Qm.t[:, t, :], Pm.t[:, t, :], True, True, r=[Qm, Pm], w=[gbank])
                k.op("act", lambda e: e.copy(out=GT.t[:, i0_:i0_ + 64, t0:t0 + 8],
                                             in_=gbank.t[:].rearrange("p (t i) -> p i t", t=8)),
                     r=[gbank], w=[GT_s[half]])

            build(0)
            yield
            build(1)
            yield
            for sl in range(32):
                consume(sl)
                if sl + 2 < 32:
                    build(sl + 2)
                yield

        ybank = [pb[1], pb[2], pb[3], pb[4]]
        tp0 = _V()
        tp0.t = pb[0].t[:].bitcast(BF16)
        tp0.s = pb[0].s
        pu_slots = [(pb[5].t[:, 0:256], pb[5].s), (pb[6].t[:, 0:256], pb[6].s)]

        def step(gens):
            for gx in gens:
                next(gx, None)

        def drain(gens):
            for gx in gens:
                for _ in gx:
                    pass

        def eloop(g, first_gens, second_gens):
            hfT = hfT2[g % 2]
            n_i = n_eg * 4
            n_half = 64
            bufs = {}

            def u_part(i):
                eg, c = i // 4, i % 4
                if c == 0:
                    ub, vb = ub_r.next(), vb_r.next()
                    k.dma(ub.t[:], us_s[eg], ub, w=[ub])
                    k.dma(vb.t, vs_s[eg], vb, w=[vb])
                    bufs[eg] = (ub, vb)
                ub, vb = bufs[eg]
                pu_ap, pu_s = pu_slots[i % 2]
                for kc in range(8):
                    k.mm(pu_ap, ub.t[:, kc * 512 + c * 128:kc * 512 + (c + 1) * 128], hfT.t[:, kc, :], kc == 0, kc == 7,
                         r=[ub, hfT], w=[pu_s])
                gl = gl_r.next()
                k.act(gl.t[:], pu_ap, AF.Gelu, r=[pu_s], w=[gl])
                ga = ga_r.next()
                gts = GT_s[0] if i < 64 else GT_s[1]
                k.op("dve", lambda e, ga=ga, gl=gl, i=i: e.tensor_tensor(out=ga.t[:], in0=gl.t[:], in1=GT.t[:, i, :], op=ALU.mult),
                     r=[gl, gts], w=[ga])
                return ga

            def v_part(i, ga):
                eg, c = i // 4, i % 4
                ub, vb = bufs[eg]
                for blk in range(2):
                    for half in range(2):
                        k.mm(ybank[blk * 2 + half].t[:], ga.t[:, blk * 128:(blk + 1) * 128],
                             vb.t[:, c * 1024 + half * 512:c * 1024 + (half + 1) * 512], i == 0, i == n_i - 1,
                             r=[ga, vb], w=[ybank[blk * 2 + half]])

            gas = {0: u_part(0)}
            for i in range(n_i):
                if i == n_half:
                    drain(first_gens)
                if i + 1 < n_i:
                    if i + 1 == n_half:
                        drain(first_gens)
                    gas[i + 1] = u_part(i + 1)
                v_part(i, gas.pop(i))
                if i >= 1:
                    step(first_gens if i < n_half else second_gens)
            drain(first_gens)
            drain(second_gens)

        def finalize(g):
            xts = xts_of.pop(g)
            for blk in range(2):
                xt = xts[blk]
                xo = xt
                for half in range(2):
                    k.op("dve", lambda e, xo=xo, xt=xt, blk=blk, half=half: e.tensor_tensor(
                        out=xo.t[:, half * 512:(half + 1) * 512], in0=ybank[blk * 2 + half].t[:],
                        in1=xt.t[:, half * 512:(half + 1) * 512], op=ALU.add), r=[ybank[blk * 2 + half], xt], w=[xo])
                rs, rs_s = rstd_of(xo.t[:], D, [xo])
                k.op("dve", lambda e, xo=xo, rs=rs: e.scalar_tensor_tensor(out=xo.t[:], in0=xo.t[:], scalar=rs, in1=gfin.t[:],
                                                                         op0=ALU.mult, op1=ALU.mult), r=[xo, rs_s, gfin], w=[xo])
                row0 = (g * 2 + blk) * 128
                k.dma(out_d[row0:row0 + 128, :], xo.t[:], xo, r=[xo])

        for _ in phase1(0):
            pass
        drain([ggen_half(0, 0), ggen_half(0, 1)])
        for g in range(n_grp):
            first = []
            if g > 0:
                first.append(ggen_half(g, 1))
            if g + 1 < n_grp:
                first.append(phase1(g + 1))
            second = [ggen_half(g + 1, 0)] if g + 1 < n_grp else []
            eloop(g, first, second)
            finalize(g)
            trip = trip2[g % 2]
            if dbg == "p2" and g == n_grp - 1:
                dt_ = k.sb("dbgt", [128, 1024], F32)
                k.op("pool", lambda e: e.tensor_copy(out=dt_.t[:, 0:768], in_=trip.t[:].rearrange("p a t -> p (a t)")), r=[trip], w=[dt_])
                k.dma(dbg_d[:, 0:768], dt_.t[:, 0:768], dt_, r=[dt_])
    k.pop_scope()

    P.state = dict(locals())
    return P


def finish(P):
    P.k.S.barrier()
    P.k.S.emit()
    P.st.close()
    return P.nc


def _bf(a):
    return np.asarray(a, dtype=np.float32).astype(ml_dtypes.bfloat16)


def make_in_maps(x, mem, positions, norm_mix, w_in, q_a_norm, w_q_b, kv_a_norm, w_kv_b,
                 swa_sinks, out_norm_mla, out_norm_swa, w_out, norm_cross, norm_mem,
                 w_cq, w_ck, w_cv, w_co, norm_ffn, peer_w_q, peer_keys, peer_u, peer_v, norm_final):
    f32 = np.float32
    x = np.asarray(x, f32)
    mem = np.asarray(mem, f32)
    positions = np.asarray(positions, np.int32)
    w_in0 = np.asarray(w_in[0], f32)
    shared = {}
    shared["identb"] = np.eye(128, dtype=f32).astype(ml_dtypes.bfloat16)
    shared["identf"] = np.eye(128, dtype=f32)
    inv = (np.float32(10000.0) ** (-np.arange(32, dtype=f32) / np.float32(32))).astype(f32)
    shared["invf"] = np.ascontiguousarray(np.broadcast_to(inv[None, :], (128, 32))).astype(f32)
    kk = np.arange(128, dtype=f32)
    kaug = np.zeros((2, 2, 128), f32)
    kaug[0, :, :] = 1.0
    kaug[1, 0, :] = kk - 128.0
    kaug[1, 1, :] = kk
    shared["kaug"] = _bf(kaug.reshape(2, 256))
    slopes = 2.0 ** (-(np.arange(1, 9, dtype=f32)))
    qaug = np.zeros((2, 2, 4, 128), f32)
    for kvh in range(2):
        for g in range(4):
            s = slopes[kvh * 4 + g]
            qaug[0, kvh, g, :] = -s * kk * 8.0
            qaug[1, kvh, g, :] = s * 8.0
    shared["qaug"] = _bf(qaug.reshape(2, 1024))
    shared["iota128"] = np.ascontiguousarray(np.broadcast_to(kk[None, :], (128, 128))).astype(f32)
    shared["iota16"] = np.ascontiguousarray(np.broadcast_to(kk[None, :16], (128, 16))).astype(f32)
    gc = np.zeros((128, NGC), f32)

    def cols(vec, c0):
        v = np.asarray(vec, f32).reshape(-1, 128)
        gc[:, c0:c0 + v.shape[0]] = v.T

    cols(norm_mix[0], G_MIX)
    cols(norm_cross[0], G_CROSS)
    cols(norm_mem[0], G_MEM)
    cols(norm_ffn[0], G_FFN)
    cols(q_a_norm[0], G_QA)
    cols(kv_a_norm[0], G_KVA)
    cols(np.concatenate([np.asarray(out_norm_mla[0], f32), np.asarray(out_norm_swa[0], f32)]), G_OUT)
    shared["gcols"] = gc
    shared["kvarow"] = np.asarray(kv_a_norm[0], f32).reshape(1, 128)
    shared["sinks"] = np.asarray(swa_sinks[0], f32).reshape(1, 8)
    shared["gfin"] = np.asarray(norm_final, f32).reshape(1, D)
    shared["w_kvl"] = np.ascontiguousarray(w_in0[:, 256:448])
    shared["w_in"] = w_in0
    wqb = np.asarray(w_q_b[0], f32).reshape(256, 4, 192)
    shared["w_qb"] = np.ascontiguousarray(np.concatenate([wqb[:, :, :128].reshape(256, 512),
                                                          wqb[:, :, 128:].reshape(256, 256)], axis=1))
    wkvb = np.asarray(w_kv_b[0], f32).reshape(128, 4, 256)
    shared["w_ukT"] = np.ascontiguousarray(wkvb[:, :, :128].transpose(2, 1, 0)).reshape(128, 512)
    shared["w_uv"] = np.ascontiguousarray(wkvb[:, :, 128:]).reshape(128, 512)
    shared["w_out"] = np.asarray(w_out[0], f32)
    shared["w_cq"] = np.asarray(w_cq[0], f32)
    shared["w_ck"] = np.asarray(w_ck[0], f32)
    shared["w_cv"] = np.asarray(w_cv[0], f32)
    shared["w_co"] = np.asarray(w_co[0], f32)
    shared["w_pq"] = np.asarray(peer_w_q[0], f32)
    pk = np.asarray(peer_keys[0], f32)
    shared["keysT"] = np.ascontiguousarray(pk.transpose(3, 0, 1, 2)).reshape(128, 2048)
    shared["uT"] = np.ascontiguousarray(np.asarray(peer_u[0], f32).T)
    shared["v"] = np.asarray(peer_v[0], f32)

    tri_c = (kk[:, None] <= kk[None, :]).astype(f32)
    tri_p = (kk[:, None] > kk[None, :]).astype(f32)
    in_maps = []
    for c in range(NCORE):
        b, r = c // 4, c % 4
        qb = own_blocks(r)
        m = dict(shared)
        m["xb"] = x[b]
        xo = x[b].reshape(NBLK, 128, D)
        m["xown"] = np.ascontiguousarray(xo[qb]).reshape(NOWN * 128, D)
        prev = [max(n - 1, 0) for n in qb]
        m["xprev"] = np.ascontiguousarray(xo[prev]).reshape(NOWN * 128, D)
        m["memb"] = mem[b]
        pb_ = positions[b].reshape(NBLK, 128)
        m["posb"] = np.ascontiguousarray(pb_.T)
        m["poso"] = np.ascontiguousarray(pb_[qb].T)
        mm_ = np.zeros((NOWN, 128, 4, 128), f32)
        sm_ = np.zeros((NOWN, 128, 2, 128), f32)
        for ki, n in enumerate(qb):
            L = n_keyblocks(ki)
            for jj in range(4):
                j = L - 4 + jj
                if j < n:
                    mm_[ki, :, jj, :] = 1.0
                elif j == n:
                    mm_[ki, :, jj, :] = tri_c
            if n > 0:
                sm_[ki, :, 0, :] = tri_p
            sm_[ki, :, 1, :] = tri_c
        m["mmask"] = _bf(mm_.reshape(NOWN, 128, 512))
        m["smask"] = _bf(sm_.reshape(NOWN, 128, 256))
        in_maps.append(m)
    return in_maps


def kernel(**inputs):
    in_maps = make_in_maps(**inputs)
    P = build()
    nc = finish(P)
    res = run_bass_kernel_spmd(nc, in_maps, core_ids=list(range(NCORE)))
    out = np.zeros((2, SEQ, D), np.float32)
    for c in range(NCORE):
        b, r = c // 4, c % 4
        qb = own_blocks(r)
        o = np.asarray(res.results[c]["out"], np.float32).reshape(NOWN, 128, D)
        ov = out[b].reshape(NBLK, 128, D)
        ov[qb] = o
    return out
```

```python
import numpy as np
import ml_dtypes
from contextlib import ExitStack

import concourse.bass as bass
import concourse.mybir as mybir
from concourse.bass_utils import run_bass_kernel_spmd

F32 = mybir.dt.float32
BF16 = mybir.dt.bfloat16
I32 = mybir.dt.int32
U32 = mybir.dt.uint32
AF = mybir.ActivationFunctionType
ALU = mybir.AluOpType
AX = mybir.AxisListType

D = 1024
SEQ = 16384
NBLK = SEQ // 128
NCORE = 8
EPS = 1e-6
NOWN = 32
NEXP = 16384


def own_blocks(r):
    s = set()
    for m in range(16):
        s.add(8 * m + r)
        s.add(8 * m + 7 - r)
    return sorted(s)


class T:
    __slots__ = ("name", "w", "r", "dsem", "dcnt")

    def __init__(self, name):
        self.name = name
        self.w = None
        self.r = {}
        self.dsem = None
        self.dcnt = 0


class Sched:
    ENGS = ("pe", "act", "dve", "pool", "sp")

    def __init__(self, nc, st):
        self.nc = nc
        self.st = st
        self.prog = {e: [] for e in self.ENGS}
        self.seen = {e: {} for e in self.ENGS}
        self.esem = {}
        for e in ("pe", "act", "dve", "pool"):
            self.esem[e] = st.enter_context(nc.semaphore("s_" + e))
        self.bsem = st.enter_context(nc.semaphore("s_bar"))
        self.bcnt = 0
        self.dtiles = []
        self.fuse_waits = True

    def _deps(self, eng, reads, writes):
        evs = []
        for t in reads:
            if t.w is not None:
                evs.append(t.w)
        for t in writes:
            if t.w is not None:
                evs.append(t.w)
            evs.extend(t.r.values())
        waits = []
        seen = self.seen[eng]
        for ev in evs:
            if ev[0] == "e":
                _, f, idx = ev
                if f == eng and eng == "pe":
                    continue
                if seen.get(f, -1) >= idx:
                    continue
                seen[f] = idx
                self.prog[f][idx]["inc"] = True
                waits.append(ev)
            else:
                _, owner, cnt = ev
                key = ("d", id(owner))
                if seen.get(key, 0) >= cnt:
                    continue
                cur = owner.dcnt
                seen[key] = cur
                waits.append(("d", owner, cur))
        return waits

    def op(self, eng, fn, reads=(), writes=()):
        waits = self._deps(eng, reads, writes)
        idx = len(self.prog[eng])
        self.prog[eng].append({"fn": fn, "waits": waits, "inc": False, "dma": None})
        ev = ("e", eng, idx)
        for t in reads:
            t.r[eng] = ev
        for t in writes:
            t.w = ev
            t.r = {}

    def dma(self, fn, owner, reads=(), writes=(), q="sp"):
        waits = self._deps(q, reads, writes)
        if owner.dsem is None:
            owner.dsem = self.st.enter_context(self.nc.semaphore("d_" + owner.name))
            self.dtiles.append(owner)
        owner.dcnt += 16
        self.prog[q].append({"fn": fn, "waits": waits, "inc": False, "dma": owner})
        ev = ("d", owner, owner.dcnt)
        for t in reads:
            t.r[("d", id(owner))] = ev
        for t in writes:
            t.w = ev
            t.r = {}

    def barrier(self):
        waits = []
        for o in self.dtiles:
            if o.dcnt > 0:
                waits.append(("d", o, o.dcnt))
        for e in ("pe", "act", "dve", "pool"):
            idx = len(self.prog[e]) - 1
            while idx >= 0 and self.prog[e][idx]["fn"] is None:
                idx -= 1
            if idx >= 0:
                self.prog[e][idx]["inc"] = True
                waits.append(("e", e, idx))
        self.bcnt += 1
        self.prog["sp"].append({"fn": None, "waits": waits, "inc": False, "dma": None, "bar": self.bcnt})
        for e in ("pe", "act", "dve", "pool"):
            self.prog[e].append({"fn": None, "waits": [("b", self.bcnt)], "inc": False, "dma": None})

    def emit(self):
        nc = self.nc
        for e in ("pe", "act", "dve", "pool"):
            c = 0
            for rec in self.prog[e]:
                if rec["inc"]:
                    c += 1
                    rec["semval"] = c
        engmap = {"pe": "tensor", "act": "scalar", "dve": "vector", "pool": "gpsimd", "sp": "sync"}
        with nc.Block() as block:
            for e in self.ENGS:
                def body(engobj, e=e):
                    def semval(w):
                        if w[0] == "e":
                            return self.esem[w[1]], self.prog[w[1]][w[2]]["semval"]
                        elif w[0] == "d":
                            return w[1].dsem, w[2]
                        return self.bsem, w[1]
                    for rec in self.prog[e]:
                        ws = rec["waits"]
                        fuse = None
                        if rec["fn"] is not None and ws and rec["dma"] is None and self.fuse_waits:
                            fuse = ws[-1]
                            ws = ws[:-1]
                        for w in ws:
                            engobj.wait_ge(*semval(w))
                        if rec["fn"] is None:
                            if rec.get("bar") is not None:
                                engobj.sem_inc(self.bsem, 1)
                            continue
                        ins = rec["fn"]()
                        if fuse is not None:
                            ins._wait_ge(*semval(fuse))
                        if rec["dma"] is not None:
                            ins.then_inc(rec["dma"].dsem, 16)
                        elif rec["inc"]:
                            ins.then_inc(self.esem[e], 1)
                getattr(block, engmap[e])(body)


class Buf:
    def __init__(self, t, name):
        self.t = t
        self.s = T(name)


class K:
    def __init__(self, nc, st):
        self.nc = nc
        self.st = st
        self.S = Sched(nc, st)
        self.eng = {"pe": nc.tensor, "act": nc.scalar, "dve": nc.vector, "pool": nc.gpsimd, "sp": nc.sync}
        self._n = 0
        self.main_st = st

    def push_scope(self):
        self.st = ExitStack()

    def pop_scope(self):
        self.S.barrier()
        self.st.close()
        self.st = self.main_st

    def sb(self, name, shape, dt):
        return Buf(self.st.enter_context(self.nc.sbuf_tensor("sb_" + name, shape, dt)), name)

    def ps(self, name, shape, dt):
        return Buf(self.st.enter_context(self.nc.psum_tensor("pp_" + name, shape, dt)), name)

    def ring(self, name, shape, dt, n):
        return Ring([self.sb("%s%d" % (name, i), shape, dt) for i in range(n)])

    def op(self, eng, fn, r=(), w=()):
        e = self.eng[eng]
        self.S.op(eng, lambda: fn(e), [getattr(x, "s", x) for x in r], [getattr(x, "s", x) for x in w])

    def dma(self, out, in_, owner, r=(), w=(), q="sp", **kw):
        e = self.eng[q]
        self.S.dma(lambda: e.dma_start(out=out, in_=in_, **kw), getattr(owner, "s", owner),
                   [getattr(x, "s", x) for x in r], [getattr(x, "s", x) for x in w], q=q)

    def mm(self, out, lhsT, rhs, start, stop, r, w, skip=False):
        self.op("pe", lambda e: e.matmul(out, lhsT, rhs, start=start, stop=stop, skip_group_check=skip), r, w)

    def tr(self, out, in_, ident, r, w):
        self.op("pe", lambda e: e.transpose(out, in_, ident), r, w)

    def act(self, out, in_, func, r, w, scale=1.0, bias=0.0):
        self.op("act", lambda e: e.activation(out, in_, func, bias=bias, scale=scale), r, w)


class Ring:
    def __init__(self, bufs):
        self.bufs = bufs
        self.i = 0

    def next(self):
        b = self.bufs[self.i % len(self.bufs)]
        self.i += 1
        return b


G_MIX, G_CROSS, G_MEM, G_FFN, G_QA, G_KVA, G_OUT = 0, 8, 16, 24, 32, 34, 35
NGC = 43
C1 = 6.28125
C2 = float(2.0 * np.pi - 6.28125)
MAGIC = 12582912.0
PI_LO = 3.1415925
NEG = -1.0e30


def n_keyblocks(kidx):
    m = kidx // 2
    return 8 * m + 4 if kidx % 2 == 0 else 8 * m + 8


class StopBuild(Exception):
    pass


class Prog:
    def __init__(self, cfg):
        self.cfg = cfg
        nc = bass.Bass("TRN2", target_bir_lowering=False)
        self.nc = nc
        self.st = ExitStack()
        self.k = K(nc, self.st)
        self.dram = {}

    def din(self, name, shape, dt):
        t = self.nc.dram_tensor(name, list(shape), dt, kind="ExternalInput").ap()
        self.dram[name] = t
        return t

    def dscr(self, name, shape, dt):
        t = self.nc.dram_tensor(name, list(shape), dt, kind="Internal").ap()
        self.dram[name] = t
        return t

    def dout(self, name, shape, dt):
        t = self.nc.dram_tensor(name, list(shape), dt, kind="ExternalOutput").ap()
        self.dram[name] = t
        return t


def build(cfg=None):
    cfg = dict(cfg or {})
    n_sb = cfg.get("n_sb", 16)
    n_own = cfg.get("n_own", NOWN)
    n_grp = cfg.get("n_grp", NOWN // 2)
    n_eg = cfg.get("n_eg", 32)
    do_p0 = cfg.get("p0", True)
    do_p1a = cfg.get("p1a", True)
    do_p1b = cfg.get("p1b", True)
    do_p2 = cfg.get("p2", True)
    dbg = cfg.get("dbg", None)

    P = Prog(cfg)
    nc, k, st = P.nc, P.k, P.st

    xb = P.din("xb", [SEQ, D], F32)
    xown = P.din("xown", [NOWN * 128, D], F32)
    xprev = P.din("xprev", [NOWN * 128, D], F32)
    memb = P.din("memb", [256, D], F32)
    posb = P.din("posb", [128, NBLK], I32)
    poso = P.din("poso", [128, NOWN], I32)
    mmask_d = P.din("mmask", [NOWN, 128, 4 * 128], BF16)
    smask_d = P.din("smask", [NOWN, 128, 2 * 128], BF16)
    identb_d = P.din("identb", [128, 128], BF16)
    identf_d = P.din("identf", [128, 128], F32)
    invf_d = P.din("invf", [128, 32], F32)
    kaug_d = P.din("kaug", [2, 2 * 128], BF16)
    qaug_d = P.din("qaug", [2, 2 * 512], BF16)
    iota128_d = P.din("iota128", [128, 128], F32)
    iota16_d = P.din("iota16", [128, 16], F32)
    gcols_d = P.din("gcols", [128, NGC], F32)
    kvarow_d = P.din("kvarow", [1, 128], F32)
    sinks_d = P.din("sinks", [1, 8], F32)
    gfin_d = P.din("gfin", [1, D], F32)
    w_kvl_d = P.din("w_kvl", [D, 192], F32)
    w_in_d = P.din("w_in", [D, 1216], F32)
    w_qb_d = P.din("w_qb", [256, 768], F32)
    w_ukT_d = P.din("w_ukT", [128, 512], F32)
    w_uv_d = P.din("w_uv", [128, 512], F32)
    w_out_d = P.din("w_out", [D, D], F32)
    w_cq_d = P.din("w_cq", [D, 512], F32)
    w_ck_d = P.din("w_ck", [D, 512], F32)
    w_cv_d = P.din("w_cv", [D, 512], F32)
    w_co_d = P.din("w_co", [512, D], F32)
    w_pq_d = P.din("w_pq", [D, 2048], F32)
    keysT_d = P.din("keysT", [128, 2048], F32)
    uT_d = P.din("uT", [D, NEXP], F32)
    v_d = P.din("v", [NEXP, D], F32)

    kc_s = P.dscr("kc_s", [16, 128, 1024], BF16)
    kr_s = P.dscr("kr_s", [16, 64, 1024], BF16)
    vc_s = P.dscr("vc_s", [16, 128, 8 * 129], BF16)
    x2_s = P.dscr("x2_s", [NOWN * 128, D], F32)
    us_s = P.dscr("us_s", [32, 128, 4096], BF16)
    vs_s = P.dscr("vs_s", [32, 128, 4096], BF16)
    out_d = P.dout("out", [NOWN * 128, D], F32)
    dbg_d = P.dout("dbg", [128, 4096], F32) if dbg else None

    ps_tp = k.ps("ps_tp", [128, 1024], BF16)
    pb = [k.ps("ps_b%d" % i, [128, 512], F32) for i in range(7)]

    identb = k.sb("identb", [128, 128], BF16)
    identf = k.sb("identf", [128, 128], F32)
    invf = k.sb("invf", [128, 32], F32)
    gcols = k.sb("gcols", [128, NGC], F32)
    k.dma(identb.t[:], identb_d, identb, w=[identb])
    k.dma(identf.t[:], identf_d, identf, w=[identf])
    k.dma(invf.t[:], invf_d, invf, w=[invf])
    k.dma(gcols.t[:], gcols_d, gcols, w=[gcols])

    scal_t = k.sb("scal", [128, 64], F32)
    scal_s = [T("scal%d" % i) for i in range(64)]
    scal_i = [0]

    def new_scal():
        i = scal_i[0] % 64
        scal_i[0] += 1
        return scal_t.t[:, i:i + 1], scal_s[i]

    junk = k.sb("junk", [128, 1024], BF16)
    xring = k.ring("xt", [128, D], F32, 3)
    hbring = k.ring("hb", [128, D], BF16, 2)
    hTring = k.ring("hT", [128, 8, 128], BF16, 2)
    ropet = k.ring("ropet", [128, 2, 8, 32], F32, 2)
    stage = k.ring("stage", [128, 2048], F32, 2)

    def rstd_of(x_ap, F, x_tiles, eng_sq="dve"):
        ss, ss_s = new_scal()
        k.op("dve", lambda e: e.scalar_tensor_tensor(out=junk.t[:, 0:F], in0=x_ap, scalar=1.0, in1=x_ap,
                                                     op0=ALU.mult, op1=ALU.mult, accum_out=ss),
             r=x_tiles, w=[junk, ss_s])
        ln, ln_s = new_scal()
        k.act(ln, ss, AF.Ln, r=[ss_s], w=[ln_s], scale=1.0 / F, bias=EPS)
        rs, rs_s = new_scal()
        k.act(rs, ln, AF.Exp, r=[ln_s], w=[rs_s], scale=-0.5)
        return rs, rs_s

    def load_cast(dst, src_d, KC, N, gcol0=None, eng_cycle=("dve", "pool")):
        src_v = src_d.rearrange("(c p) n -> p c n", p=128)
        per = max(1, 2048 // N)
        ei = 0
        for c0 in range(0, KC, per):
            c1 = min(KC, c0 + per)
            sg = stage.next()
            k.dma(sg.t[:, 0:(c1 - c0) * N].rearrange("p (c n) -> p c n", n=N), src_v[:, c0:c1, :], sg, w=[sg])
            for c in range(c0, c1):
                eng = eng_cycle[ei % len(eng_cycle)]
                ei += 1
                src_ap = sg.t[:, (c - c0) * N:(c - c0 + 1) * N]
                dst_ap = dst.t[:, c, :]
                if gcol0 is None:
                    k.op(eng, lambda e, o=dst_ap, i=src_ap: e.tensor_copy(out=o, in_=i), r=[sg], w=[dst])
                else:
                    gc = gcols.t[:, gcol0 + c:gcol0 + c + 1]
                    k.op(eng, lambda e, o=dst_ap, i=src_ap, g=gc: e.tensor_scalar(out=o, in0=i, scalar1=g, scalar2=1.0,
                                                                                 op0=ALU.mult, op1=ALU.mult),
                         r=[sg, gcols], w=[dst])

    def transposes(dst, src, nchunk, width, r, w, evac_eng="act", dst_ap=None, pst=None):
        pst = pst or ps_tp
        for c in range(nchunk):
            k.tr(pst.t[0:width, c * 128:(c + 1) * 128], src[:, c * width:(c + 1) * width], identb.t[:],
                 r=r + [identb], w=[pst])
        o = dst_ap if dst_ap is not None else dst.t[0:width, 0:nchunk, :]
        i = pst.t[0:width, 0:nchunk * 128].rearrange("p (c t) -> p c t", t=128)
        if evac_eng == "act":
            k.op("act", lambda e: e.copy(out=o, in_=i), r=[pst], w=w)
        else:
            k.op(evac_eng, lambda e: e.tensor_copy(out=o, in_=i), r=[pst], w=w)

    k.push_scope()
    castring = [None]
    P0Q = cfg.get("p0q", "sp")

    def p0_gen():
        uT_v = uT_d.rearrange("(c p) e -> p c e", p=128)
        v_v = v_d.rearrange("(g c p) d -> g p c d", p=128, c=4)
        engs3 = ("pool", "pool", "pool")
        ei = 0
        for g in range(n_eg):
            for hf in range(2):
                sg = stage.next()
                k.dma(sg.t[:].rearrange("p (c e) -> p c e", e=512), uT_v[:, hf * 4:(hf + 1) * 4, g * 512:(g + 1) * 512], sg, w=[sg], q=P0Q)
                cb = castring[0].next()
                for cc in range(4):
                    c = hf * 4 + cc
                    eng = engs3[ei % 3]
                    ei += 1
                    o = cb.t[:, cc * 512:(cc + 1) * 512]
                    i = sg.t[:, cc * 512:(cc + 1) * 512]
                    gc = gcols.t[:, G_FFN + c:G_FFN + c + 1]
                    if eng == "act":
                        k.op("act", lambda e, o=o, i=i, gc=gc: e.activation(o, i, AF.Copy, scale=gc), r=[sg, gcols], w=[cb])
                    else:
                        k.op(eng, lambda e, o=o, i=i, gc=gc: e.tensor_scalar(out=o, in0=i, scalar1=gc, scalar2=1.0,
                                                                             op0=ALU.mult, op1=ALU.mult),
                             r=[sg, gcols], w=[cb])
                k.dma(us_s[g][:, hf * 2048:(hf + 1) * 2048], cb.t[:], cb, r=[cb], q=P0Q)
                yield
            for hf in range(2):
                sg = stage.next()
                k.dma(sg.t[:].rearrange("p (c d) -> p c d", d=1024), v_v[g][:, hf * 2:(hf + 1) * 2, :], sg, w=[sg], q=P0Q)
                cb = castring[0].next()
                for cc in range(2):
                    eng = engs3[ei % 3]
                    ei += 1
                    o = cb.t[:, cc * 1024:(cc + 1) * 1024]
                    i = sg.t[:, cc * 1024:(cc + 1) * 1024]
                    if eng == "act":
                        k.op("act", lambda e, o=o, i=i: e.copy(out=o, in_=i), r=[sg], w=[cb])
                    else:
                        k.op(eng, lambda e, o=o, i=i: e.tensor_copy(out=o, in_=i), r=[sg], w=[cb])
                k.dma(vs_s[g][:, hf * 2048:(hf + 1) * 2048], cb.t[:], cb, r=[cb], q=P0Q)
                yield

    castring[0] = k.ring("castA", [128, 2048], BF16, 2)
    p0g = p0_gen() if do_p0 else None

    wkvl = k.sb("wkvl", [128, 8, 192], BF16)
    load_cast(wkvl, w_kvl_d, 8, 192, G_MIX)
    posb_i = k.sb("posb_i", [128, NBLK], I32)
    posb_f = k.sb("posb_f", [128, NBLK], F32)
    k.dma(posb_i.t[:], posb, posb_i, w=[posb_i])
    k.op("dve", lambda e: e.tensor_copy(out=posb_f.t[:], in_=posb_i.t[:]), r=[posb_i], w=[posb_f])

    kvsring = k.ring("kvs", [128, 192], F32, 4)
    krb_ring = k.ring("krb", [128, 64], BF16, 3)
    kcst = k.ring("kcst", [128, 1024], BF16, 2)
    krst = k.ring("krst", [64, 1024], BF16, 2)
    vcst = k.ring("vcst", [128, 8, 129], BF16, 2)

    def rope_tables_bulk(name, pos_f, nb, tab=None):
        if tab is None:
            tab = k.sb(name, [128, 2, nb, 32], F32)
        wk = k.sb(name + "_w", [128, 3, nb, 32], F32)
        ang = wk.t[:, 0]
        k.op("dve", lambda e: e.tensor_tensor(out=ang, in0=pos_f.t[:].unsqueeze(2).to_broadcast([128, nb, 32]),
                                              in1=invf.t[:].unsqueeze(1).to_broadcast([128, nb, 32]), op=ALU.mult),
             r=[pos_f, invf], w=[wk])
        for which in (0, 1):
            a2 = wk.t[:, 1]
            nn = wk.t[:, 2]
            off = 0.0 if which == 0 else float(np.pi / 2)
            k.op("dve", lambda e, off=off: e.tensor_scalar(out=a2, in0=ang, scalar1=off, scalar2=None, op0=ALU.add), r=[wk], w=[wk])
            k.op("dve", lambda e: e.tensor_scalar(out=nn, in0=a2, scalar1=float(1.0 / (2 * np.pi)), scalar2=MAGIC,
                                                  op0=ALU.mult, op1=ALU.add), r=[wk], w=[wk])
            k.op("dve", lambda e: e.tensor_scalar(out=nn, in0=nn, scalar1=-MAGIC, scalar2=None, op0=ALU.add), r=[wk], w=[wk])
            k.op("dve", lambda e: e.scalar_tensor_tensor(out=a2, in0=nn, scalar=-C1, in1=a2, op0=ALU.mult, op1=ALU.add), r=[wk], w=[wk])
            k.op("dve", lambda e: e.scalar_tensor_tensor(out=a2, in0=nn, scalar=-C2, in1=a2, op0=ALU.mult, op1=ALU.add), r=[wk], w=[wk])
            k.op("dve", lambda e: e.tensor_scalar(out=a2, in0=a2, scalar1=-PI_LO, scalar2=PI_LO, op0=ALU.max, op1=ALU.min), r=[wk], w=[wk])
            k.act(tab.t[:, which], a2, AF.Sin, r=[wk], w=[tab])
        return tab

    def apply_rope(out_bf, x_ap, nh, sin_ap, cos_ap, tg, x_tiles, out_tiles):
        rt = ropet.next()
        xv = x_ap.rearrange("p (h two j) -> p h two j", two=2, j=32)
        cosb = cos_ap.unsqueeze(1).unsqueeze(1).to_broadcast([128, nh, 2, 32])
        sinb = sin_ap.unsqueeze(1).unsqueeze(1).to_broadcast([128, nh, 2, 32])
        xc = rt.t[:, 0, 0:2 * nh, :].rearrange("p (h two) j -> p h two j", two=2)
        xs = rt.t[:, 1, 0:2 * nh, :].rearrange("p (h two) j -> p h two j", two=2)
        k.op("dve", lambda e: e.tensor_tensor(out=xc, in0=xv, in1=cosb, op=ALU.mult), r=x_tiles + [tg], w=[rt])
        k.op("dve", lambda e: e.tensor_tensor(out=xs, in0=xv, in1=sinb, op=ALU.mult), r=x_tiles + [tg], w=[rt])
        ov = out_bf.rearrange("p (h two j) -> p h two j", two=2, j=32)
        k.op("dve", lambda e: e.tensor_tensor(out=ov[:, :, 0, :], in0=xc[:, :, 0, :], in1=xs[:, :, 1, :], op=ALU.subtract),
             r=[rt], w=out_tiles)
        k.op("dve", lambda e: e.tensor_tensor(out=ov[:, :, 1, :], in0=xc[:, :, 1, :], in1=xs[:, :, 0, :], op=ALU.add),
             r=[rt], w=out_tiles)

    def norm_T(x_ap, x_tiles, hbr=None, hTr=None, evac_eng="act"):
        hbr = hbr or hbring
        hTr = hTr or hTring
        rs, rs_s = rstd_of(x_ap, D, x_tiles)
        hb = hbr.next()
        k.op("dve", lambda e: e.tensor_scalar(out=hb.t[:], in0=x_ap, scalar1=rs, scalar2=None, op0=ALU.mult),
             r=x_tiles + [rs_s], w=[hb])
        hT = hTr.next()
        transposes(hT, hb.t[:], 8, 128, [hb], [hT], evac_eng=evac_eng)
        return hT

    tabb = rope_tables_bulk("tabb", posb_f, NBLK)
    if do_p1a:
        sbufs = {}
        kvs_of = {}
        hbk_r = k.ring("hbk", [128, D], BF16, 3)
        hTk_r = k.ring("hTk", [128, 8, 128], BF16, 3)

        xk6 = k.ring("xk6", [128, D], F32, 6)
        sA = {}

        def stageA1(kb):
            sbi, j = kb // 8, kb % 8
            if j == 0:
                kcs, krs, vcs = kcst.next(), krst.next(), vcst.next()
                k.op("pool", lambda e: e.memset(vcs.t[:, :, 128:129], 1.0), w=[vcs])
                sbufs[sbi] = (kcs, krs, vcs)
            xt = xk6.next()
            k.dma(xt.t[:], xb[kb * 128:(kb + 1) * 128, :], xt, w=[xt])
            rs, rs_s = rstd_of(xt.t[:], D, [xt])
            sA[kb] = (xt, rs, rs_s)

        def stageA2(kb):
            xt, rs, rs_s = sA.pop(kb)
            hb = hbk_r.next()
            k.op("dve", lambda e: e.tensor_scalar(out=hb.t[:], in0=xt.t[:], scalar1=rs, scalar2=None, op0=ALU.mult),
                 r=[xt, rs_s], w=[hb])
            hT = hTk_r.next()
            transposes(hT, hb.t[:], 8, 128, [hb], [hT])
            pk = pb[3 + kb % 3]
            for c in range(8):
                k.mm(pk.t[:, 0:192], hT.t[:, c, :], wkvl.t[:, c, :], c == 0, c == 7, r=[hT, wkvl], w=[pk])
            kvs = kvsring.next()
            k.op("act", lambda e: e.copy(out=kvs.t[:], in_=pk.t[:, 0:192]), r=[pk], w=[kvs])
            kvs_of[kb] = kvs

        sB = {}

        def stageB1(kb):
            kvs = kvs_of[kb]
            rs, rs_s = rstd_of(kvs.t[:, 0:128], 128, [kvs])
            krb = krb_ring.next()
            apply_rope(krb.t[:], kvs.t[:, 128:192], 1, tabb.t[:, 0, kb, :], tabb.t[:, 1, kb, :], tabb, [kvs], [krb])
            sB[kb] = (rs, rs_s, krb)

        def stageB2(kb):
            sbi, j = kb // 8, kb % 8
            kcs, krs, vcs = sbufs[sbi]
            kvs = kvs_of.pop(kb)
            rs, rs_s, krb = sB.pop(kb)
            k.op("dve", lambda e: e.tensor_scalar(out=vcs.t[:, j, 0:128], in0=kvs.t[:, 0:128],
                                                  scalar1=rs, scalar2=None, op0=ALU.mult), r=[kvs, rs_s], w=[vcs])
            pt = pb[2]
            ptb = pt.t[:].bitcast(BF16)
            k.tr(ptb[:, 0:128], vcs.t[:, j, 0:128], identb.t[:], r=[vcs, identb], w=[pt])
            k.tr(ptb[0:64, 128:256], krb.t[:], identb.t[:], r=[krb, identb], w=[pt])
            k.op("act", lambda e: e.copy(out=kcs.t[:, j * 128:(j + 1) * 128], in_=ptb[:, 0:128]), r=[pt], w=[kcs])
            k.op("act", lambda e: e.copy(out=krs.t[:, j * 128:(j + 1) * 128], in_=ptb[0:64, 128:256]), r=[pt], w=[krs])
            if j == 7:
                k.dma(kc_s[sbi], kcs.t[:], kcs, r=[kcs])
                k.dma(kr_s[sbi], krs.t[:], krs, r=[krs])
                k.dma(vc_s[sbi], vcs.t[:].rearrange("p a b -> p (a b)"), vcs, r=[vcs])

        nkb = n_sb * 8
        for t_ in range(nkb + 3):
            if t_ < nkb:
                stageA1(t_)
            if 0 <= t_ - 1 < nkb:
                stageA2(t_ - 1)
            if 0 <= t_ - 2 < nkb:
                stageB1(t_ - 2)
            if 0 <= t_ - 3 < nkb:
                stageB2(t_ - 3)
                if p0g is not None and (t_ % 2 == 0 or not do_p1b):
                    next(p0g, None)
        kcs, krs, vcs = sbufs[n_sb - 1]
    if p0g is not None and not do_p1b:
        for _ in p0g:
            pass

    if dbg == "p1a":
        dt_ = k.sb("dbgt", [128, 4096], F32)
        k.op("dve", lambda e: e.memset(dt_.t[:], 0.0), w=[dt_])
        k.op("dve", lambda e: e.tensor_copy(out=dt_.t[:, 0:1024], in_=kcs.t[:]), r=[kcs], w=[dt_])
        k.op("dve", lambda e: e.tensor_copy(out=dt_.t[0:64, 1024:2048], in_=krs.t[:]), r=[krs], w=[dt_])
        k.op("dve", lambda e: e.tensor_copy(out=dt_.t[:, 2048:2048 + 1032], in_=vcs.t[:].rearrange("p a b -> p (a b)")),
             r=[vcs], w=[dt_])
        k.dma(dbg_d, dt_.t[:], dt_, r=[dt_])


    k.pop_scope()
    k.push_scope()
    if do_p1b:
        castring[0] = k.ring("castB", [128, 2048], BF16, 2)
        win = k.sb("win", [128, 8, 1216], BF16)
        load_cast(win, w_in_d, 8, 1216, G_MIX)
        wqb = k.sb("wqb", [128, 2, 768], BF16)
        load_cast(wqb, w_qb_d, 2, 768, G_QA)
        wout = k.sb("wout", [128, 8, 1024], BF16)
        load_cast(wout, w_out_d, 8, 1024, G_OUT)
        wcq = k.sb("wcq", [128, 8, 512], BF16)
        load_cast(wcq, w_cq_d, 8, 512, G_CROSS)
        wco = k.sb("wco", [128, 4, 1024], BF16)
        load_cast(wco, w_co_d, 4, 1024, None)
        lvl = cfg.get("lvl", 99)
        if lvl < 1:
            raise StopBuild(P)
        kvab = k.sb("kvab", [128, 128], F32)
        k.dma(kvab.t[:], kvarow_d[0].partition_broadcast(128), kvab, w=[kvab])
        wukT = k.sb("wukT", [128, 4, 128], BF16)
        sg = stage.next()
        k.dma(sg.t[:, 0:512], w_ukT_d, sg, w=[sg])
        k.op("dve", lambda e, sg=sg: e.tensor_tensor(out=wukT.t[:], in0=sg.t[:, 0:512].rearrange("p (h r) -> p h r", r=128),
                                                     in1=kvab.t[:].unsqueeze(1).to_broadcast([128, 4, 128]), op=ALU.mult),
             r=[sg, kvab], w=[wukT])
        wuv = k.sb("wuv", [128, 4, 128], BF16)
        sg = stage.next()
        k.dma(sg.t[:, 0:512], w_uv_d, sg, w=[sg])
        k.op("dve", lambda e, sg=sg: e.tensor_scalar(out=wuv.t[:].rearrange("p h d -> p (h d)"), in0=sg.t[:, 0:512],
                                                     scalar1=gcols.t[:, G_KVA:G_KVA + 1], scalar2=None, op0=ALU.mult),
             r=[sg, gcols], w=[wuv])
        if lvl < 2:
            raise StopBuild(P)
        esink = k.sb("esink", [128, 8], F32)
        k.dma(esink.t[:], sinks_d[0].partition_broadcast(128), esink, w=[esink])
        k.act(esink.t[:], esink.t[:], AF.Exp, r=[esink], w=[esink])
        if lvl < 3:
            raise StopBuild(P)
        kaug = k.sb("kaug", [2, 2, 128], BF16)
        qaug = k.sb("qaug", [2, 2, 512], BF16)
        k.dma(kaug.t[:].rearrange("p a b -> p (a b)"), kaug_d, kaug, w=[kaug])
        k.dma(qaug.t[:].rearrange("p a b -> p (a b)"), qaug_d, qaug, w=[qaug])
        poso_i = k.sb("poso_i", [128, NOWN], I32)
        poso_f = k.sb("poso_f", [128, NOWN], F32)
        k.dma(poso_i.t[:], poso, poso_i, w=[poso_i])
        k.op("dve", lambda e: e.tensor_copy(out=poso_f.t[:], in_=poso_i.t[:]), r=[poso_i], w=[poso_f])
        tabo = k.sb("tabo", [128, 2, NOWN, 32], F32)

        if lvl < 4:
            raise StopBuild(P)
        KxT = k.sb("KxT", [128, 4, 256], BF16)
        Vx = k.sb("Vx", [128, 2, 4, 129], BF16)
        k.op("pool", lambda e: e.memset(Vx.t[:, :, :, 128:129], 1.0), w=[Vx])
        main_scope = k.st
        k.st = ExitStack()
        rope_tables_bulk("tabo", poso_f, NOWN, tab=tabo)
        memT = k.sb("memT", [128, 8, 256], BF16)
        wck = k.sb("wck", [128, 8, 512], BF16)
        wcv = k.sb("wcv", [128, 8, 512], BF16)
        load_cast(wck, w_ck_d, 8, 512, G_MEM)
        load_cast(wcv, w_cv_d, 8, 512, G_MEM)
        for mc in range(2):
            xt = xring.next()
            k.dma(xt.t[:], memb[mc * 128:(mc + 1) * 128, :], xt, w=[xt])
            rs, rs_s = rstd_of(xt.t[:], D, [xt])
            hb = hbring.next()
            k.op("dve", lambda e, hb=hb, xt=xt, rs=rs: e.tensor_scalar(out=hb.t[:], in0=xt.t[:], scalar1=rs, scalar2=None,
                                                                       op0=ALU.mult), r=[xt, rs_s], w=[hb])
            transposes(None, hb.t[:], 8, 128, [hb], [memT], dst_ap=memT.t[:, :, mc * 128:(mc + 1) * 128])
        for h in range(4):
            for c in range(8):
                k.mm(pb[0].t[:, 0:256], wck.t[:, c, h * 128:(h + 1) * 128], memT.t[:, c, :], c == 0, c == 7,
                     r=[wck, memT], w=[pb[0]])
            k.op("act", lambda e, h=h: e.copy(out=KxT.t[:, h, :], in_=pb[0].t[:, 0:256]), r=[pb[0]], w=[KxT])
        for mc in range(2):
            for c in range(8):
                k.mm(pb[1].t[:, 0:512], memT.t[:, c, mc * 128:(mc + 1) * 128], wcv.t[:, c, :], c == 0, c == 7,
                     r=[wcv, memT], w=[pb[1]])
            k.op("act", lambda e, mc=mc: e.copy(out=Vx.t[:, mc, :, 0:128],
                                                in_=pb[1].t[:, 0:512].rearrange("p (h d) -> p h d", d=128)),
                 r=[pb[1]], w=[Vx])

        k.S.barrier()
        k.st.close()
        k.st = main_scope
        if lvl < 5:
            raise StopBuild(P)
        prj = k.ring("prj", [128, 1216], F32, 1)
        qsb_r = k.ring("qsb", [128, 512], BF16, 2)
        QsT_r = k.ring("QsT", [64, 8, 128], BF16, 2)
        KsT_r = k.ring("KsT", [64, 2, 2, 128], BF16, 2)
        Vs_r = k.ring("Vs", [128, 2, 2, 65], BF16, 2)
        ksb_r = k.ring("ksb", [128, 2, 256], BF16, 2)
        cqn_r = k.ring("cqn", [128, 256], BF16, 2)
        cqnT_r = k.ring("cqnT", [128, 2, 128], BF16, 2)
        qnT_r = k.ring("qnT", [128, 4, 128], BF16, 2)
        QT_r = k.ring("QT", [128, 512], BF16, 2)
        qrb_r = k.ring("qrb", [128, 256], BF16, 2)
        QrT_r = k.ring("QrT", [128, 4, 128], BF16, 2)
        PT_r = k.ring("PT", [128, 512], BF16, 3)
        PS_r = k.ring("PS", [128, 512], BF16, 3)
        mk_r = k.ring("mk", [128, 4, 128], BF16, 2)
        smk_r = k.ring("smk", [128, 2, 128], BF16, 2)
        kcc_r = k.ring("kcc", [128, 1024], BF16, 3)
        krc_r = k.ring("krc", [128, 1024], BF16, 3)
        vcc_r = k.ring("vcc", [128, 8, 129], BF16, 3)
        den_r = k.ring("den", [128, 8], F32, 4)
        olat_r = k.ring("olat", [128, 512], BF16, 2)
        olatT_r = k.ring("olatT", [128, 4, 128], BF16, 2)
        of32_r = k.ring("of32", [128, 512], F32, 2)
        mix_r = k.ring("mix", [128, 1024], BF16, 2)
        x1_r = k.ring("x1", [128, D], F32, 2)
        QxT_r = k.ring("QxT", [128, 4, 128], BF16, 2)
        for bq in QrT_r.bufs:
            k.op("pool", lambda e, bq=bq: e.memset(bq.t[64:128, :, :], 0.0), w=[bq])
        for bq in krc_r.bufs:
            k.op("pool", lambda e, bq=bq: e.memset(bq.t[64:128, :], 0.0), w=[bq])
        xo_r = Ring(xring.bufs[0:2])
        xp_r = Ring([xring.bufs[2], k.sb("xp1", [128, D], F32)])

        def norm_bank(pbuf, nhb, dv, extra_ap, extra_tiles, out_ap, out_tiles):
            dn = den_r.next()
            pv = pbuf.t[:, 0:nhb * (dv + 1)].rearrange("p (h d) -> p h d", d=dv + 1)
            den_src = pv[:, :, dv:dv + 1].rearrange("p h o -> p (h o)")
            if extra_ap is not None:
                k.op("dve", lambda e: e.tensor_tensor(out=dn.t[:, 0:nhb], in0=den_src, in1=extra_ap, op=ALU.add),
                     r=[pbuf] + extra_tiles, w=[dn])
            else:
                k.op("dve", lambda e: e.tensor_copy(out=dn.t[:, 0:nhb], in_=den_src), r=[pbuf], w=[dn])
            k.op("dve", lambda e: e.reciprocal(out=dn.t[:, 0:nhb], in_=dn.t[:, 0:nhb]), r=[dn], w=[dn])
            for hh in range(nhb):
                k.op("dve", lambda e, hh=hh: e.tensor_scalar(out=out_ap[:, hh * dv:(hh + 1) * dv], in0=pv[:, hh, 0:dv],
                                                            scalar1=dn.t[:, hh:hh + 1], scalar2=None, op0=ALU.mult),
                     r=[pbuf, dn], w=out_tiles)

        SC_MLA = float(192.0 ** -0.5)
        SC_SWA = 0.125
        SC_X = float(128.0 ** -0.5)
        st_ = {}

        def pre(ki):
            S = {}
            st_[ki] = S
            xt = xo_r.next()
            S["xt"] = xt
            k.dma(xt.t[:], xown[ki * 128:(ki + 1) * 128, :], xt, w=[xt], q="act")
            hT = norm_T(xt.t[:], [xt], evac_eng="dve")
            yield
            pj = prj.next()
            for (c0, c1, pbk) in ((0, 512, pb[0]), (512, 1024, pb[1]), (1024, 1216, pb[2])):
                for c in range(8):
                    k.mm(pbk.t[:, 0:c1 - c0], hT.t[:, c, :], win.t[:, c, c0:c1], c == 0, c == 7, r=[hT, win], w=[pbk])
                k.op("dve", lambda e, pbk=pbk, c0=c0, c1=c1: e.tensor_copy(out=pj.t[:, c0:c1], in_=pbk.t[:, 0:c1 - c0]),
                     r=[pbk], w=[pj])
                yield
            xp = xp_r.next()
            k.dma(xp.t[:], xprev[ki * 128:(ki + 1) * 128, :], xp, w=[xp], q="act")
            hTp = norm_T(xp.t[:], [xp], evac_eng="dve")
            yield
            for c in range(8):
                k.mm(pb[2].t[:, 0:256], hTp.t[:, c, :], win.t[:, c, 960:1216], c == 0, c == 7, r=[hTp, win], w=[pb[2]])
            ksb = ksb_r.next()
            k.op("dve", lambda e: e.tensor_copy(out=ksb.t[:, 0, :], in_=pb[2].t[:, 0:256]), r=[pb[2]], w=[ksb])
            k.op("dve", lambda e: e.tensor_copy(out=ksb.t[:, 1, :], in_=pj.t[:, 960:1216]), r=[pj], w=[ksb])
            yield
            KsT = KsT_r.next()
            Vs = Vs_r.next()
            k.op("dve", lambda e: e.memset(Vs.t[:, :, :, 64:65], 1.0), w=[Vs])
            for slot in range(2):
                for kvh in range(2):
                    k.tr(ps_tp.t[0:64, (slot * 2 + kvh) * 128:(slot * 2 + kvh + 1) * 128], ksb.t[:, slot, kvh * 64:(kvh + 1) * 64],
                         identb.t[:], r=[ksb, identb], w=[ps_tp])
                k.op("dve", lambda e, slot=slot: e.tensor_copy(
                    out=Vs.t[:, slot, :, 0:64], in_=ksb.t[:, slot, 128:256].rearrange("p (h d) -> p h d", d=64)),
                    r=[ksb], w=[Vs])
            k.op("dve", lambda e: e.tensor_copy(out=KsT.t[:].rearrange("p a b t -> p (a b t)"), in_=ps_tp.t[0:64, 0:512]),
                 r=[ps_tp], w=[KsT])
            yield
            qsb = qsb_r.next()
            k.op("dve", lambda e: e.tensor_copy(out=qsb.t[:], in_=pj.t[:, 448:960]), r=[pj], w=[qsb])
            QsT = QsT_r.next()
            transposes(QsT, qsb.t[:], 8, 64, [qsb], [QsT], evac_eng="dve")
            smk = smk_r.next()
            k.dma(smk.t[:].rearrange("p a b -> p (a b)"), smask_d[ki], smk, w=[smk], q="act")
            yield
            ob = of32_r.next()
            for kvh in range(2):
                for slot in range(2):
                    psc = pb[slot % 2]
                    k.mm(psc.t[:], KsT.t[:, slot, kvh, :], QsT.t[:, kvh * 4:(kvh + 1) * 4, :].rearrange("p g t -> p (g t)"),
                         True, False, r=[KsT, QsT], w=[psc])
                    k.mm(psc.t[:], kaug.t[:, slot, :], qaug.t[:, kvh, :], False, True, r=[kaug, qaug], w=[psc])
                    PT = PS_r.next()
                    k.act(PT.t[:], psc.t[:], AF.Exp, r=[psc], w=[PT], scale=SC_SWA)
                    k.op("dve", lambda e, PT=PT, slot=slot: e.tensor_tensor(
                        out=PT.t[:].rearrange("p (g t) -> p g t", t=128), in0=PT.t[:].rearrange("p (g t) -> p g t", t=128),
                        in1=smk.t[:, slot, :].unsqueeze(1).to_broadcast([128, 4, 128]), op=ALU.mult), r=[PT, smk], w=[PT])
                    yield
                    for g in range(4):
                        k.mm(pb[2].t[:, g * 65:(g + 1) * 65], PT.t[:, g * 128:(g + 1) * 128], Vs.t[:, slot, kvh, :],
                             slot == 0 and g == 0, slot == 1, r=[PT, Vs], w=[pb[2]], skip=True)
                    yield
                norm_bank(pb[2], 4, 64, esink.t[:, kvh * 4:(kvh + 1) * 4], [esink], ob.t[:, kvh * 256:(kvh + 1) * 256], [ob])
                yield
            mix = mix_r.next()
            S["mix"] = mix
            rs, rs_s = rstd_of(ob.t[:], 512, [ob])
            k.op("dve", lambda e: e.tensor_scalar(out=mix.t[:, 512:1024], in0=ob.t[:], scalar1=rs,
                                                  scalar2=None, op0=ALU.mult), r=[ob, rs_s], w=[mix])
            yield
            rs2, rs2_s = rstd_of(pj.t[:, 0:256], 256, [pj])
            cqn = cqn_r.next()
            k.op("dve", lambda e: e.tensor_scalar(out=cqn.t[:], in0=pj.t[:, 0:256], scalar1=rs2,
                                                  scalar2=None, op0=ALU.mult), r=[pj, rs2_s], w=[cqn])
            cqnT = cqnT_r.next()
            transposes(cqnT, cqn.t[:], 2, 128, [cqn], [cqnT], evac_eng="dve")
            yield
            for h in range(4):
                for c in range(2):
                    k.mm(pb[0].t[:, h * 128:(h + 1) * 128], wqb.t[:, c, h * 128:(h + 1) * 128], cqnT.t[:, c, :], c == 0, c == 1,
                         r=[wqb, cqnT], w=[pb[0]])
            qnT = qnT_r.next()
            k.op("dve", lambda e: e.tensor_copy(out=qnT.t[:].rearrange("p h t -> p (h t)"), in_=pb[0].t[:]), r=[pb[0]], w=[qnT])
            yield
            for h in range(4):
                k.mm(pb[1].t[:, h * 128:(h + 1) * 128], wukT.t[:, h, :], qnT.t[:, h, :], True, True, r=[wukT, qnT], w=[pb[1]])
            QT = QT_r.next()
            S["QT"] = QT
            k.op("dve", lambda e: e.tensor_copy(out=QT.t[:], in_=pb[1].t[:]), r=[pb[1]], w=[QT])
            yield
            for c in range(2):
                k.mm(pb[2].t[:, 0:256], cqnT.t[:, c, :], wqb.t[:, c, 512:768], c == 0, c == 1, r=[wqb, cqnT], w=[pb[2]])
            qrb = qrb_r.next()
            apply_rope(qrb.t[:], pb[2].t[:, 0:256], 4, tabo.t[:, 0, ki, :], tabo.t[:, 1, ki, :], tabo, [pb[2]], [qrb])
            yield
            QrT = QrT_r.next()
            S["QrT"] = QrT
            transposes(QrT, qrb.t[:], 4, 64, [qrb], [QrT], evac_eng="dve")
            mk = mk_r.next()
            S["mk"] = mk
            k.dma(mk.t[:].rearrange("p a b -> p (a b)"), mmask_d[ki], mk, w=[mk], q="act")
            yield

        def mla(ki, gens):
            S = st_[ki]
            QT, QrT, mk = S["QT"], S["QrT"], S["mk"]
            L = n_keyblocks(ki)
            nch = (L + 7) // 8
            chunks = []
            po = [pb[5], pb[6]]

            def load_chunk(ci):
                kcc, krc, vcc = kcc_r.next(), krc_r.next(), vcc_r.next()
                nb = min(8, L - ci * 8)
                KQ = cfg.get("kvq", "act")
                k.dma(kcc.t[:, 0:nb * 128], kc_s[ci, :, 0:nb * 128], kcc, w=[kcc], q=KQ)
                k.dma(krc.t[0:64, 0:nb * 128], kr_s[ci, :, 0:nb * 128], krc, w=[krc], q=KQ)
                k.dma(vcc.t[:, 0:nb, :].rearrange("p a b -> p (a b)"), vc_s[ci, :, 0:nb * 129], vcc, w=[vcc], q=KQ)
                return (kcc, krc, vcc)

            def scores(j, psc):
                kcc, krc, vcc = chunks[j // 8]
                jj = j % 8
                k.mm(psc.t[:], kcc.t[:, jj * 128:(jj + 1) * 128], QT.t[:], True, False, r=[kcc, QT], w=[psc])
                k.mm(psc.t[:], krc.t[:, jj * 128:(jj + 1) * 128], QrT.t[:].rearrange("p h t -> p (h t)"), False, True,
                     r=[krc, QrT], w=[psc])

            def step_others():
                for gx in gens:
                    try:
                        next(gx)
                        return
                    except StopIteration:
                        continue

            chunks.append(load_chunk(0))
            if nch > 1:
                chunks.append(load_chunk(1))
            scores(0, pb[3])
            for j in range(L):
                if j + 1 < L:
                    if (j + 1) % 8 == 0 and (j + 1) // 8 + 1 < nch:
                        chunks.append(load_chunk((j + 1) // 8 + 1))
                    scores(j + 1, pb[3 + ((j + 1) % 2)])
                psc = pb[3 + (j % 2)]
                PT = PT_r.next()
                k.act(PT.t[:], psc.t[:], AF.Exp, r=[psc], w=[PT], scale=SC_MLA)
                if j >= L - 4:
                    jj = j - (L - 4)
                    k.op("dve", lambda e, PT=PT, jj=jj: e.tensor_tensor(
                        out=PT.t[:].rearrange("p (g t) -> p g t", t=128), in0=PT.t[:].rearrange("p (g t) -> p g t", t=128),
                        in1=mk.t[:, jj, :].unsqueeze(1).to_broadcast([128, 4, 128]), op=ALU.mult), r=[PT, mk], w=[PT])
                vcc = chunks[j // 8][2]
                for h in range(4):
                    pbo = po[h // 2]
                    hh = h % 2
                    k.mm(pbo.t[:, hh * 129:(hh + 1) * 129], PT.t[:, h * 128:(h + 1) * 128], vcc.t[:, j % 8, :],
                         j == 0 and hh == 0, j == L - 1, r=[PT, vcc], w=[pbo], skip=True)
                step_others()
            olat = olat_r.next()
            S["olat"] = olat
            norm_bank(pb[5], 2, 128, None, [], olat.t[:, 0:256], [olat])
            norm_bank(pb[6], 2, 128, None, [], olat.t[:, 256:512], [olat])

        def post(ki):
            S = st_[ki]
            olat, mix, xt = S["olat"], S["mix"], S["xt"]
            olatT = olatT_r.next()
            transposes(olatT, olat.t[:], 4, 128, [olat], [olatT], evac_eng="dve")
            yield
            for h in range(4):
                k.mm(pb[0].t[:, h * 128:(h + 1) * 128], olatT.t[:, h, :], wuv.t[:, h, :], True, True, r=[olatT, wuv], w=[pb[0]])
            oa = of32_r.next()
            k.op("dve", lambda e: e.tensor_copy(out=oa.t[:], in_=pb[0].t[:]), r=[pb[0]], w=[oa])
            yield
            rs, rs_s = rstd_of(oa.t[:], 512, [oa])
            k.op("dve", lambda e: e.tensor_scalar(out=mix.t[:, 0:512], in0=oa.t[:], scalar1=rs,
                                                  scalar2=None, op0=ALU.mult), r=[oa, rs_s], w=[mix])
            mixT = hTring.next()
            transposes(mixT, mix.t[:], 8, 128, [mix], [mixT], evac_eng="dve")
            yield
            x1 = x1_r.next()
            for half in range(2):
                for c in range(8):
                    k.mm(pb[half].t[:], mixT.t[:, c, :], wout.t[:, c, half * 512:(half + 1) * 512], c == 0, c == 7,
                         r=[mixT, wout], w=[pb[half]])
                k.op("dve", lambda e, half=half: e.tensor_tensor(
                    out=x1.t[:, half * 512:(half + 1) * 512], in0=pb[half].t[:], in1=xt.t[:, half * 512:(half + 1) * 512],
                    op=ALU.add), r=[pb[half], xt], w=[x1])
                yield
            if dbg == "p1b" and ki == n_own - 1:
                x1c = k.sb("x1c", [128, D], F32)
                S["x1c"] = x1c
                k.op("pool", lambda e: e.tensor_copy(out=x1c.t[:], in_=x1.t[:]), r=[x1], w=[x1c])
            h2T = norm_T(x1.t[:], [x1], evac_eng="dve")
            yield
            for h in range(4):
                for c in range(8):
                    k.mm(pb[2].t[:, h * 128:(h + 1) * 128], wcq.t[:, c, h * 128:(h + 1) * 128], h2T.t[:, c, :], c == 0, c == 7,
                         r=[wcq, h2T], w=[pb[2]])
                if h % 2 == 1:
                    yield
            QxT = QxT_r.next()
            k.op("dve", lambda e: e.tensor_copy(out=QxT.t[:].rearrange("p h t -> p (h t)"), in_=pb[2].t[:]), r=[pb[2]], w=[QxT])
            yield
            PTs = []
            for mc in range(2):
                psc = pb[mc]
                for h in range(4):
                    k.mm(psc.t[:, h * 128:(h + 1) * 128], KxT.t[:, h, mc * 128:(mc + 1) * 128], QxT.t[:, h, :], True, True,
                         r=[KxT, QxT], w=[psc])
                PT = PS_r.next()
                k.act(PT.t[:], psc.t[:], AF.Exp, r=[psc], w=[PT], scale=SC_X)
                PTs.append(PT)
                yield
            ox = olat_r.next()
            for hp in range(2):
                for mc in range(2):
                    for hh in range(2):
                        h = hp * 2 + hh
                        k.mm(pb[2].t[:, hh * 129:(hh + 1) * 129], PTs[mc].t[:, h * 128:(h + 1) * 128], Vx.t[:, mc, h, :],
                             mc == 0 and hh == 0, mc == 1, r=[PTs[mc], Vx], w=[pb[2]], skip=True)
                norm_bank(pb[2], 2, 128, None, [], ox.t[:, hp * 256:(hp + 1) * 256], [ox])
                yield
            oxT = olatT_r.next()
            transposes(oxT, ox.t[:], 4, 128, [ox], [oxT], evac_eng="dve")
            yield
            x2 = x1
            for half in range(2):
                for c in range(4):
                    k.mm(pb[half].t[:], oxT.t[:, c, :], wco.t[:, c, half * 512:(half + 1) * 512], c == 0, c == 3,
                         r=[oxT, wco], w=[pb[half]])
                k.op("dve", lambda e, half=half: e.tensor_tensor(
                    out=x2.t[:, half * 512:(half + 1) * 512], in0=pb[half].t[:], in1=x1.t[:, half * 512:(half + 1) * 512],
                    op=ALU.add), r=[pb[half], x1], w=[x2])
                yield
            k.dma(x2_s[ki * 128:(ki + 1) * 128, :], x2.t[:], x2, r=[x2])
            if dbg == "p1b" and ki == n_own - 1:
                dt_ = k.sb("dbgt", [128, 1024], F32)
                for (c0, srcb, wd) in ((0, mix, 1024), (1024, S["x1c"], 1024), (2048, x2, 1024), (3072, olat, 512)):
                    k.op("dve", lambda e, srcb=srcb, wd=wd: e.tensor_copy(out=dt_.t[:, 0:wd], in_=srcb.t[:]), r=[srcb], w=[dt_])
                    k.dma(dbg_d[:, c0:c0 + wd], dt_.t[:, 0:wd], dt_, r=[dt_], w=[])
            st_.pop(ki)

        stop = cfg.get("p1b_stop")
        n_run = 0 if stop == "setup" else n_own
        if n_run > 0:
            for _ in pre(0):
                pass
        prev_post = None
        for ki in range(n_run):
            gens = []
            if prev_post is not None:
                gens.append(prev_post)
            nxt = pre(ki + 1) if ki + 1 < n_run else None
            if nxt is not None:
                gens.append(nxt)
            mla(ki, gens + ([p0g] if p0g is not None else []))
            for gx in gens:
                for _ in gx:
                    pass
            prev_post = post(ki)
        if prev_post is not None:
            for _ in prev_post:
                pass
        if p0g is not None:
            for _ in p0g:
                pass

    k.pop_scope()


    k.push_scope()
    if do_p2:
        wq_s = P.dscr("wq_s", [8, 128, 2048], BF16)
        gfin = k.sb("gfin", [128, D], F32)
        k.dma(gfin.t[:], gfin_d[0].partition_broadcast(128), gfin, w=[gfin])
        iota128 = k.sb("iota128", [128, 128], F32)
        iota16 = k.sb("iota16", [128, 16], F32)
        k.dma(iota128.t[:], iota128_d, iota128, w=[iota128])
        k.dma(iota16.t[:], iota16_d, iota16, w=[iota16])
        keysT = k.sb("keysT", [128, 16, 128], BF16)
        sg = stage.next()
        k.dma(sg.t[:], keysT_d, sg, w=[sg])
        k.op("dve", lambda e, sg=sg: e.tensor_copy(out=keysT.t[:].rearrange("p a b -> p (a b)"), in_=sg.t[:]), r=[sg], w=[keysT])
        wq_r = k.ring("wqc", [128, 8, 256], BF16, 2)
        wpq_v = w_pq_d.rearrange("(c p) n -> p c n", p=128)
        for fp in range(8):
            sg = stage.next()
            k.dma(sg.t[:].rearrange("p (c n) -> p c n", n=256), wpq_v[:, :, fp * 256:(fp + 1) * 256], sg, w=[sg])
            wc = wq_r.next()
            for c in range(8):
                eng = ("dve", "pool")[c % 2]
                k.op(eng, lambda e, wc=wc, sg=sg, c=c: e.tensor_scalar(out=wc.t[:, c, :], in0=sg.t[:, c * 256:(c + 1) * 256],
                                                                      scalar1=gcols.t[:, G_FFN + c:G_FFN + c + 1], scalar2=1.0,
                                                                      op0=ALU.mult, op1=ALU.mult), r=[sg, gcols], w=[wc])
            k.dma(wq_s[fp], wc.t[:].rearrange("p c n -> p (c n)"), wc, r=[wc])
        k.S.barrier()

        GT = k.sb("GT", [128, 128, 256], BF16)
        hfT2 = [k.sb("hfT%d" % i, [128, 8, 256], BF16) for i in range(2)]
        trip2 = [k.sb("trip%d" % i, [128, 3, 256], F32) for i in range(2)]
        x2r = Ring(list(xring.bufs) + [k.sb("xt3", [128, D], F32)])
        qT = k.sb("qT", [128, 16, 256], BF16)
        sc = k.sb("sc", [128, 2048], F32)
        sc_s = [T("sc%d" % i) for i in range(16)]
        tops = k.sb("tops", [128, 16, 16], F32)
        tops_s = [T("tops%d" % i) for i in range(16)]
        idxu = k.sb("idxu", [128, 16, 16], U32)
        idxu_s = [T("idxu%d" % i) for i in range(16)]
        idxf = k.sb("idxf", [128, 16, 16], F32)
        big8 = k.sb("big8", [128, 2048], F32)
        cand_s = [T("cand%d" % i) for i in range(8)]
        best = k.sb("best", [128, 8, 16], F32)
        best_s = [T("best%d" % i) for i in range(8)]
        posu = k.sb("posu", [128, 8, 16], U32)
        posu_s = [T("posu%d" % i) for i in range(8)]
        apu = k.sb("apu", [128, 8, 16], U32)
        bpu = k.sb("bpu", [128, 8, 16], U32)
        apf = k.sb("apf", [128, 8, 16], F32)
        bpf = k.sb("bpf", [128, 8, 16], F32)
        sel = k.sb("sel", [128, 3, 128], F32)
        exb = k.sb("exb", [128, 8, 16], F32)
        sm8 = k.sb("sm8", [128, 8], F32)
        Pm_r = k.ring("Pm", [128, 8, 64], BF16, 3)
        Qm_r = k.ring("Qm", [128, 8, 128], BF16, 3)
        ubn = [k.sb("ub%d" % i, [128, 4096], BF16) for i in range(2)]
        ub_r = Ring(ubn)

        class _V:
            pass
        vbn = []
        for sgb in stage.bufs:
            o = _V()
            o.t = sgb.t[:].bitcast(BF16)
            o.s = sgb.s
            vbn.append(o)
        vb_r = Ring(vbn)
        gl_r = k.ring("gl", [128, 256], BF16, 4)
        ga_r = k.ring("ga", [128, 256], BF16, 4)
        xts_of = {}
        pq = pb[0]

        def phase1(g):
            bs = g % 2
            hfT, trip = hfT2[bs], trip2[bs]
            xts = []
            xts_of[g] = xts
            for blk in range(2):
                xt = x2r.next()
                row0 = (g * 2 + blk) * 128
                k.dma(xt.t[:], x2_s[row0:row0 + 128, :], xt, w=[xt])
                xts.append(xt)
                rs, rs_s = rstd_of(xt.t[:], D, [xt])
                hb = hbring.next()
                k.op("dve", lambda e, hb=hb, xt=xt, rs=rs: e.tensor_scalar(out=hb.t[:], in0=xt.t[:], scalar1=rs, scalar2=None,
                                                                           op0=ALU.mult), r=[xt, rs_s], w=[hb])
                yield
                transposes(None, hb.t[:], 8, 128, [hb], [hfT], dst_ap=hfT.t[:, :, blk * 128:(blk + 1) * 128], pst=tp0)
                yield
            for fp in range(8):
                wc = wq_r.next()
                k.dma(wc.t[:].rearrange("p c n -> p (c n)"), wq_s[fp], wc, w=[wc])
                for f2 in range(2):
                    for c in range(8):
                        k.mm(pq.t[:, f2 * 256:(f2 + 1) * 256], wc.t[:, c, f2 * 128:(f2 + 1) * 128], hfT.t[:, c, :], c == 0, c == 7,
                             r=[wc, hfT], w=[pq])
                k.op("act", lambda e, fp=fp: e.copy(out=qT.t[:, fp * 2:fp * 2 + 2, :].rearrange("p a b -> p (a b)"),
                                                   in_=pq.t[:]), r=[pq], w=[qT])
                yield
            for blk in range(2):
                for qd in range(4):
                    for i4 in range(4):
                        hc = qd * 4 + i4
                        k.mm(pq.t[:, i4 * 128:(i4 + 1) * 128], qT.t[:, hc, blk * 128:(blk + 1) * 128], keysT.t[:, hc, :], True, True,
                             r=[qT, keysT], w=[pq])
                    k.op("act", lambda e, qd=qd: e.copy(out=sc.t[:, qd * 512:(qd + 1) * 512], in_=pq.t[:]),
                         r=[pq], w=sc_s[qd * 4:qd * 4 + 4])
                    yield

                def grp(i):
                    return sc.t[:, i * 128:(i + 1) * 128]
                for i in range(16):
                    k.op("dve", lambda e, i=i: e.max(out=tops.t[:, i, 0:8], in_=grp(i)), r=[sc_s[i]], w=[tops_s[i]])
                yield
                for i in range(16):
                    k.op("dve", lambda e, i=i: e.max_index(out=idxu.t[:, i, 0:8], in_max=tops.t[:, i, 0:8], in_values=grp(i)),
                         r=[sc_s[i], tops_s[i]], w=[idxu_s[i]])
                yield
                for i in range(16):
                    k.op("dve", lambda e, i=i: e.match_replace(out=grp(i), in_to_replace=tops.t[:, i, 0:8], in_values=grp(i),
                                                              imm_value=NEG), r=[tops_s[i], sc_s[i]], w=[sc_s[i]])
                yield
                for i in range(16):
                    k.op("dve", lambda e, i=i: e.max(out=tops.t[:, i, 8:16], in_=grp(i)), r=[sc_s[i]], w=[tops_s[i]])
                yield
                for i in range(16):
                    k.op("dve", lambda e, i=i: e.max_index(out=idxu.t[:, i, 8:16], in_max=tops.t[:, i, 8:16], in_values=grp(i)),
                         r=[sc_s[i], tops_s[i]], w=[idxu_s[i]])
                k.op("dve", lambda e: e.tensor_copy(out=idxf.t[:], in_=idxu.t[:]), r=idxu_s, w=[idxf])
                yield
                tv = tops.t[:].rearrange("p (h c) a -> p h c a", c=2)
                candv = big8.t[:].rearrange("p (h a b) -> p h a b", a=16, b=16)
                k.op("dve", lambda e: e.tensor_tensor(out=candv, in0=tv[:, :, 0, :].unsqueeze(3).to_broadcast([128, 8, 16, 16]),
                                                      in1=tv[:, :, 1, :].unsqueeze(2).to_broadcast([128, 8, 16, 16]), op=ALU.add),
                     r=tops_s, w=cand_s)
                yield

                def cnd(h):
                    return big8.t[:, h * 256:(h + 1) * 256]
                for h in range(8):
                    k.op("dve", lambda e, h=h: e.max(out=best.t[:, h, 0:8], in_=cnd(h)), r=[cand_s[h]], w=[best_s[h]])
                yield
                for h in range(8):
                    k.op("dve", lambda e, h=h: e.max_index(out=posu.t[:, h, 0:8], in_max=best.t[:, h, 0:8], in_values=cnd(h)),
                         r=[cand_s[h], best_s[h]], w=[posu_s[h]])
                yield
                for h in range(8):
                    k.op("dve", lambda e, h=h: e.match_replace(out=cnd(h), in_to_replace=best.t[:, h, 0:8], in_values=cnd(h),
                                                              imm_value=NEG), r=[best_s[h], cand_s[h]], w=[cand_s[h]])
                yield
                for h in range(8):
                    k.op("dve", lambda e, h=h: e.max(out=best.t[:, h, 8:16], in_=cnd(h)), r=[cand_s[h]], w=[best_s[h]])
                yield
                for h in range(8):
                    k.op("dve", lambda e, h=h: e.max_index(out=posu.t[:, h, 8:16], in_max=best.t[:, h, 8:16], in_values=cnd(h)),
                         r=[cand_s[h], best_s[h]], w=[posu_s[h]])
                yield
                k.op("dve", lambda e: e.tensor_single_scalar(out=apu.t[:], in_=posu.t[:], scalar=4, op=ALU.logical_shift_right),
                     r=posu_s, w=[apu])
                k.op("dve", lambda e: e.tensor_single_scalar(out=bpu.t[:], in_=posu.t[:], scalar=15, op=ALU.bitwise_and),
                     r=posu_s, w=[bpu])
                k.op("dve", lambda e: e.tensor_copy(out=apf.t[:], in_=apu.t[:]), r=[apu], w=[apf])
                k.op("dve", lambda e: e.tensor_copy(out=bpf.t[:], in_=bpu.t[:]), r=[bpu], w=[bpf])
                yield
                Ev = big8.t[:].rearrange("p (h r a) -> p h r a", r=16, a=16)
                iov = iota16.t[:].unsqueeze(1).unsqueeze(1).to_broadcast([128, 8, 16, 16])
                fv = idxf.t[:].rearrange("p (h c) a -> p h c a", c=2)
                for which, pf in ((0, apf), (1, bpf)):
                    k.op("dve", lambda e, pf=pf: e.tensor_tensor(out=Ev, in0=pf.t[:].unsqueeze(3).to_broadcast([128, 8, 16, 16]),
                                                                in1=iov, op=ALU.is_equal), r=[pf, iota16] + cand_s, w=cand_s)
                    yield
                    k.op("dve", lambda e, which=which: e.tensor_tensor(
                        out=Ev, in0=Ev, in1=fv[:, :, which, :].unsqueeze(2).to_broadcast([128, 8, 16, 16]), op=ALU.mult),
                        r=[idxf] + cand_s, w=cand_s)
                    yield
                    k.op("dve", lambda e, which=which: e.tensor_reduce(
                        out=sel.t[:, which, :].rearrange("p (h r) -> p h r", r=16), in_=Ev, axis=AX.X, op=ALU.add),
                        r=cand_s, w=[sel])
                    yield
                k.op("dve", lambda e: e.tensor_tensor(out=exb.t[:], in0=best.t[:], in1=best.t[:, :, 0:1].to_broadcast([128, 8, 16]),
                                                      op=ALU.subtract), r=best_s, w=[exb])
                k.act(exb.t[:], exb.t[:], AF.Exp, r=[exb], w=[exb])
                k.op("dve", lambda e: e.tensor_reduce(out=sm8.t[:], in_=exb.t[:], axis=AX.X, op=ALU.add), r=[exb], w=[sm8])
                k.op("dve", lambda e: e.reciprocal(out=sm8.t[:], in_=sm8.t[:]), r=[sm8], w=[sm8])
                k.op("dve", lambda e: e.tensor_tensor(out=sel.t[:, 2, :].rearrange("p (h r) -> p h r", r=16), in0=exb.t[:],
                                                      in1=sm8.t[:].unsqueeze(2).to_broadcast([128, 8, 16]), op=ALU.mult),
                     r=[exb, sm8], w=[sel])
                yield
                for w3 in range(3):
                    k.tr(pq.t[:, w3 * 128:(w3 + 1) * 128], sel.t[:, w3, :], identf.t[:], r=[sel, identf], w=[pq])
                k.op("act", lambda e, blk=blk: e.copy(out=trip.t[:, :, blk * 128:(blk + 1) * 128],
                                                      in_=pq.t[:, 0:384].rearrange("p (a t) -> p a t", t=128)),
                     r=[pq], w=[trip])
                yield

        GT_s = [T("GT_lo"), T("GT_hi")]
        gbank = _V()
        gbank.t = ps_tp.t[:].bitcast(F32)
        gbank.s = ps_tp.s

        def ggen_half(g, half):
            trip = trip2[g % 2]
            i0_ = half * 64
            io_q = iota128.t[:].unsqueeze(1).to_broadcast([128, 8, 128])
            io_p = iota128.t[:, i0_:i0_ + 64].unsqueeze(1).to_broadcast([128, 8, 64])
            slabs = {}

            def build(sl):
                t0 = sl * 8
                Pm, Qm = Pm_r.next(), Qm_r.next()
                slabs[sl] = (Pm, Qm)
                k.op("dve", lambda e: e.tensor_tensor(
                    out=Qm.t[:], in0=io_q, in1=trip.t[:, 1, t0:t0 + 8].unsqueeze(2).to_broadcast([128, 8, 128]), op=ALU.is_equal),
                    r=[iota128, trip], w=[Qm])
                k.op("dve", lambda e: e.tensor_tensor(
                    out=Pm.t[:], in0=io_p, in1=trip.t[:, 0, t0:t0 + 8].unsqueeze(2).to_broadcast([128, 8, 64]),
                    op=ALU.is_equal), r=[iota128, trip], w=[Pm])
                k.op("pool", lambda e: e.tensor_tensor(
                    out=Pm.t[:], in0=Pm.t[:], in1=trip.t[:, 2, t0:t0 + 8].unsqueeze(2).to_broadcast([128, 8, 64]),
                    op=ALU.mult), r=[Pm, trip], w=[Pm])

            def consume(sl):
                t0 = sl * 8
                Pm, Qm = slabs.pop(sl)
                for t in range(8):
                    k.mm(gbank.t[:, t * 64:(t + 1) * 64], Qm.t[:, t, :], Pm.t[:, t, :], True, True, r=[Qm, Pm], w=[gbank])
                k.op("act", lambda e: e.copy(out=GT.t[:, i0_:i0_ + 64, t0:t0 + 8],
                                             in_=gbank.t[:].rearrange("p (t i) -> p i t", t=8)),
                     r=[gbank], w=[GT_s[half]])

            build(0)
            yield
            build(1)
            yield
            for sl in range(32):
                consume(sl)
                if sl + 2 < 32:
                    build(sl + 2)
                yield

        ybank = [pb[1], pb[2], pb[3], pb[4]]
        tp0 = _V()
        tp0.t = pb[0].t[:].bitcast(BF16)
        tp0.s = pb[0].s
        pu_slots = [(pb[5].t[:, 0:256], pb[5].s), (pb[6].t[:, 0:256], pb[6].s)]

        def step(gens):
            for gx in gens:
                next(gx, None)

        def drain(gens):
            for gx in gens:
                for _ in gx:
                    pass

        def eloop(g, first_gens, second_gens):
            hfT = hfT2[g % 2]
            n_i = n_eg * 4
            n_half = 64
            bufs = {}

            def u_part(i):
                eg, c = i // 4, i % 4
                if c == 0:
                    ub, vb = ub_r.next(), vb_r.next()
                    k.dma(ub.t[:], us_s[eg], ub, w=[ub])
                    k.dma(vb.t, vs_s[eg], vb, w=[vb])
                    bufs[eg] = (ub, vb)
                ub, vb = bufs[eg]
                pu_ap, pu_s = pu_slots[i % 2]
                for kc in range(8):
                    k.mm(pu_ap, ub.t[:, kc * 512 + c * 128:kc * 512 + (c + 1) * 128], hfT.t[:, kc, :], kc == 0, kc == 7,
                         r=[ub, hfT], w=[pu_s])
                gl = gl_r.next()
                k.act(gl.t[:], pu_ap, AF.Gelu, r=[pu_s], w=[gl])
                ga = ga_r.next()
                gts = GT_s[0] if i < 64 else GT_s[1]
                k.op("dve", lambda e, ga=ga, gl=gl, i=i: e.tensor_tensor(out=ga.t[:], in0=gl.t[:], in1=GT.t[:, i, :], op=ALU.mult),
                     r=[gl, gts], w=[ga])
                return ga

            def v_part(i, ga):
                eg, c = i // 4, i % 4
                ub, vb = bufs[eg]
                for blk in range(2):
                    for half in range(2):
                        k.mm(ybank[blk * 2 + half].t[:], ga.t[:, blk * 128:(blk + 1) * 128],
                             vb.t[:, c * 1024 + half * 512:c * 1024 + (half + 1) * 512], i == 0, i == n_i - 1,
                             r=[ga, vb], w=[ybank[blk * 2 + half]])

            gas = {0: u_part(0)}
            flush_stores()
            for i in range(n_i):
                if i == n_half:
                    drain(first_gens)
                if i + 1 < n_i:
                    if i + 1 == n_half:
                        drain(first_gens)
                    gas[i + 1] = u_part(i + 1)
                v_part(i, gas.pop(i))
                if i >= 1:
                    step(first_gens if i < n_half else second_gens)
            drain(first_gens)
            drain(second_gens)

        pending_stores = []

        def flush_stores():
            while pending_stores:
                dst, xo = pending_stores.pop(0)
                k.dma(dst, xo.t[:], xo, r=[xo])

        def finalize(g):
            xts = xts_of.pop(g)
            for blk in range(2):
                xt = xts[blk]
                xo = xt
                for half in range(2):
                    k.op("dve", lambda e, xo=xo, xt=xt, blk=blk, half=half: e.tensor_tensor(
                        out=xo.t[:, half * 512:(half + 1) * 512], in0=ybank[blk * 2 + half].t[:],
                        in1=xt.t[:, half * 512:(half + 1) * 512], op=ALU.add), r=[ybank[blk * 2 + half], xt], w=[xo])
                rs, rs_s = rstd_of(xo.t[:], D, [xo])
                k.op("dve", lambda e, xo=xo, rs=rs: e.scalar_tensor_tensor(out=xo.t[:], in0=xo.t[:], scalar=rs, in1=gfin.t[:],
                                                                         op0=ALU.mult, op1=ALU.mult), r=[xo, rs_s, gfin], w=[xo])
                row0 = (g * 2 + blk) * 128
                pending_stores.append((out_d[row0:row0 + 128, :], xo))

        for _ in phase1(0):
            pass
        drain([ggen_half(0, 0), ggen_half(0, 1)])
        for g in range(n_grp):
            first = []
            if g > 0:
                first.append(ggen_half(g, 1))
            if g + 1 < n_grp:
                first.append(phase1(g + 1))
            second = [ggen_half(g + 1, 0)] if g + 1 < n_grp else []
            eloop(g, first, second)
            finalize(g)
            trip = trip2[g % 2]
            if dbg == "p2" and g == n_grp - 1:
                dt_ = k.sb("dbgt", [128, 1024], F32)
                k.op("pool", lambda e: e.tensor_copy(out=dt_.t[:, 0:768], in_=trip.t[:].rearrange("p a t -> p (a t)")), r=[trip], w=[dt_])
                k.dma(dbg_d[:, 0:768], dt_.t[:, 0:768], dt_, r=[dt_])
    if do_p2:
        flush_stores()
    k.pop_scope()

    P.state = dict(locals())
    return P


def finish(P):
    P.k.S.barrier()
    P.k.S.emit()
    P.st.close()
    return P.nc


def _bf(a):
    return np.asarray(a, dtype=np.float32).astype(ml_dtypes.bfloat16)


def make_in_maps(x, mem, positions, norm_mix, w_in, q_a_norm, w_q_b, kv_a_norm, w_kv_b,
                 swa_sinks, out_norm_mla, out_norm_swa, w_out, norm_cross, norm_mem,
                 w_cq, w_ck, w_cv, w_co, norm_ffn, peer_w_q, peer_keys, peer_u, peer_v, norm_final):
    f32 = np.float32
    x = np.asarray(x, f32)
    mem = np.asarray(mem, f32)
    positions = np.asarray(positions, np.int32)
    w_in0 = np.asarray(w_in[0], f32)
    shared = {}
    shared["identb"] = np.eye(128, dtype=f32).astype(ml_dtypes.bfloat16)
    shared["identf"] = np.eye(128, dtype=f32)
    inv = (np.float32(10000.0) ** (-np.arange(32, dtype=f32) / np.float32(32))).astype(f32)
    shared["invf"] = np.ascontiguousarray(np.broadcast_to(inv[None, :], (128, 32))).astype(f32)
    kk = np.arange(128, dtype=f32)
    kaug = np.zeros((2, 2, 128), f32)
    kaug[0, :, :] = 1.0
    kaug[1, 0, :] = kk - 128.0
    kaug[1, 1, :] = kk
    shared["kaug"] = _bf(kaug.reshape(2, 256))
    slopes = 2.0 ** (-(np.arange(1, 9, dtype=f32)))
    qaug = np.zeros((2, 2, 4, 128), f32)
    for kvh in range(2):
        for g in range(4):
            s = slopes[kvh * 4 + g]
            qaug[0, kvh, g, :] = -s * kk * 8.0
            qaug[1, kvh, g, :] = s * 8.0
    shared["qaug"] = _bf(qaug.reshape(2, 1024))
    shared["iota128"] = np.ascontiguousarray(np.broadcast_to(kk[None, :], (128, 128))).astype(f32)
    shared["iota16"] = np.ascontiguousarray(np.broadcast_to(kk[None, :16], (128, 16))).astype(f32)
    gc = np.zeros((128, NGC), f32)

    def cols(vec, c0):
        v = np.asarray(vec, f32).reshape(-1, 128)
        gc[:, c0:c0 + v.shape[0]] = v.T

    cols(norm_mix[0], G_MIX)
    cols(norm_cross[0], G_CROSS)
    cols(norm_mem[0], G_MEM)
    cols(norm_ffn[0], G_FFN)
    cols(q_a_norm[0], G_QA)
    cols(kv_a_norm[0], G_KVA)
    cols(np.concatenate([np.asarray(out_norm_mla[0], f32), np.asarray(out_norm_swa[0], f32)]), G_OUT)
    shared["gcols"] = gc
    shared["kvarow"] = np.asarray(kv_a_norm[0], f32).reshape(1, 128)
    shared["sinks"] = np.asarray(swa_sinks[0], f32).reshape(1, 8)
    shared["gfin"] = np.asarray(norm_final, f32).reshape(1, D)
    shared["w_kvl"] = np.ascontiguousarray(w_in0[:, 256:448])
    shared["w_in"] = w_in0
    wqb = np.asarray(w_q_b[0], f32).reshape(256, 4, 192)
    shared["w_qb"] = np.ascontiguousarray(np.concatenate([wqb[:, :, :128].reshape(256, 512),
                                                          wqb[:, :, 128:].reshape(256, 256)], axis=1))
    wkvb = np.asarray(w_kv_b[0], f32).reshape(128, 4, 256)
    shared["w_ukT"] = np.ascontiguousarray(wkvb[:, :, :128].transpose(2, 1, 0)).reshape(128, 512)
    shared["w_uv"] = np.ascontiguousarray(wkvb[:, :, 128:]).reshape(128, 512)
    shared["w_out"] = np.asarray(w_out[0], f32)
    shared["w_cq"] = np.asarray(w_cq[0], f32)
    shared["w_ck"] = np.asarray(w_ck[0], f32)
    shared["w_cv"] = np.asarray(w_cv[0], f32)
    shared["w_co"] = np.asarray(w_co[0], f32)
    shared["w_pq"] = np.asarray(peer_w_q[0], f32)
    pk = np.asarray(peer_keys[0], f32)
    shared["keysT"] = np.ascontiguousarray(pk.transpose(3, 0, 1, 2)).reshape(128, 2048)
    shared["uT"] = np.ascontiguousarray(np.asarray(peer_u[0], f32).T)
    shared["v"] = np.asarray(peer_v[0], f32)

    tri_c = (kk[:, None] <= kk[None, :]).astype(f32)
    tri_p = (kk[:, None] > kk[None, :]).astype(f32)
    in_maps = []
    for c in range(NCORE):
        b, r = c // 4, c % 4
        qb = own_blocks(r)
        m = dict(shared)
        m["xb"] = x[b]
        xo = x[b].reshape(NBLK, 128, D)
        m["xown"] = np.ascontiguousarray(xo[qb]).reshape(NOWN * 128, D)
        prev = [max(n - 1, 0) for n in qb]
        m["xprev"] = np.ascontiguousarray(xo[prev]).reshape(NOWN * 128, D)
        m["memb"] = mem[b]
        pb_ = positions[b].reshape(NBLK, 128)
        m["posb"] = np.ascontiguousarray(pb_.T)
        m["poso"] = np.ascontiguousarray(pb_[qb].T)
        mm_ = np.zeros((NOWN, 128, 4, 128), f32)
        sm_ = np.zeros((NOWN, 128, 2, 128), f32)
        for ki, n in enumerate(qb):
            L = n_keyblocks(ki)
            for jj in range(4):
                j = L - 4 + jj
                if j < n:
                    mm_[ki, :, jj, :] = 1.0
                elif j == n:
                    mm_[ki, :, jj, :] = tri_c
            if n > 0:
                sm_[ki, :, 0, :] = tri_p
            sm_[ki, :, 1, :] = tri_c
        m["mmask"] = _bf(mm_.reshape(NOWN, 128, 512))
        m["smask"] = _bf(sm_.reshape(NOWN, 128, 256))
        in_maps.append(m)
    return in_maps


def kernel(**inputs):
    in_maps = make_in_maps(**inputs)
    P = build()
    nc = finish(P)
    res = run_bass_kernel_spmd(nc, in_maps, core_ids=list(range(NCORE)))
    out = np.zeros((2, SEQ, D), np.float32)
    for c in range(NCORE):
        b, r = c // 4, c % 4
        qb = own_blocks(r)
        o = np.asarray(res.results[c]["out"], np.float32).reshape(NOWN, 128, D)
        ov = out[b].reshape(NBLK, 128, D)
        ov[qb] = o
    return out
```
